# Optimizing a Trainium2 kernel written in Bass

```python
import jax
import jax.numpy as jnp
from jax import lax
import numpy as np

D_MODEL = 1024
BATCH = 8
SEQ = 8192
DEPTH = 1

D_RNN = 1024
RNN_BLOCKS = 8
RNN_BW = D_RNN // RNN_BLOCKS
CONV_WIDTH = 4
LRU_C = 8.0
HEAD_DIM = 128
HEADS_PER_GROUP = 4
ATTN_GROUPS = ((128, 1), (512, 4), (2048, 16))
N_GROUPS = 3
ATTN_WIDTH = HEADS_PER_GROUP * HEAD_DIM
ROPE_THETA = 500000.0
ROPE_DIM = HEAD_DIM // 4
ATTN_BLK = 64
NEG_INF = -1e30
D_IN = 2 * D_RNN + 3 * N_GROUPS * ATTN_WIDTH + 2 * D_MODEL
N_EXPERTS = 16
CAPACITY_FACTOR = 2
D_FF = 2048
RMS_EPS = 1e-6

kernel_name = 'hybrid_rglru_dilated_attn_ec_moe'


def _rmsnorm(x, g):
    xf = x.astype(jnp.float32)
    y = xf * lax.rsqrt(jnp.mean(xf * xf, axis=-1, keepdims=True) + RMS_EPS)
    return (y * g.astype(jnp.float32)).astype(x.dtype)


def _split_points():
    widths = [D_RNN, D_RNN] + [ATTN_WIDTH] * (3 * N_GROUPS) + [D_MODEL, D_MODEL]
    pts, acc = [], 0
    for w in widths[:-1]:
        acc += w
        pts.append(acc)
    return pts


def _centred_depthwise_conv(x, w, b):
    S = x.shape[1]
    left = CONV_WIDTH // 2
    right = CONV_WIDTH - 1 - left
    xp = jnp.pad(x, ((0, 0), (left, right), (0, 0)))
    y = b
    for tap in range(CONV_WIDTH):
        y = y + w[tap] * xp[:, tap:tap + S]
    return y


def _linear_scan_combine(earlier, later):
    a1, b1 = earlier
    a2, b2 = later
    return a1 * a2, a2 * b1 + b2


def _rg_lru(x, w_gates, b_gates, lam, reverse):
    B, S, C = x.shape
    xb = x.reshape(B, S, RNN_BLOCKS, RNN_BW)
    gates = jnp.einsum('bsni,gnij->gbsnj', xb, w_gates).reshape(2, B, S, C)
    gates = gates.astype(jnp.float32) + b_gates.astype(jnp.float32)[:, None, None, :]
    r = jax.nn.sigmoid(gates[0])
    i = jax.nn.sigmoid(gates[1])
    log_a = -LRU_C * r * jax.nn.softplus(-lam.astype(jnp.float32))
    a = jnp.exp(log_a)
    u = x.astype(jnp.float32) * i * jnp.sqrt(-jnp.expm1(2.0 * log_a))
    _, h = lax.associative_scan(_linear_scan_combine, (a, u), axis=1, reverse=reverse)
    return h


def _rotary_tables(S):
    pos = jnp.arange(S, dtype=jnp.float32)
    inv = ROPE_THETA ** (-jnp.arange(0, ROPE_DIM, 2, dtype=jnp.float32) / ROPE_DIM)
    ang = pos[:, None] * inv[None, :]
    return jnp.cos(ang), jnp.sin(ang)


def _partial_rotary(t, cos, sin):
    half = ROPE_DIM // 2
    tf = t.astype(jnp.float32)
    x1, x2, rest = tf[..., :half], tf[..., half:ROPE_DIM], tf[..., ROPE_DIM:]
    c = cos[None, :, None, :]
    s = sin[None, :, None, :]
    out = jnp.concatenate([x1 * c - x2 * s, x2 * c + x1 * s, rest], axis=-1)
    return out.astype(t.dtype)


def _dilated_window_attention(q, k, v, radius, dilation):
    B, S, H, Dh = q.shape
    L = S // dilation
    nblk = -(-L // ATTN_BLK)
    Lp = nblk * ATTN_BLK

    def split(t):
        return t.reshape(B, L, dilation, H, Dh).transpose(0, 2, 3, 1, 4)

    qb = jnp.pad(split(q), ((0, 0), (0, 0), (0, 0), (0, Lp - L), (0, 0)))
    qb = qb.reshape(B, dilation, H, nblk, ATTN_BLK, Dh)

    def windows(t):
        tp = jnp.pad(split(t), ((0, 0), (0, 0), (0, 0), (ATTN_BLK, ATTN_BLK + Lp - L), (0, 0)))
        tp = tp.reshape(B, dilation, H, nblk + 2, ATTN_BLK, Dh)
        return jnp.concatenate([tp[:, :, :, :-2], tp[:, :, :, 1:-1], tp[:, :, :, 2:]], axis=4)

    kw = windows(k)
    vw = windows(v)
    s = jnp.einsum('brhnqd,brhnkd->brhnqk', qb, kw).astype(jnp.float32) * (Dh ** -0.5)
    jq = jnp.arange(nblk)[:, None] * ATTN_BLK + jnp.arange(ATTN_BLK)[None, :]
    jk = (jnp.arange(nblk)[:, None] - 1) * ATTN_BLK + jnp.arange(3 * ATTN_BLK)[None, :]
    rel = jk[:, None, :] - jq[:, :, None]
    valid = (jnp.abs(rel) <= radius) & (jk[:, None, :] >= 0) & (jk[:, None, :] < L)
    s = jnp.where(valid, s, NEG_INF)
    m = jnp.max(s, axis=-1, keepdims=True)
    p = jnp.exp(s - m)
    l = jnp.sum(p, axis=-1, keepdims=True)
    o = jnp.einsum('brhnqk,brhnkd->brhnqd', p, vw.astype(jnp.float32)) / l
    lse = (m + jnp.log(l))[..., 0]

    def merge(t):
        t = t.reshape((B, dilation, H, Lp) + t.shape[5:])[:, :, :, :L]
        t = jnp.moveaxis(t, 3, 1)
        return t.reshape((B, S, H) + t.shape[4:])

    return merge(o), merge(lse)


def _mixer_sublayer(x, cos, sin, norm_g, w_in, b_in, conv_w, conv_b, rg_w, rg_b, rg_lambda,
                    p_rnn, p_attn, w_out):
    B, S, _ = x.shape
    h = _rmsnorm(x, norm_g)
    proj = jnp.einsum('bsd,de->bse', h, w_in) + b_in
    parts = jnp.split(proj, _split_points(), axis=-1)
    xr, gr = parts[0], parts[1]
    q_parts = parts[2:2 + N_GROUPS]
    k_parts = parts[2 + N_GROUPS:2 + 2 * N_GROUPS]
    v_parts = parts[2 + 2 * N_GROUPS:2 + 3 * N_GROUPS]
    gate_a, gate_b = parts[-2], parts[-1]

    xc = _centred_depthwise_conv(xr, conv_w, conv_b)
    h_rnn = (_rg_lru(xc, rg_w[0], rg_b[0], rg_lambda[0], False)
             + _rg_lru(xc, rg_w[1], rg_b[1], rg_lambda[1], True))
    y_rnn = (jax.nn.gelu(gr.astype(jnp.float32)) * h_rnn).astype(x.dtype)
    branch_a = jnp.einsum('bsc,cd->bsd', y_rnn, p_rnn)

    outs, lses = [], []
    for g, (window, dilation) in enumerate(ATTN_GROUPS):
        shp = (B, S, HEADS_PER_GROUP, HEAD_DIM)
        q = _partial_rotary(q_parts[g].reshape(shp), cos, sin)
        k = _partial_rotary(k_parts[g].reshape(shp), cos, sin)
        v = v_parts[g].reshape(shp)
        o, lse = _dilated_window_attention(q, k, v, window // (2 * dilation), dilation)
        outs.append(o)
        lses.append(lse)
    wts = jax.nn.softmax(jnp.stack(lses, axis=0), axis=0)
    o = jnp.sum(wts[..., None] * jnp.stack(outs, axis=0), axis=0)
    y_attn = o.reshape(B, S, ATTN_WIDTH).astype(x.dtype)
    branch_b = jnp.einsum('bsc,cd->bsd', y_attn, p_attn)

    merged = jax.nn.sigmoid(gate_a) * branch_a + jax.nn.sigmoid(gate_b) * branch_b
    return x + jnp.einsum('bsd,de->bse', merged, w_out)


def _expert_choice_sublayer(x, norm_g, w_router, b_router, w_gate, w_up, w_down):
    B, S, _ = x.shape
    h = _rmsnorm(x, norm_g)
    logits = jnp.einsum('bsd,de->bse', h, w_router).astype(jnp.float32) + b_router.astype(jnp.float32)
    aff = jax.nn.softmax(logits, axis=-1)
    cap = CAPACITY_FACTOR * S // N_EXPERTS
    gates, idx = lax.top_k(jnp.swapaxes(aff, 1, 2), cap)
    bidx = jnp.arange(B)[:, None, None]
    xg = h[bidx, idx]
    hid = jax.nn.silu(jnp.einsum('becd,edf->becf', xg, w_gate)) * jnp.einsum('becd,edf->becf', xg, w_up)
    eo = jnp.einsum('becf,efd->becd', hid, w_down) * gates[..., None].astype(x.dtype)
    y = jnp.zeros_like(x).at[bidx, idx].add(eo)
    return x + y


def setup_inputs(seed: int = 0) -> dict:
    key = jax.random.key(seed)
    ks = jax.random.split(key, 20)
    f32 = jnp.float32
    nrm = lambda k, shape, scale: jax.random.normal(k, shape, f32) * scale
    u = jax.random.uniform(ks[8], (DEPTH, 2, D_RNN), f32, 0.9, 0.999)
    s_init = u ** (1.0 / LRU_C)
    return {
        'x': jax.random.normal(ks[0], (BATCH, SEQ, D_MODEL), f32),
        'norm_mix': 1.0 + nrm(ks[1], (DEPTH, D_MODEL), 0.05),
        'w_in': nrm(ks[2], (DEPTH, D_MODEL, D_IN), D_MODEL ** -0.5),
        'b_in': nrm(ks[3], (DEPTH, D_IN), 0.02),
        'conv_w': nrm(ks[4], (DEPTH, CONV_WIDTH, D_RNN), CONV_WIDTH ** -0.5),
        'conv_b': nrm(ks[5], (DEPTH, D_RNN), 0.02),
        'rg_w': nrm(ks[6], (DEPTH, 2, 2, RNN_BLOCKS, RNN_BW, RNN_BW), RNN_BW ** -0.5),
        'rg_b': nrm(ks[7], (DEPTH, 2, 2, D_RNN), 0.02),
        'rg_lambda': jnp.log(s_init) - jnp.log1p(-s_init),
        'p_rnn': nrm(ks[9], (DEPTH, D_RNN, D_MODEL), D_RNN ** -0.5),
        'p_attn': nrm(ks[10], (DEPTH, ATTN_WIDTH, D_MODEL), ATTN_WIDTH ** -0.5),
        'w_out': nrm(ks[11], (DEPTH, D_MODEL, D_MODEL), D_MODEL ** -0.5),
        'norm_ffn': 1.0 + nrm(ks[12], (DEPTH, D_MODEL), 0.05),
        'w_router': nrm(ks[13], (DEPTH, D_MODEL, N_EXPERTS), D_MODEL ** -0.5),
        'b_router': nrm(ks[14], (DEPTH, N_EXPERTS), 0.01),
        'w_gate': nrm(ks[15], (DEPTH, N_EXPERTS, D_MODEL, D_FF), D_MODEL ** -0.5),
        'w_up': nrm(ks[16], (DEPTH, N_EXPERTS, D_MODEL, D_FF), D_MODEL ** -0.5),
        'w_down': nrm(ks[17], (DEPTH, N_EXPERTS, D_FF, D_MODEL), D_FF ** -0.5),
        'norm_final': 1.0 + nrm(ks[18], (D_MODEL,), 0.05),
    }


def reference(x, norm_mix, w_in, b_in, conv_w, conv_b, rg_w, rg_b, rg_lambda, p_rnn, p_attn,
              w_out, norm_ffn, w_router, b_router, w_gate, w_up, w_down, norm_final):
    cos, sin = _rotary_tables(x.shape[1])
    for layer in range(DEPTH):
        x = _mixer_sublayer(x, cos, sin, norm_mix[layer], w_in[layer], b_in[layer],
                            conv_w[layer], conv_b[layer], rg_w[layer], rg_b[layer],
                            rg_lambda[layer], p_rnn[layer], p_attn[layer], w_out[layer])
        x = _expert_choice_sublayer(x, norm_ffn[layer], w_router[layer], b_router[layer],
                                    w_gate[layer], w_up[layer], w_down[layer])
    return _rmsnorm(x, norm_final)
```

```python
import numpy as np
from contextlib import ExitStack
import concourse.bass as bass
import concourse.mybir as mybir
from concourse.bass_utils import run_bass_kernel_spmd

F32 = mybir.dt.float32
F32R = mybir.dt.float32r
BF16 = mybir.dt.bfloat16
I32 = mybir.dt.int32
AF = mybir.ActivationFunctionType
ALU = mybir.AluOpType
AX = mybir.AxisListType

ENGS = ("sync", "scalar", "vector", "gpsimd", "tensor")
EPOCH = 30000
S = 8192
D = 1024
NCH = 68
XW = 1048


class Res:
    __slots__ = ("name", "w", "r", "acc")

    def __init__(self, name, acc=False):
        self.name = name
        self.w = {}
        self.r = {}
        self.acc = acc


class Prog:
    def __init__(self, nc, same_engine_sync=("scalar", "vector", "gpsimd")):
        self.nc = nc
        self.ops = {e: [] for e in ENGS}
        self.cnt = {e: 0 for e in ENGS}
        self.seen = {e: {} for e in ENGS}
        self.dma_vals = {}
        self.same = set(same_engine_sync)
        self.sem_keys = []
        self.final = {}

    def _key(self, k):
        if k not in self.final:
            self.sem_keys.append(k)
        return k

    def op(self, eng, fn, reads=(), writes=(), dma=None):
        need = {}
        for r in reads:
            for k, v in r.w.items():
                if need.get(k, 0) < v:
                    need[k] = v
        for w in writes:
            if not w.acc:
                for k, v in w.w.items():
                    if need.get(k, 0) < v:
                        need[k] = v
            for k, v in w.r.items():
                if need.get(k, 0) < v:
                    need[k] = v
        waits = []
        seen = self.seen[eng]
        for k, v in need.items():
            if seen.get(k, 0) >= v:
                continue
            if k[0] == eng and eng not in self.same:
                continue
            waits.append((k, v))
            seen[k] = v
        if dma is None:
            c = self.cnt[eng]
            self.cnt[eng] = c + 1
            key = self._key((eng, c // EPOCH))
            ev = (key, c % EPOCH + 1)
            inc = 1
        else:
            key = self._key(("dma", dma))
            val = self.dma_vals.get(key, 0) + 16
            self.dma_vals[key] = val
            ev = (key, val)
            inc = 16
        self.final[key] = ev[1]
        for r in reads:
            if r.r.get(ev[0], 0) < ev[1]:
                r.r[ev[0]] = ev[1]
        for w in writes:
            if w.acc:
                if w.w.get(ev[0], 0) < ev[1]:
                    w.w[ev[0]] = ev[1]
            else:
                w.w = {ev[0]: ev[1]}
                w.r = {}
        self.ops[eng].append((waits, fn, key, inc))

    def wait_all(self, eng, own=False):
        waits = []
        for k, v in self.final.items():
            if self.seen[eng].get(k, 0) < v and (own or k[0] != eng):
                waits.append((k, v))
                self.seen[eng][k] = v
        if waits:
            self.ops[eng].append((waits, None, None, 0))

    def barrier(self):
        for e in ENGS:
            self.wait_all(e, own=(e in self.same))

    def emit(self, st):
        nc = self.nc
        if not hasattr(self, "sems"):
            self.sems = {}
        sems = self.sems
        for k in self.sem_keys:
            if k not in sems:
                sems[k] = st.enter_context(nc.semaphore("s_" + "_".join(str(x) for x in k)))
        self.nblk = getattr(self, "nblk", 0) + 1
        with nc.named_scope(f"ph{self.nblk}"), nc.Block() as block:
            for e in ENGS:
                ops = self.ops[e]
                if not ops:
                    continue

                def body(eng, ops=ops):
                    for waits, fn, key, inc in ops:
                        for k, v in waits:
                            eng.wait_ge(sems[k], v)
                        if fn is not None:
                            fn(eng).then_inc(sems[key], inc)
                getattr(block, e)(body)
        self.ops = {e: [] for e in ENGS}


def chunk_kind(c):
    if c < 8:
        return "xr"
    if c < 16:
        return "gr"
    if c < 28:
        return "q"
    if c < 40:
        return "k"
    if c < 52:
        return "v"
    return "gate"


DIL = (1, 4, 16)


def build(phases=6, debug=False):
    nc = bass.Bass("TRN2", target_bir_lowering=False)

    def din(name, shape, dt=F32):
        return nc.dram_tensor(name, list(shape), dt, kind="ExternalInput").ap()

    def dscr(name, shape, dt=F32):
        kind = "ExternalOutput" if debug else "Internal"
        return nc.dram_tensor(name, list(shape), dt, kind=kind).ap()

    xT = din("xT", [D, S])
    xtok = din("xtok", [S, D])
    w_in = din("w_in", [NCH, 128, 8, 128])
    b_in = din("b_in", [128, NCH])
    gmix = din("gmix", [128, 8])
    convw = din("convw", [128, 4, 8])
    convb = din("convb", [128, 8])
    rgw = din("rgw", [8, 128, 4, 128])
    rgb = din("rgb", [128, 4, 8])
    lam = din("lam", [128, 2, 8])
    prnn = din("prnn", [128, 8, D])
    pattn = din("pattn", [128, 4, D])
    wout = din("wout", [128, 8, D])
    gffn = din("gffn", [128, D])
    gfin = din("gfin", [128, D])
    wr = din("wr", [128, 8, 16])
    br = din("br", [128, 16])
    wg = din("wg", [16, 16, 128, 8, 128])
    wu = din("wu", [16, 16, 128, 8, 128])
    wd = din("wd", [16, 2, 128, 16, 512])
    cosT = din("cosT", [64, S])
    sinT = din("sinT", [64, S])
    cst = din("cst", [128, 128 + 128 + 384 + 64])

    out = nc.dram_tensor("out", [S, D], F32, kind="ExternalOutput").ap()

    XR = dscr("XR", [8, 128, S])
    GG = dscr("GG", [8, 128, S])
    QKV = dscr("QKV", [36, 128, S], BF16)
    SG = dscr("SG", [16, 128, S])
    YT = dscr("YT", [8, 128, S])
    OT = dscr("OT", [4, 128, S])
    X2 = dscr("X2", [S, D])
    H2X = dscr("H2X", [S, XW])
    XG = [dscr(f"XG{e}", [1024, XW]) for e in range(16)]

    P = Prog(nc)
    op = P.op
    R2 = lambda n: [Res(n + "0"), Res(n + "1")]
    bregs = {}

    def bc_reg(e, val):
        if val not in bregs:
            r = e.alloc_register(f"bc{val}")
            e.reg_mov(r, val)
            bregs[val] = r
        return bregs[val]
    glob = ExitStack()

    def sb(st, name, shape, dt=F32):
        return st.enter_context(nc.sbuf_tensor(name, list(shape), dt))

    ident = sb(glob, "ident", [128, 128])
    ident_bf = sb(glob, "ident_bf", [128, 128], BF16)
    ut_bf = sb(glob, "ut_bf", [128, 128], BF16)
    ones_bf = sb(glob, "ones_bf", [128, 128], BF16)
    ones_r = sb(glob, "ones_r", [128, 128], F32R)
    pmask = [sb(glob, f"pmask{i}", [128, 512], BF16) for i in range(3)]
    cst_t = sb(glob, "cst_t", [128, 704])
    one_c = sb(glob, "one_c", [128, 1])
    eps_c = sb(glob, "eps_c", [128, 1])
    AFFt = sb(glob, "AFFt", [128, 64, 16])
    R_const = Res("const")
    R_aff = Res("aff")
    op("sync", lambda e: e.dma_start(out=cst_t[:], in_=cst), writes=[R_const], dma="cst")
    op("vector", lambda e: e.tensor_copy(out=ident[:], in_=cst_t[:, 0:128]), reads=[R_const], writes=[R_const])
    op("vector", lambda e: e.tensor_copy(out=ident_bf[:], in_=cst_t[:, 0:128]), reads=[R_const], writes=[R_const])
    op("vector", lambda e: e.tensor_copy(out=ut_bf[:], in_=cst_t[:, 128:256]), reads=[R_const], writes=[R_const])
    for i_, (o0_, o1_) in enumerate(((64, 64), (128, 64), (64, 0))):
        op("vector", lambda e, i_=i_, o0_=o0_: e.tensor_copy(out=pmask[i_][:, 0:256], in_=cst_t[:, 256 + o0_:512 + o0_]),
           reads=[R_const], writes=[R_const])
        op("vector", lambda e, i_=i_, o1_=o1_: e.tensor_copy(out=pmask[i_][:, 256:512], in_=cst_t[:, 256 + o1_:512 + o1_]),
           reads=[R_const], writes=[R_const])
    op("vector", lambda e: e.memset(ones_bf[:], 1.0), writes=[R_const])
    op("vector", lambda e: e.memset(ones_r[:].bitcast(F32), 1.0), writes=[R_const])
    op("vector", lambda e: e.memset(one_c[:], 1.0), writes=[R_const])
    op("vector", lambda e: e.memset(eps_c[:], 1e-6), writes=[R_const])
    RC = [R_const]

    def _phase1():
        with ExitStack() as ph:
            hT = sb(ph, "hT", [128, 8, 2048], F32R)
            xs = [sb(ph, f"xs{i}", [128, 8, 512]) for i in range(2)]
            rstd2 = [sb(ph, f"rstd{i}", [128, 512]) for i in range(2)]
            R_rstd2 = [Res("rstd0"), Res("rstd1")]
            wch = [sb(ph, f"wch{i}", [128, 8, 128], F32R) for i in range(3)]
            stg = [sb(ph, f"stg{i}", [128, 2048]) for i in range(4)]
            stgb = [sb(ph, f"stgb{i}", [128, 2048], BF16) for i in range(4)]
            rtmp = [sb(ph, f"rtmp{i}", [64, 2048]) for i in range(2)]
            cos_t = sb(ph, "cos_t", [64, 2048])
            sin_t = sb(ph, "sin_t", [64, 2048])
            bin_t = sb(ph, "bin_t", [128, NCH])
            binq_t = sb(ph, "binq_t", [128, NCH])
            gmix_t = sb(ph, "gmix_t", [128, 8])
            pb = [ph.enter_context(nc.psum_tensor(f"pbA{i}", [128, 512], F32)) for i in range(8)]
            R_pb = [Res(f"pbA{i}") for i in range(8)]
            R_hTt = [Res(f"hT{j}") for j in range(4)]
            R_xs = [Res("xs0"), Res("xs1")]
            R_wch = [Res(f"wch{i}") for i in range(3)]
            R_stg = [Res(f"stg{i}") for i in range(4)]
            R_stgb = [Res(f"stgb{i}") for i in range(4)]
            R_rtmp = [Res("rtmp0"), Res("rtmp1")]
            R_cs = Res("cossin")
            R_scrA = Res("scrA", acc=True)
            QSC = 128.0 ** -0.5

            op("sync", lambda e: e.dma_start(out=bin_t[:], in_=b_in), writes=RC, dma="cst")
            op("sync", lambda e: e.dma_start(out=gmix_t[:], in_=gmix), writes=RC, dma="cst")
            op("vector", lambda e: e.tensor_scalar(out=binq_t[:], in0=bin_t[:], scalar1=QSC, scalar2=None, op0=ALU.mult),
               reads=RC, writes=RC)
            xT_v = xT.rearrange("(k p) t -> p k t", p=128)

            def load_x(st_, j_):
                tt_ = st_ * 2048 + j_ * 512
                op("sync", lambda e, j_=j_, tt_=tt_: e.dma_start(out=xs[j_ % 2][:], in_=xT_v[:, :, tt_:tt_ + 512]),
                   writes=[R_xs[j_ % 2]], dma=f"xs{j_ % 2}")

            def issue_w(idx):
                cc = idx % NCH
                op("gpsimd", lambda e, idx=idx, cc=cc: e.dma_start(out=wch[idx % 3][:], in_=w_in[cc]),
                   writes=[R_wch[idx % 3]], dma=f"wch{idx % 3}")
            pbi = 0
            wi = 0
            si = 0
            pending = []
            for st_i in range(4):
                t0 = st_i * 2048
                op("sync", lambda e, t0=t0: e.dma_start(out=cos_t[:], in_=cosT[:, t0:t0 + 2048]), writes=[R_cs], dma="cs")
                op("sync", lambda e, t0=t0: e.dma_start(out=sin_t[:], in_=sinT[:, t0:t0 + 2048]), writes=[R_cs], dma="cs")
                if st_i == 0:
                    load_x(0, 0)
                    load_x(0, 1)
                for j in range(4):
                    xb = xs[j % 2]
                    Rx = R_xs[j % 2]
                    rs = rstd2[j % 2]
                    Rrs = R_rstd2[j % 2]
                    js_ = slice(j * 512, (j + 1) * 512)
                    op("scalar", lambda e, xb=xb, js_=js_: e.activation(out=hT[:, :, js_], in_=xb[:], func=AF.Square),
                       reads=[Rx], writes=[R_hTt[j]])
                    pbk = pbi % 8
                    pbi += 1
                    for k in range(8):
                        op("tensor", lambda e, k=k, pbk=pbk, js_=js_: e.matmul(pb[pbk][:], lhsT=ones_r[:], rhs=hT[:, k, js_],
                                                                              start=(k == 0), stop=(k == 7)),
                           reads=[R_hTt[j]] + RC, writes=[R_pb[pbk]])
                    op("scalar", lambda e, pbk=pbk, rs=rs: e.activation(out=rs[:], in_=pb[pbk][:], func=AF.Sqrt,
                                                                        bias=eps_c[:, 0:1], scale=1.0 / D),
                       reads=[R_pb[pbk]] + RC, writes=[Rrs])
                    op("vector", lambda e, rs=rs: e.reciprocal(out=rs[:], in_=rs[:]), reads=[Rrs], writes=[Rrs])
                    for k in range(8):
                        op("vector", lambda e, k=k, xb=xb, js_=js_, rs=rs: e.scalar_tensor_tensor(
                            out=hT[:, k, js_], in0=xb[:, k, :], scalar=gmix_t[:, k:k + 1],
                            in1=rs[:], op0=ALU.mult, op1=ALU.mult),
                           reads=[Rx, Rrs] + RC, writes=[R_hTt[j]])
                    if j + 2 < 4:
                        load_x(st_i, j + 2)
                for c in range(NCH):
                    kind = chunk_kind(c)
                    wb = wch[wi % 3]
                    Rw = R_wch[wi % 3]
                    if wi == 0:
                        issue_w(0)
                        issue_w(1)
                    if wi + 2 < 4 * NCH:
                        issue_w(wi + 2)
                    wi += 1
                    if c == 56 and st_i + 1 < 4:
                        load_x(st_i + 1, 0)
                        load_x(st_i + 1, 1)
                    sgi = si % 4
                    si += 1
                    use_b = kind in ("q", "k", "v")
                    for j in range(4):
                        pbk = pbi % 8
                        pbi += 1
                        for k in range(8):
                            op("tensor", lambda e, k=k, pbk=pbk, wb=wb, j=j: e.matmul(
                                pb[pbk][:], lhsT=wb[:, k, :], rhs=hT[:, k, j * 512:(j + 1) * 512],
                                start=(k == 0), stop=(k == 7)),
                               reads=[Rw, R_hTt[j]], writes=[R_pb[pbk]])
                        js = slice(j * 512, (j + 1) * 512)
                        if kind == "xr":
                            fn = lambda e, pbk=pbk, js=js, c=c, sgi=sgi: e.activation(
                                out=stg[sgi][:, js], in_=pb[pbk][:], func=AF.Identity, bias=bin_t[:, c:c + 1], scale=1.0)
                            wr_ = [R_stg[sgi]]
                        elif kind == "gr":
                            fn = lambda e, pbk=pbk, js=js, c=c, sgi=sgi: e.activation(
                                out=stg[sgi][:, js], in_=pb[pbk][:], func=AF.Gelu, bias=bin_t[:, c:c + 1], scale=1.0)
                            wr_ = [R_stg[sgi]]
                        elif kind == "gate":
                            fn = lambda e, pbk=pbk, js=js, c=c, sgi=sgi: e.activation(
                                out=stg[sgi][:, js], in_=pb[pbk][:], func=AF.Sigmoid, bias=bin_t[:, c:c + 1], scale=1.0)
                            wr_ = [R_stg[sgi]]
                        elif kind == "q":
                            fn = lambda e, pbk=pbk, js=js, c=c, sgi=sgi: e.activation(
                                out=stg[sgi][:, js], in_=pb[pbk][:], func=AF.Identity, bias=binq_t[:, c:c + 1], scale=QSC)
                            wr_ = [R_stg[sgi]]
                        elif kind == "k":
                            fn = lambda e, pbk=pbk, js=js, c=c, sgi=sgi: e.activation(
                                out=stg[sgi][:, js], in_=pb[pbk][:], func=AF.Identity, bias=bin_t[:, c:c + 1], scale=1.0)
                            wr_ = [R_stg[sgi]]
                        else:
                            dv = DIL[((c - 16) % 12) // 4]
                            nj = 512 // dv
                            fn = lambda e, pbk=pbk, j=j, c=c, sgi=sgi, dv=dv, nj=nj: e.activation(
                                out=stgb[sgi][:].rearrange("p (r l) -> p r l", r=dv)[:, :, j * nj:(j + 1) * nj],
                                in_=pb[pbk][:].rearrange("p (l r) -> p r l", r=dv),
                                func=AF.Identity, bias=bin_t[:, c:c + 1], scale=1.0)
                            wr_ = [R_stgb[sgi]]
                        op("scalar", fn, reads=[R_pb[pbk]] + RC, writes=wr_)
                    tail_ops = []
                    if use_b:
                        g = ((c - 16) % 12) // 4
                        d = DIL[g]
                        sgt = stg[sgi]
                        sbt = stgb[sgi]
                        if kind in ("q", "k"):
                            op("vector", lambda e, sgt=sgt: e.tensor_tensor(
                                out=rtmp[0][0:32, :], in0=sgt[32:64, :], in1=sin_t[32:64, :], op=ALU.mult),
                               reads=[R_stg[sgi], R_cs], writes=[R_rtmp[0]])
                            op("vector", lambda e, sgt=sgt: e.tensor_tensor(
                                out=rtmp[0][32:64, :], in0=sgt[0:32, :], in1=sin_t[0:32, :], op=ALU.mult),
                               reads=[R_stg[sgi], R_cs], writes=[R_rtmp[0]])
                            op("vector", lambda e, sgt=sgt: e.tensor_tensor(
                                out=rtmp[1][:, :], in0=sgt[0:64, :], in1=cos_t[:, :], op=ALU.mult),
                               reads=[R_cs, R_stg[sgi]], writes=[R_rtmp[1]])
                            op("vector", lambda e, sgt=sgt: e.tensor_tensor(
                                out=sgt[0:64, :], in0=rtmp[0][:, :], in1=rtmp[1][:, :], op=ALU.add),
                               reads=[R_rtmp[0], R_rtmp[1]], writes=[R_stg[sgi]])
                            tail_ops.append(("scalar", lambda e, sgt=sgt, sbt=sbt, d=d: e.activation(
                                out=sbt[:].rearrange("p (r j) -> p r j", r=d),
                                in_=sgt[:].rearrange("p (j r) -> p r j", r=d), func=AF.Copy),
                                [R_stg[sgi]], [R_stgb[sgi]], None))
                        qi = c - 16
                        j0 = t0 // d
                        nj = 2048 // d
                        dst = QKV[qi].rearrange("p (r l) -> p r l", r=d)[:, :, j0:j0 + nj]
                        src = sbt[:].rearrange("p (r j) -> p r j", r=d)
                        tail_ops.append(("sync", lambda e, dst=dst, src=src: e.dma_start(out=dst, in_=src),
                                         [R_stgb[sgi]], [R_scrA], f"stgb{sgi}"))
                    else:
                        if kind == "xr":
                            dst = XR[c][:, t0:t0 + 2048]
                        elif kind == "gr":
                            dst = GG[c - 8][:, t0:t0 + 2048]
                        else:
                            dst = SG[c - 52][:, t0:t0 + 2048]
                        tail_ops.append(("sync", lambda e, dst=dst, sgi=sgi: e.dma_start(out=dst, in_=stg[sgi][:]),
                                         [R_stg[sgi]], [R_scrA], f"stg{sgi}"))
                    for (en_, fn_, rd_, wr2_, dm_) in pending:
                        op(en_, fn_, reads=rd_, writes=wr2_, dma=dm_)
                    pending[:] = tail_ops
            for (en_, fn_, rd_, wr2_, dm_) in pending:
                op(en_, fn_, reads=rd_, writes=wr2_, dma=dm_)
            P.barrier()
            P.emit(glob)
    _phase1()
    if phases <= 1:
        return finish(nc, P, glob, out)

    SEG = 1024
    NSEG = S // SEG
    def _phase2():
        with ExitStack() as ph:
            xc2 = [sb(ph, f"xc{i}", [128, S], F32R) for i in range(2)]
            hf = sb(ph, "hf", [128, S])
            raw = [sb(ph, f"raw{i}", [128, SEG + 3]) for i in range(2)]
            xcr = [sb(ph, f"xcr{i}", [128, SEG]) for i in range(2)]
            r_ = [sb(ph, f"r_{i}", [128, SEG]) for i in range(2)]
            i_ = [sb(ph, f"i_{i}", [128, SEG]) for i in range(2)]
            a2_ = [sb(ph, f"a2_{i}", [128, SEG]) for i in range(2)]
            hb_ = [sb(ph, f"hb_{i}", [128, SEG]) for i in range(2)]
            gg_ = [sb(ph, f"gg_{i}", [128, SEG]) for i in range(2)]
            ys_ = [sb(ph, f"ys_{i}", [128, SEG]) for i in range(2)]
            rgw_t = [sb(ph, f"rgw_t{i}", [128, 4, 128], F32R) for i in range(2)]
            rgb_t = sb(ph, "rgb_t", [128, 4, 8])
            kap = sb(ph, "kap", [128, 2, 8])
            kap2 = sb(ph, "kap2", [128, 2, 8])
            hrgb_t = sb(ph, "hrgb_t", [128, 4, 8])
            quarter_c = sb(ph, "quarter_c", [128, 1])
            cw_t = sb(ph, "cw_t", [128, 4, 8])
            cb_t = sb(ph, "cb_t", [128, 8])
            pb = [ph.enter_context(nc.psum_tensor(f"pbB{i}", [128, 512], F32)) for i in range(8)]
            R_pb = [Res(f"pbB{i}") for i in range(8)]
            R_xc2, R_hf = R2("xc"), Res("hf")
            R_raw, R_xcr, R_r, R_i, R_a2, R_hb, R_gg, R_ys, R_rgw = (R2("raw"), R2("xcr"), R2("r"), R2("i"), R2("a2"),
                                                                     R2("hb"), R2("gg"), R2("ys"), R2("rgw"))
            R_scrB = Res("scrB", acc=True)
            op("sync", lambda e: e.dma_start(out=rgb_t[:], in_=rgb), writes=RC, dma="cst")
            op("sync", lambda e: e.dma_start(out=kap[:], in_=lam), writes=RC, dma="cst")
            op("sync", lambda e: e.dma_start(out=cw_t[:], in_=convw), writes=RC, dma="cst")
            op("sync", lambda e: e.dma_start(out=cb_t[:], in_=convb), writes=RC, dma="cst")
            op("vector", lambda e: e.tensor_scalar(out=hrgb_t[:], in0=rgb_t[:], scalar1=0.5, scalar2=None, op0=ALU.mult),
               reads=RC, writes=RC)
            op("vector", lambda e: e.memset(quarter_c[:], 0.25), writes=RC)
            op("scalar", lambda e: e.activation(out=kap[:], in_=kap[:], func=AF.Exp, scale=-1.0), reads=RC, writes=RC)
            op("scalar", lambda e: e.activation(out=kap[:], in_=kap[:], func=AF.Ln, bias=one_c[:, 0:1], scale=1.0),
               reads=RC, writes=RC)
            op("vector", lambda e: e.tensor_scalar(out=kap2[:], in0=kap[:], scalar1=-4.0, scalar2=None, op0=ALU.mult),
               reads=RC, writes=RC)
            op("vector", lambda e: e.tensor_scalar(out=kap[:], in0=kap[:], scalar1=-8.0, scalar2=None, op0=ALU.mult),
               reads=RC, writes=RC)
            steps = []
            for c in range(8):
                for dirn in range(2):
                    for sgm in (range(NSEG) if dirn == 0 else range(NSEG - 1, -1, -1)):
                        steps.append((c, dirn, sgm))
            pbs = [0]

            def load_w(c):
                op("gpsimd", lambda e, c=c: e.dma_start(out=rgw_t[c % 2][:], in_=rgw[c]), writes=[R_rgw[c % 2]], dma=f"rgw{c % 2}")

            def stageX(n):
                c, dirn, sgm = steps[n]
                b2 = n % 2
                xc, R_xc = xc2[c % 2], R_xc2[c % 2]
                wt, Rwt = rgw_t[c % 2], R_rgw[c % 2]
                t0 = sgm * SEG
                ts_ = slice(t0, t0 + SEG)
                if dirn == 0 and sgm == 0 and c + 1 < 8:
                    load_w(c + 1)
                if dirn == 0:
                    rw = raw[b2]
                    lo = max(t0 - 2, 0)
                    hi = min(t0 + SEG + 1, S)
                    if sgm == 0:
                        op("vector", lambda e, rw=rw: e.memset(rw[:, 0:2], 0.0), writes=[R_raw[b2]])
                    if sgm == NSEG - 1:
                        op("vector", lambda e, rw=rw: e.memset(rw[:, SEG + 2:SEG + 3], 0.0), writes=[R_raw[b2]])
                    o0 = lo - (t0 - 2)
                    op("sync", lambda e, rw=rw, c=c, lo=lo, hi=hi, o0=o0: e.dma_start(
                        out=rw[:, o0:o0 + hi - lo], in_=XR[c][:, lo:hi]), writes=[R_raw[b2]], dma=f"raw{b2}")
                    ct = xcr[b2]
                    op("vector", lambda e, rw=rw, c=c, ct=ct: e.tensor_scalar(
                        out=ct[:], in0=rw[:, 0:SEG], scalar1=cw_t[:, 0, c:c + 1], scalar2=cb_t[:, c:c + 1],
                        op0=ALU.mult, op1=ALU.add), reads=[R_raw[b2]] + RC, writes=[R_xcr[b2]])
                    for tap in range(1, 4):
                        op("vector", lambda e, rw=rw, c=c, ts_=ts_, tap=tap, xc=xc, ct=ct: e.scalar_tensor_tensor(
                            out=(xc[:, ts_] if tap == 3 else ct[:]), in0=rw[:, tap:tap + SEG],
                            scalar=cw_t[:, tap, c:c + 1], in1=ct[:], op0=ALU.mult, op1=ALU.add),
                           reads=[R_raw[b2], R_xcr[b2]] + RC, writes=([R_xc] if tap == 3 else [R_xcr[b2]]))
                else:
                    op("sync", lambda e, b2=b2, c=c, ts_=ts_: e.dma_start(out=gg_[b2][:], in_=GG[c][:, ts_]),
                       writes=[R_gg[b2]], dma=f"gg{b2}")
                for j in range(SEG // 512):
                    js = slice(j * 512, (j + 1) * 512)
                    for gt, dstt, Rd in ((0, r_[b2], R_r[b2]), (1, i_[b2], R_i[b2])):
                        pbk = pbs[0] % 8
                        pbs[0] += 1
                        gi = dirn * 2 + gt
                        op("tensor", lambda e, pbk=pbk, wt=wt, gi=gi, xc=xc, j=j, t0=t0: e.matmul(
                            pb[pbk][:], lhsT=wt[:, gi, :], rhs=xc[:, t0 + j * 512:t0 + (j + 1) * 512], start=True, stop=True),
                           reads=[Rwt, R_xc], writes=[R_pb[pbk]])
                        op("scalar", lambda e, pbk=pbk, dstt=dstt, js=js, gi=gi, c=c: e.activation(
                            out=dstt[:, js], in_=pb[pbk][:], func=AF.Tanh, bias=hrgb_t[:, gi, c:c + 1], scale=0.5),
                           reads=[R_pb[pbk]] + RC, writes=[Rd])
                rr, aa = r_[b2], a2_[b2]
                op("scalar", lambda e, rr=rr, aa=aa, dirn=dirn, c=c: e.activation(
                    out=aa[:], in_=rr[:], func=AF.Exp, scale=kap[:, dirn, c:c + 1], bias=kap[:, dirn, c:c + 1]),
                   reads=[R_r[b2]] + RC, writes=[R_a2[b2]])
                op("scalar", lambda e, rr=rr, dirn=dirn, c=c: e.activation(
                    out=rr[:], in_=rr[:], func=AF.Exp, scale=kap2[:, dirn, c:c + 1], bias=kap2[:, dirn, c:c + 1]),
                   reads=RC, writes=[R_r[b2]])
                op("scalar", lambda e, aa=aa: e.activation(
                    out=aa[:], in_=aa[:], func=AF.Sqrt, bias=quarter_c[:, 0:1], scale=-0.25),
                   reads=RC, writes=[R_a2[b2]])

            def stageY(n):
                c, dirn, sgm = steps[n]
                b2 = n % 2
                xc, R_xc = xc2[c % 2], R_xc2[c % 2]
                t0 = sgm * SEG
                ts_ = slice(t0, t0 + SEG)
                rr, ii, aa = r_[b2], i_[b2], a2_[b2]
                op("vector", lambda e, aa=aa, ts_=ts_, xc=xc: e.tensor_tensor(
                    out=aa[:], in0=aa[:], in1=xc[:, ts_].bitcast(F32), op=ALU.mult),
                   reads=[R_xc], writes=[R_a2[b2]])
                op("vector", lambda e, ii=ii, aa=aa: e.scalar_tensor_tensor(
                    out=ii[:], in0=ii[:], scalar=1.0, in1=aa[:], op0=ALU.add, op1=ALU.mult),
                   reads=[R_a2[b2]], writes=[R_i[b2]])
                if dirn == 0:
                    init = 0.0 if sgm == 0 else hf[:, t0 - 1:t0]
                    op("vector", lambda e, rr=rr, ii=ii, ts_=ts_, init=init: e.tensor_tensor_scan(
                        out=hf[:, ts_], data0=rr[:], data1=ii[:], initial=init, op0=ALU.mult, op1=ALU.add),
                       reads=[R_r[b2], R_i[b2]], writes=[R_hf])
                else:
                    hb = hb_[b2]
                    if sgm == NSEG - 1:
                        init = 0.0
                        rd_extra = []
                    else:
                        init = hb_[1 - b2][:, 0:1]
                        rd_extra = [R_hb[1 - b2]]
                    op("vector", lambda e, rr=rr, ii=ii, hb=hb, init=init: e.tensor_tensor_scan(
                        out=hb[:, ::-1], data0=rr[:, ::-1], data1=ii[:, ::-1], initial=init,
                        op0=ALU.mult, op1=ALU.add),
                       reads=[R_r[b2], R_i[b2]] + rd_extra, writes=[R_hb[b2]])
                    gg = gg_[b2]
                    ys = ys_[b2]
                    op("vector", lambda e, ys=ys, hb=hb, ts_=ts_: e.tensor_tensor(
                        out=ys[:], in0=hb[:], in1=hf[:, ts_], op=ALU.add),
                       reads=[R_hb[b2], R_hf], writes=[R_ys[b2]])
                    op("vector", lambda e, ys=ys, gg=gg: e.tensor_tensor(out=ys[:], in0=ys[:], in1=gg[:], op=ALU.mult),
                       reads=[R_gg[b2]], writes=[R_ys[b2]])
                    op("sync", lambda e, ys=ys, c=c, ts_=ts_: e.dma_start(out=YT[c][:, ts_], in_=ys[:]),
                       reads=[R_ys[b2]], writes=[R_scrB], dma=f"ys{b2}")

            load_w(0)
            stageX(0)
            for n in range(len(steps)):
                if n + 1 < len(steps):
                    stageX(n + 1)
                stageY(n)
            P.barrier()
            P.emit(glob)
    _phase2()
    if phases <= 2:
        return finish(nc, P, glob, out)

    def _phase3():
        with ExitStack() as ph:
            Qd = [sb(ph, f"Qd{i}", [128, S], BF16) for i in range(2)]
            Kd = [sb(ph, f"Kd{i}", [128, S], BF16) for i in range(2)]
            Vd = sb(ph, "Vd", [128, S], BF16)
            Vt = sb(ph, "Vt", [128, 64, 128], BF16)
            NUM = sb(ph, "NUM", [128, S])
            DEN = sb(ph, "DEN", [128, S])
            pT = [sb(ph, f"pT{i}", [128, 512], BF16) for i in range(4)]
            ostg = [sb(ph, f"ostg{i}", [128, 1024]) for i in range(2)]
            ps_s = [ph.enter_context(nc.psum_tensor(f"ps_s{i}", [128, 512], F32)) for i in range(3)]
            ps_n = [ph.enter_context(nc.psum_tensor(f"ps_n{i}", [128, 512], F32)) for i in range(2)]
            ps_d = [ph.enter_context(nc.psum_tensor(f"ps_d{i}", [128, 512], F32)) for i in range(2)]
            ps_t = [ph.enter_context(nc.psum_tensor(f"ps_t{i}", [128, 512], BF16)) for i in range(1)]
            R_Qd, R_Kd = R2("Qd"), R2("Kd")
            R_Vd, R_Vt, R_NUM, R_DEN = Res("Vd"), Res("Vt"), Res("NUM"), Res("DEN")
            R_pT = [Res(f"pT{i}") for i in range(4)]
            R_ostg = R2("ostg")
            R_pss, R_psn, R_psd, R_pst = R2("pss"), R2("psn"), R2("psd"), R2("pst")
            R_pss4 = [Res(f"pss4{i}") for i in range(4)]
            R_scrC = Res("scrC", acc=True)
            ucnt = 0
            pcnt = 0
            for h in range(4):
                for g in range(3):
                    d = DIL[g]
                    L = S // d
                    u2 = ucnt % 2
                    ucnt += 1
                    qd, kd = Qd[u2], Kd[u2]
                    op("sync", lambda e, qd=qd, g=g, h=h: e.dma_start(out=qd[:], in_=QKV[g * 4 + h]),
                       writes=[R_Qd[u2]], dma=f"Qd{u2}")
                    op("sync", lambda e, kd=kd, g=g, h=h: e.dma_start(out=kd[:], in_=QKV[12 + g * 4 + h]),
                       writes=[R_Kd[u2]], dma=f"Kd{u2}")
                    op("sync", lambda e, g=g, h=h: e.dma_start(out=Vd[:], in_=QKV[24 + g * 4 + h]),
                       writes=[R_Vd], dma="Vd")
                    tviews = [(ps_t[0][:], R_pst[0])] + [(ps_s[i_][:].bitcast(BF16)[:, 0:512], R_pss4[i_]) for i_ in range(3)]
                    for m4 in range(16):
                        tv, Rtv = tviews[m4 % 4]
                        for q in range(4):
                            m = m4 * 4 + q
                            op("tensor", lambda e, tv=tv, q=q, m=m: e.transpose(
                                out=tv[:, q * 128:(q + 1) * 128], in_=Vd[:, m * 128:(m + 1) * 128], identity=ident_bf[:]),
                               reads=[R_Vd] + RC, writes=[Rtv])
                        if m4 % 2 == 0:
                            op("vector", lambda e, tv=tv, m4=m4: e.tensor_copy(
                                out=Vt[:, m4 * 4:(m4 + 1) * 4, :].rearrange("p a b -> p (a b)"), in_=tv),
                               reads=[Rtv], writes=[R_Vt])
                        else:
                            op("scalar", lambda e, tv=tv, m4=m4: e.activation(
                                out=Vt[:, m4 * 4:(m4 + 1) * 4, :].rearrange("p a b -> p (a b)"), in_=tv, func=AF.Copy),
                               reads=[Rtv], writes=[R_Vt])
                    contribs = []
                    for m in range(64):
                        p0 = m * 128
                        r = p0 // L
                        lo, hi = r * L, (r + 1) * L
                        if p0 == lo:
                            qs, var = p0, 1
                        elif p0 + 128 == hi:
                            qs, var = p0 - 128, 2
                        else:
                            qs, var = p0 - 64, 0
                        qe = qs + 256
                        parts = []
                        sb0 = (qs // 512) * 512
                        while sb0 < qe:
                            a, b = max(qs, sb0), min(qe, sb0 + 512)
                            parts.append((sb0 // 512, a, b))
                            sb0 += 512
                        contribs.append((qs, qe, var, parts))
                    last_of = {}
                    for m, (_, _, _, parts) in enumerate(contribs):
                        for (sbi, a, b) in parts:
                            last_of[sbi] = m
                    started = set()
                    LOOK = 2

                    def front(pi):
                        s3 = pi % 3
                        pt = pT[pi % 4]
                        Rp = R_pT[pi % 4]
                        for hf_ in range(2):
                            m = 2 * pi + hf_
                            qs, qe, var, parts = contribs[m]
                            op("tensor", lambda e, s3=s3, hf_=hf_, m=m, qs=qs, qe=qe, kd=kd, qd=qd: e.matmul(
                                ps_s[s3][:, hf_ * 256:(hf_ + 1) * 256], lhsT=kd[:, m * 128:(m + 1) * 128], rhs=qd[:, qs:qe],
                                start=True, stop=True),
                               reads=[R_Qd[u2], R_Kd[u2]], writes=[R_pss4[s3]])
                        v0, v1 = contribs[2 * pi][2], contribs[2 * pi + 1][2]
                        pm = pmask[{(0, 0): 0, (1, 0): 1, (0, 2): 2}[(v0, v1)]]
                        op("scalar", lambda e, s3=s3, pt=pt: e.activation(out=pt[:], in_=ps_s[s3][:], func=AF.Exp),
                           reads=[R_pss4[s3]], writes=[Rp])
                        op("vector", lambda e, pt=pt, pm=pm: e.tensor_tensor(out=pt[:], in0=pt[:], in1=pm[:], op=ALU.mult),
                           reads=RC, writes=[Rp])

                    def back(pi):
                        pt = pT[pi % 4]
                        Rp = R_pT[pi % 4]
                        for hf_ in range(2):
                            m = 2 * pi + hf_
                            qs, qe, var, parts = contribs[m]
                            for (sbi, a, b) in parts:
                                n2 = sbi % 2
                                first = sbi not in started
                                started.add(sbi)
                                c0, c1 = a - sbi * 512, b - sbi * 512
                                r0, r1 = hf_ * 256 + a - qs, hf_ * 256 + b - qs
                                op("tensor", lambda e, n2=n2, m=m, pt=pt, r0=r0, r1=r1, c0=c0, c1=c1, first=first: e.matmul(
                                    ps_n[n2][:, c0:c1], lhsT=Vt[:, m, :], rhs=pt[:, r0:r1], start=first, stop=False,
                                    skip_group_check=True),
                                   reads=[R_Vt, Rp], writes=[R_psn[n2]])
                                op("tensor", lambda e, n2=n2, pt=pt, r0=r0, r1=r1, c0=c0, c1=c1, first=first: e.matmul(
                                    ps_d[n2][:, c0:c1], lhsT=ones_bf[:], rhs=pt[:, r0:r1], start=first, stop=False,
                                    skip_group_check=True),
                                   reads=[Rp] + RC, writes=[R_psd[n2]])
                                if last_of[sbi] == m:
                                    P0 = sbi * 512
                                    r = P0 // L
                                    j0 = P0 - r * L
                                    nat = slice(r + d * j0, r + d * (j0 + 511) + 1, d) if d > 1 else slice(P0, P0 + 512)
                                    if g == 0:
                                        op("vector", lambda e, n2=n2, nat=nat: e.tensor_copy(out=NUM[:, nat], in_=ps_n[n2][:]),
                                           reads=[R_psn[n2]], writes=[R_NUM])
                                        op("scalar", lambda e, n2=n2, nat=nat: e.activation(out=DEN[:, nat], in_=ps_d[n2][:], func=AF.Copy),
                                           reads=[R_psd[n2]], writes=[R_DEN])
                                    else:
                                        op("vector", lambda e, n2=n2, nat=nat: e.tensor_tensor(
                                            out=NUM[:, nat], in0=ps_n[n2][:], in1=NUM[:, nat], op=ALU.add),
                                           reads=[R_psn[n2]], writes=[R_NUM])
                                        op("vector", lambda e, n2=n2, nat=nat: e.tensor_tensor(
                                            out=DEN[:, nat], in0=ps_d[n2][:], in1=DEN[:, nat], op=ALU.add),
                                           reads=[R_psd[n2]], writes=[R_DEN])

                    for mm in range(32 + LOOK):
                        if mm < 32:
                            front(mm)
                        if mm >= LOOK:
                            back(mm - LOOK)
                for pc in range(8):
                    o2 = pc % 2
                    cs = slice(pc * 1024, (pc + 1) * 1024)
                    op("vector", lambda e, cs=cs: e.reciprocal(out=DEN[:, cs], in_=DEN[:, cs]), writes=[R_DEN])
                    op("vector", lambda e, cs=cs, o2=o2: e.tensor_tensor(out=ostg[o2][:], in0=NUM[:, cs], in1=DEN[:, cs], op=ALU.mult),
                       reads=[R_NUM, R_DEN], writes=[R_ostg[o2]])
                    op("sync", lambda e, cs=cs, o2=o2, h=h: e.dma_start(out=OT[h][:, cs], in_=ostg[o2][:]),
                       reads=[R_ostg[o2]], writes=[R_scrC], dma=f"ostg{o2}")
            P.barrier()
            P.emit(glob)
    _phase3()
    if phases <= 3:
        return finish(nc, P, glob, out)

    TD = 256
    def _phase4():
        with ExitStack() as ph:
            prnn_t = sb(ph, "prnn_t", [128, 8, D], F32R)
            pattn_t = sb(ph, "pattn_t", [128, 4, D], F32R)
            wout_t = sb(ph, "wout_t", [128, 8, D], F32R)
            wr_t = sb(ph, "wr_t", [128, 8, 16])
            br_t = sb(ph, "br_t", [128, 16])
            gffn_t = sb(ph, "gffn_t", [128, D])
            yt = [sb(ph, f"yt{i}", [128, 8, TD], F32R) for i in range(2)]
            ot = [sb(ph, f"ot{i}", [128, 4, TD], F32R) for i in range(2)]
            sga = [sb(ph, f"sga{i}", [128, 8, TD]) for i in range(2)]
            sgb = [sb(ph, f"sgb{i}", [128, 8, TD]) for i in range(2)]
            mg = sb(ph, "mg", [128, 8, TD], F32R)
            mtmp = [sb(ph, f"mtmp{i}", [128, TD]) for i in range(2)]
            mtmp2 = [sb(ph, f"mtmpb{i}", [128, TD]) for i in range(2)]
            R_mtmp2 = R2("mtmp2")
            xt_ = [sb(ph, f"xt_{i}", [128, D]) for i in range(4)]
            x2_ = [sb(ph, f"x2_{i}", [128, D]) for i in range(2)]
            h2_ = [sb(ph, f"h2_{i}", [128, XW]) for i in range(2)]
            h2T = sb(ph, "h2T", [128, 8, 128])
            sm = sb(ph, "sm", [128, 8])
            junk = sb(ph, "junk", [128, D])
            lg = sb(ph, "lg", [128, 16])
            pb = [ph.enter_context(nc.psum_tensor(f"pbD{i}", [128, 512], F32)) for i in range(8)]
            R_pb = [Res(f"pbD{i}") for i in range(8)]
            R_wD = Res("wD")
            R_yt, R_ot, R_sga, R_sgb, R_mtmp, R_xt, R_x2, R_h2 = (R2("yt"), R2("ot"), R2("sga"), R2("sgb"), R2("mtmp"),
                                                                  R2("xt"), R2("x2"), R2("h2"))
            R_mg, R_h2T, R_sm, R_junk, R_lg = Res("mg"), Res("h2T"), Res("sm"), Res("junk"), Res("lg")
            R_xt = [Res(f"xt{i}") for i in range(4)]
            R_scrD = Res("scrD", acc=True)
            op("gpsimd", lambda e: e.dma_start(out=prnn_t[:], in_=prnn, max_dma_last_dim=4096), writes=[R_wD], dma="wD")
            op("gpsimd", lambda e: e.dma_start(out=pattn_t[:], in_=pattn, max_dma_last_dim=4096), writes=[R_wD], dma="wD")
            op("gpsimd", lambda e: e.dma_start(out=wout_t[:], in_=wout, max_dma_last_dim=4096), writes=[R_wD], dma="wD")
            op("sync", lambda e: e.dma_start(out=wr_t[:], in_=wr), writes=[R_wD], dma="wD")
            op("sync", lambda e: e.dma_start(out=br_t[:], in_=br), writes=[R_wD], dma="wD")
            op("sync", lambda e: e.dma_start(out=gffn_t[:], in_=gffn), writes=[R_wD], dma="wD")
            pbi = 0
            bcnt = 0
            YT_v = YT.rearrange("c p t -> p c t")
            OT_v = OT.rearrange("c p t -> p c t")
            SG_v = SG.rearrange("c p t -> p c t")
            def loadsD(ti):
                t0 = ti * TD
                b2 = ti % 2
                op("gpsimd", lambda e, b2=b2, t0=t0: e.dma_start(out=yt[b2][:], in_=YT_v[:, :, t0:t0 + TD]),
                   writes=[R_yt[b2]], dma=f"yt{b2}")
                op("gpsimd", lambda e, b2=b2, t0=t0: e.dma_start(out=ot[b2][:], in_=OT_v[:, :, t0:t0 + TD]),
                   writes=[R_ot[b2]], dma=f"ot{b2}")
                op("sync", lambda e, b2=b2, t0=t0: e.dma_start(out=sga[b2][:], in_=SG_v[:, 0:8, t0:t0 + TD]),
                   writes=[R_sga[b2]], dma=f"sga{b2}")
                op("sync", lambda e, b2=b2, t0=t0: e.dma_start(out=sgb[b2][:], in_=SG_v[:, 8:16, t0:t0 + TD]),
                   writes=[R_sgb[b2]], dma=f"sgb{b2}")
                for bb in range(TD // 128):
                    q4 = (ti % 2) * 2 + bb
                    tb = (ti * (TD // 128) + bb) * 128
                    op("sync", lambda e, q4=q4, tb=tb: e.dma_start(out=xt_[q4][:], in_=xtok[tb:tb + 128, :]),
                       writes=[R_xt[q4]], dma=f"xt{q4}")

            def router_tail(q2, blk, tb):
                nonlocal pbi
                pa = pbi % 8
                pbi += 1
                pa2 = pbi % 8
                pbi += 1
                for k in range(8):
                    pp = pa if k < 4 else pa2
                    op("tensor", lambda e, pp=pp, k=k, q2=q2: e.transpose(
                        out=pb[pp][:, (k % 4) * 128:(k % 4 + 1) * 128], in_=h2_[q2][:, k * 128:(k + 1) * 128], identity=ident[:]),
                       reads=[R_h2[q2]] + RC, writes=[R_pb[pp]])
                op("vector", lambda e, pa=pa: e.tensor_copy(out=h2T[:, 0:4, :].rearrange("p a b -> p (a b)"), in_=pb[pa][:]),
                   reads=[R_pb[pa]], writes=[R_h2T])
                op("scalar", lambda e, pa2=pa2: e.activation(out=h2T[:, 4:8, :].rearrange("p a b -> p (a b)"), in_=pb[pa2][:], func=AF.Copy),
                   reads=[R_pb[pa2]], writes=[R_h2T])
                pa = pbi % 8
                pbi += 1
                for k in range(8):
                    op("tensor", lambda e, pa=pa, k=k: e.matmul(
                        pb[pa][:, 0:16], lhsT=h2T[:, k, :], rhs=wr_t[:, k, :], start=(k == 0), stop=(k == 7)),
                       reads=[R_h2T, R_wD], writes=[R_pb[pa]])
                op("vector", lambda e, pa=pa: e.tensor_tensor(out=lg[:], in0=pb[pa][:, 0:16], in1=br_t[:], op=ALU.add),
                   reads=[R_pb[pa], R_wD], writes=[R_lg])
                op("vector", lambda e: e.reduce_max(out=sm[:, 3:4], in_=lg[:], axis=AX.X), reads=[R_lg], writes=[R_sm])
                op("vector", lambda e: e.tensor_scalar(out=sm[:, 3:4], in0=sm[:, 3:4], scalar1=-1.0, scalar2=None, op0=ALU.mult),
                   writes=[R_sm])
                op("scalar", lambda e: e.activation(out=lg[:], in_=lg[:], func=AF.Exp, bias=sm[:, 3:4], scale=1.0,
                                                    accum_out=sm[:, 4:5]), reads=[R_sm], writes=[R_lg, R_sm])
                op("vector", lambda e: e.reciprocal(out=sm[:, 5:6], in_=sm[:, 4:5]), writes=[R_sm])
                op("vector", lambda e, blk=blk: e.tensor_scalar(
                    out=AFFt[:, blk, :], in0=lg[:], scalar1=sm[:, 5:6], scalar2=None, op0=ALU.mult),
                   reads=[R_lg, R_sm], writes=[R_aff])
                op("gpsimd", lambda e, q2=q2, blk=blk: e.tensor_copy(out=h2_[q2][:, D:D + 16], in_=AFFt[:, blk, :]),
                   reads=[R_aff], writes=[R_h2[q2]])
                op("gpsimd", lambda e, q2=q2, blk=blk: e.tensor_copy(out=h2_[q2][:, D + 16:D + 24],
                                                                      in_=cst_t[:, 640 + blk:641 + blk].to_broadcast([128, 8])),
                   reads=RC, writes=[R_h2[q2]])
                op("sync", lambda e, q2=q2, tb=tb: e.dma_start(out=H2X[tb:tb + 128, :], in_=h2_[q2][:]),
                   reads=[R_h2[q2]], writes=[R_scrD], dma=f"h2{q2}")

            pendingD = []
            loadsD(0)
            for ti in range(S // TD):
                t0 = ti * TD
                b2 = ti % 2
                if ti + 1 < S // TD:
                    loadsD(ti + 1)
                for dc in range(8):
                    pa = pbi % 8
                    pbi += 1
                    for k in range(8):
                        op("tensor", lambda e, pa=pa, k=k, dc=dc, b2=b2: e.matmul(
                            pb[pa][:, 0:TD], lhsT=prnn_t[:, k, dc * 128:(dc + 1) * 128], rhs=yt[b2][:, k, :],
                            start=(k == 0), stop=(k == 7)), reads=[R_wD, R_yt[b2]], writes=[R_pb[pa]])
                    for k in range(4):
                        op("tensor", lambda e, pa=pa, k=k, dc=dc, b2=b2: e.matmul(
                            pb[pa][:, TD:2 * TD], lhsT=pattn_t[:, k, dc * 128:(dc + 1) * 128], rhs=ot[b2][:, k, :],
                            start=(k == 0), stop=(k == 3)), reads=[R_wD, R_ot[b2]], writes=[R_pb[pa]])
                    m2 = dc % 2
                    op("vector", lambda e, pa=pa, dc=dc, b2=b2, m2=m2: e.tensor_tensor(
                        out=mtmp[m2][:], in0=pb[pa][:, 0:TD], in1=sga[b2][:, dc, :], op=ALU.mult),
                       reads=[R_pb[pa], R_sga[b2]], writes=[R_mtmp[m2]])
                    op("vector", lambda e, pa=pa, dc=dc, b2=b2, m2=m2: e.tensor_tensor(
                        out=mtmp2[m2][:], in0=pb[pa][:, TD:2 * TD], in1=sgb[b2][:, dc, :], op=ALU.mult),
                       reads=[R_pb[pa], R_sgb[b2]], writes=[R_mtmp2[m2]])
                    op("vector", lambda e, dc=dc, m2=m2: e.tensor_tensor(
                        out=mg[:, dc, :], in0=mtmp2[m2][:], in1=mtmp[m2][:], op=ALU.add),
                       reads=[R_mtmp[m2], R_mtmp2[m2]], writes=[R_mg])
                for bb in range(TD // 128):
                    blk = ti * (TD // 128) + bb
                    tb = blk * 128
                    q2 = bcnt % 2
                    bcnt += 1
                    q4 = (ti % 2) * 2 + bb
                    for hh in range(2):
                        pa = pbi % 8
                        pbi += 1
                        for k in range(8):
                            op("tensor", lambda e, pa=pa, k=k, bb=bb, hh=hh: e.matmul(
                                pb[pa][:], lhsT=mg[:, k, bb * 128:(bb + 1) * 128], rhs=wout_t[:, k, hh * 512:(hh + 1) * 512],
                                start=(k == 0), stop=(k == 7)), reads=[R_mg, R_wD], writes=[R_pb[pa]])
                        op("vector", lambda e, pa=pa, q2=q2, hh=hh, q4=q4: e.tensor_tensor(
                            out=x2_[q2][:, hh * 512:(hh + 1) * 512], in0=pb[pa][:], in1=xt_[q4][:, hh * 512:(hh + 1) * 512],
                            op=ALU.add), reads=[R_pb[pa], R_xt[q4]], writes=[R_x2[q2]])
                    op("sync", lambda e, q2=q2, tb=tb: e.dma_start(out=X2[tb:tb + 128, :], in_=x2_[q2][:]),
                       reads=[R_x2[q2]], writes=[R_scrD], dma=f"x2{q2}")
                    op("scalar", lambda e, q2=q2: e.activation(out=junk[:], in_=x2_[q2][:], func=AF.Square, accum_out=sm[:, 0:1]),
                       reads=[R_x2[q2]], writes=[R_junk, R_sm])
                    op("scalar", lambda e: e.activation(out=sm[:, 1:2], in_=sm[:, 0:1], func=AF.Sqrt, bias=eps_c[:, 0:1], scale=1.0 / D),
                       reads=RC, writes=[R_sm])
                    op("vector", lambda e: e.reciprocal(out=sm[:, 2:3], in_=sm[:, 1:2]), writes=[R_sm])
                    op("vector", lambda e, q2=q2: e.scalar_tensor_tensor(
                        out=h2_[q2][:, 0:D], in0=x2_[q2][:], scalar=sm[:, 2:3], in1=gffn_t[:], op0=ALU.mult, op1=ALU.mult),
                       reads=[R_x2[q2], R_sm, R_wD], writes=[R_h2[q2]])
                    if pendingD:
                        pendingD.pop()()
                    pendingD.append(lambda q2=q2, blk=blk, tb=tb: router_tail(q2, blk, tb))
            if pendingD:
                pendingD.pop()()
            P.barrier()
            P.emit(glob)
    _phase4()
    if phases <= 4:
        return finish(nc, P, glob, out)

    def _phase5():
        with ExitStack() as ph:
            lo_t = sb(ph, "lo_t", [128, 16])
            hi_t = sb(ph, "hi_t", [128, 16])
            mid_t = sb(ph, "mid_t", [128, 16])
            cnt_t = sb(ph, "cnt_t", [128, 16])
            flg_t = sb(ph, "flg_t", [128, 16])
            t1_t = sb(ph, "t1_t", [128, 16])
            cmp_bf = sb(ph, "cmp_bf", [128, 1024], BF16)
            pos_f = sb(ph, "pos_f", [128, 1024])
            tot_f = sb(ph, "tot_f", [128, 1024])
            cum_f = sb(ph, "cum_f", [128, 1024])
            zero_f = sb(ph, "zero_f", [128, 64])
            off_i = sb(ph, "off_i", [128, 1024], I32)
            hrow = [sb(ph, f"hrow{i}", [128, XW]) for i in range(3)]
            xgr = [sb(ph, f"xgr{i}", [128, XW]) for i in range(2)]
            xgT = [sb(ph, f"xgT{i}", [128, 8, 1024], BF16) for i in range(2)]
            gate_t = [sb(ph, f"gate_t{i}", [128, 8]) for i in range(2)]
            tok_i = [sb(ph, f"tok_i{i}", [128, 8], I32) for i in range(2)]
            hidT = sb(ph, "hidT", [128, 16, 1024], BF16)
            wg_t = [sb(ph, f"wg_t{i}", [128, 8, 128], BF16) for i in range(3)]
            wu_t = [sb(ph, f"wu_t{i}", [128, 8, 128], BF16) for i in range(3)]
            wd_t = [sb(ph, f"wd_t{i}", [128, 16, 512], BF16) for i in range(2)]
            sg_t = [sb(ph, f"sg_t{i}", [128, 512]) for i in range(2)]
            eo_t = sb(ph, "eo_t", [128, 8, D])
            pb = [ph.enter_context(nc.psum_tensor(f"pbE{i}", [128, 512], F32)) for i in range(8)]
            R_pb = [Res(f"pbE{i}") for i in range(8)]
            R_bis, R_cmp, R_pos, R_off = Res("bis"), Res("cmp"), Res("pos"), Res("off")
            R_hrow = [Res(f"hrow{i}") for i in range(3)]
            R_xgr, R_xgT, R_gate = R2("xgr"), R2("xgT"), R2("gate")
            R_hid, R_eo = Res("hid"), Res("eo")
            R_wg = [Res(f"wg{i}") for i in range(3)]
            R_wu = [Res(f"wu{i}") for i in range(3)]
            R_wd, R_sg = R2("wd"), R2("sg")
            R_XG = [Res(f"XG{e}", acc=True) for e in range(16)]
            R_x2acc = Res("x2acc")
            aff_flat = AFFt[:].rearrange("p b e -> p (b e)")
            bc = lambda t: t[:].unsqueeze(1).to_broadcast([128, 64, 16])
            aff3 = AFFt[:]
            cmp3 = cmp_bf[:].rearrange("p (b e) -> p b e", e=16)
            op("vector", lambda e: e.memset(lo_t[:], 0.0), writes=[R_bis])
            op("vector", lambda e: e.memset(hi_t[:], 1.0), writes=[R_bis])
            op("vector", lambda e: e.memset(zero_f[:], 0.0), writes=[R_bis])
            for it in range(32):
                op("vector", lambda e: e.tensor_tensor(out=mid_t[:], in0=lo_t[:], in1=hi_t[:], op=ALU.add), writes=[R_bis])
                op("vector", lambda e: e.tensor_scalar(out=mid_t[:], in0=mid_t[:], scalar1=0.5, scalar2=None, op0=ALU.mult),
                   writes=[R_bis])
                op("vector", lambda e: e.tensor_tensor(out=cmp3, in0=aff3, in1=bc(mid_t), op=ALU.is_gt),
                   reads=[R_aff, R_bis], writes=[R_cmp])
                pa, pa2 = (2 * it) % 8, (2 * it + 1) % 8
                op("tensor", lambda e, pa=pa: e.matmul(pb[pa][:], lhsT=ones_bf[:], rhs=cmp_bf[:, 0:512], start=True, stop=True),
                   reads=[R_cmp] + RC, writes=[R_pb[pa]])
                op("tensor", lambda e, pa2=pa2: e.matmul(pb[pa2][:], lhsT=ones_bf[:], rhs=cmp_bf[:, 512:1024], start=True, stop=True),
                   reads=[R_cmp] + RC, writes=[R_pb[pa2]])
                op("vector", lambda e, pa=pa: e.tensor_reduce(
                    out=cnt_t[:], in_=pb[pa][:].rearrange("p (b e) -> p e b", e=16), axis=AX.X, op=ALU.add),
                   reads=[R_pb[pa]], writes=[R_bis])
                op("vector", lambda e, pa2=pa2: e.tensor_reduce(
                    out=t1_t[:], in_=pb[pa2][:].rearrange("p (b e) -> p e b", e=16), axis=AX.X, op=ALU.add),
                   reads=[R_pb[pa2]], writes=[R_bis])
                op("vector", lambda e: e.tensor_tensor(out=cnt_t[:], in0=cnt_t[:], in1=t1_t[:], op=ALU.add), writes=[R_bis])
                op("vector", lambda e: e.tensor_scalar(out=flg_t[:], in0=cnt_t[:], scalar1=1023.5, scalar2=None, op0=ALU.is_ge),
                   writes=[R_bis])
                op("vector", lambda e: e.tensor_tensor(out=t1_t[:], in0=flg_t[:], in1=mid_t[:], op=ALU.mult), writes=[R_bis])
                op("vector", lambda e: e.tensor_tensor(out=lo_t[:], in0=lo_t[:], in1=t1_t[:], op=ALU.max), writes=[R_bis])
                op("vector", lambda e: e.scalar_tensor_tensor(out=t1_t[:], in0=flg_t[:], scalar=2.0, in1=mid_t[:],
                                                              op0=ALU.mult, op1=ALU.add), writes=[R_bis])
                op("vector", lambda e: e.tensor_tensor(out=hi_t[:], in0=hi_t[:], in1=t1_t[:], op=ALU.min), writes=[R_bis])
            op("vector", lambda e: e.tensor_tensor(out=cmp3, in0=aff3, in1=bc(lo_t), op=ALU.is_gt),
               reads=[R_aff, R_bis], writes=[R_cmp])
            for hh in range(2):
                cs = slice(hh * 512, (hh + 1) * 512)
                op("tensor", lambda e, hh=hh, cs=cs: e.matmul(pb[hh][:], lhsT=ut_bf[:], rhs=cmp_bf[:, cs], start=True, stop=True),
                   reads=[R_cmp] + RC, writes=[R_pb[hh]])
                op("tensor", lambda e, hh=hh, cs=cs: e.matmul(pb[2 + hh][:], lhsT=ones_bf[:], rhs=cmp_bf[:, cs], start=True, stop=True),
                   reads=[R_cmp] + RC, writes=[R_pb[2 + hh]])
                op("vector", lambda e, hh=hh, cs=cs: e.tensor_copy(out=pos_f[:, cs], in_=pb[hh][:]), reads=[R_pb[hh]], writes=[R_pos])
                op("vector", lambda e, hh=hh, cs=cs: e.tensor_copy(out=tot_f[:, cs], in_=pb[2 + hh][:]), reads=[R_pb[2 + hh]], writes=[R_pos])
            tot3 = tot_f[:].rearrange("p (b e) -> p e b", e=16)
            cum3 = cum_f[:].rearrange("p (b e) -> p e b", e=16)
            for ee in range(16):
                op("vector", lambda e, ee=ee: e.tensor_tensor_scan(
                    out=cum3[:, ee, :], data0=tot3[:, ee, :], data1=zero_f[:], initial=0.0, op0=ALU.add, op1=ALU.add),
                   reads=[R_bis], writes=[R_pos])
            op("vector", lambda e: e.tensor_tensor(out=pos_f[:], in0=pos_f[:], in1=cum_f[:], op=ALU.add), writes=[R_pos])
            op("vector", lambda e: e.tensor_tensor(out=pos_f[:], in0=pos_f[:], in1=tot_f[:], op=ALU.subtract), writes=[R_pos])
            op("vector", lambda e: e.tensor_scalar(out=tot_f[:], in0=cmp_bf[:], scalar1=-1048576.0, scalar2=1048576.0,
                                                   op0=ALU.mult, op1=ALU.add), reads=[R_cmp], writes=[R_pos])
            op("vector", lambda e: e.tensor_tensor(out=pos_f[:], in0=pos_f[:], in1=tot_f[:], op=ALU.add), writes=[R_pos])
            op("vector", lambda e: e.tensor_copy(out=off_i[:], in_=pos_f[:]), reads=[R_pos], writes=[R_off])
            dcnt = [0]

            def seal_group(gn):
                for ee in range(4 * gn, 4 * gn + 4):
                    for h in range(3):
                        k = ("dma", f"sc{gn}_{h}")
                        if k in P.dma_vals:
                            R_XG[ee].w[k] = P.dma_vals[k]

            def dispatch_block(b, experts):
                gn_ = experts[0] // 4
                h3 = dcnt[0] % 3
                dcnt[0] += 1
                op("sync", lambda e, h3=h3, b=b: e.dma_start(out=hrow[h3][:], in_=H2X[b * 128:(b + 1) * 128, :]),
                   writes=[R_hrow[h3]], dma=f"hrow{h3}")
                for ee in experts:
                    col = b * 16 + ee
                    op("gpsimd", lambda e, h3=h3, ee=ee, col=col: e.indirect_dma_start(
                        out=XG[ee], out_offset=bass.IndirectOffsetOnAxis(ap=off_i[:, col:col + 1], axis=0),
                        in_=hrow[h3][:], in_offset=None, bounds_check=bc_reg(e, 1023), oob_is_err=False),
                       reads=[R_hrow[h3], R_off], writes=[R_XG[ee]], dma=f"sc{gn_}_{h3}")

            for b in range(64):
                dispatch_block(b, range(0, 4))
            seal_group(0)
            pbi = 0
            wci = 0
            sgi = 0

            def gather_expert(ee):
                x2 = ee % 2
                for sbk in range(8):
                    g2 = (ee * 8 + sbk) % 2
                    op("sync", lambda e, g2=g2, ee=ee, sbk=sbk: e.dma_start(out=xgr[g2][:], in_=XG[ee][sbk * 128:(sbk + 1) * 128, :]),
                       reads=[R_XG[ee]], writes=[R_xgr[g2]], dma=f"xgr{g2}")
                    nonlocal_pb = []
                    for half in range(2):
                        pa = gather_expert.pbi % 8
                        gather_expert.pbi += 1
                        for q in range(4):
                            k = half * 4 + q
                            op("tensor", lambda e, pa=pa, q=q, k=k, g2=g2: e.transpose(
                                out=pb[pa][:, q * 128:(q + 1) * 128], in_=xgr[g2][:, k * 128:(k + 1) * 128], identity=ident[:]),
                               reads=[R_xgr[g2]] + RC, writes=[R_pb[pa]])
                        dst = xgT[x2][:, half * 4:(half + 1) * 4, sbk * 128:(sbk + 1) * 128]
                        src = pb[pa][:].rearrange("p (a b) -> p a b", a=4)
                        if half == 0:
                            op("vector", lambda e, dst=dst, src=src: e.tensor_copy(out=dst, in_=src),
                               reads=[R_pb[pa]], writes=[R_xgT[x2]])
                        else:
                            op("scalar", lambda e, dst=dst, src=src: e.activation(out=dst, in_=src, func=AF.Copy),
                               reads=[R_pb[pa]], writes=[R_xgT[x2]])
                    op("gpsimd", lambda e, g2=g2, x2=x2, sbk=sbk, ee=ee: e.tensor_copy(
                        out=gate_t[x2][:, sbk:sbk + 1], in_=xgr[g2][:, D + ee:D + ee + 1]),
                       reads=[R_xgr[g2]], writes=[R_gate[x2]])
                    op("gpsimd", lambda e, g2=g2, x2=x2, sbk=sbk: e.tensor_copy(
                        out=tok_i[x2][:, sbk:sbk + 1], in_=xgr[g2][:, D + 16:D + 17]),
                       reads=[R_xgr[g2]], writes=[R_gate[x2]])
            gather_expert.pbi = 0

            R_accbar = [Res(f"accbar{e}", acc=True) for e in range(16)]

            def acc_scatter(ex, cb):
                x2_ = ex % 2
                rd = [R_eo, R_gate[x2_]] + ([R_accbar[ex - 1]] if ex > 0 else [])
                op("gpsimd", lambda e, cb=cb, x2_=x2_: e.indirect_dma_start(
                    out=X2, out_offset=bass.IndirectOffsetOnAxis(ap=tok_i[x2_][:, cb:cb + 1], axis=0),
                    in_=eo_t[:, cb, :], in_offset=None, bounds_check=bc_reg(e, S - 1), oob_is_err=True, compute_op=ALU.add),
                   reads=rd, writes=[R_accbar[ex]], dma="acc")

            def load_wd(ex, dh):
                op("gpsimd", lambda e, dh=dh, ex=ex: e.dma_start(out=wd_t[dh][:], in_=wd[ex, dh], max_dma_last_dim=4096),
                   writes=[R_wd[dh]], dma=f"wd{dh}")

            gather_expert(0)
            for ee in range(16):
                x2 = ee % 2
                pbi = gather_expert.pbi
                for f in range(16):
                    w3 = wci % 3
                    wci += 1
                    op("gpsimd", lambda e, w3=w3, ee=ee, f=f: e.dma_start(out=wg_t[w3][:], in_=wg[ee, f]),
                       writes=[R_wg[w3]], dma=f"wg{w3}")
                    op("gpsimd", lambda e, w3=w3, ee=ee, f=f: e.dma_start(out=wu_t[w3][:], in_=wu[ee, f]),
                       writes=[R_wu[w3]], dma=f"wu{w3}")
                    if ee > 0 and 4 <= f < 12:
                        acc_scatter(ee - 1, f - 4)
                    if f == 2:
                        load_wd(ee, 1)
                        if ee == 0:
                            load_wd(0, 0)
                    if ee // 4 + 1 < 4:
                        gn = ee // 4 + 1
                        dispatch_block((ee % 4) * 16 + f, range(4 * gn, 4 * gn + 4))
                        if ee % 4 == 3 and f == 15:
                            seal_group(gn)
                    for half in range(2):
                        hs = slice(half * 512, (half + 1) * 512)
                        pg = pbi % 8
                        pbi += 1
                        pu = pbi % 8
                        pbi += 1
                        for k in range(8):
                            op("tensor", lambda e, pg=pg, k=k, w3=w3, x2=x2, hs=hs: e.matmul(
                                pb[pg][:], lhsT=wg_t[w3][:, k, :], rhs=xgT[x2][:, k, hs], start=(k == 0), stop=(k == 7)),
                               reads=[R_wg[w3], R_xgT[x2]], writes=[R_pb[pg]])
                        for k in range(8):
                            op("tensor", lambda e, pu=pu, k=k, w3=w3, x2=x2, hs=hs: e.matmul(
                                pb[pu][:], lhsT=wu_t[w3][:, k, :], rhs=xgT[x2][:, k, hs], start=(k == 0), stop=(k == 7)),
                               reads=[R_wu[w3], R_xgT[x2]], writes=[R_pb[pu]])
                        s2 = sgi % 2
                        sgi += 1
                        op("scalar", lambda e, pg=pg, s2=s2: e.activation(out=sg_t[s2][:], in_=pb[pg][:], func=AF.Silu),
                           reads=[R_pb[pg]], writes=[R_sg[s2]])
                        op("vector", lambda e, pu=pu, s2=s2, f=f, hs=hs: e.tensor_tensor(
                            out=hidT[:, f, hs], in0=pb[pu][:], in1=sg_t[s2][:], op=ALU.mult),
                           reads=[R_pb[pu], R_sg[s2]], writes=[R_hid])
                gather_expert.pbi = pbi
                if ee + 1 < 16:
                    gather_expert(ee + 1)
                pbi = gather_expert.pbi
                for dh in range(2):
                    if dh == 1 and ee + 1 < 16:
                        load_wd(ee + 1, 0)
                    for cb in range(8):
                        pa = pbi % 8
                        pbi += 1
                        for f in range(16):
                            op("tensor", lambda e, pa=pa, f=f, cb=cb, dh=dh: e.matmul(
                                pb[pa][:], lhsT=hidT[:, f, cb * 128:(cb + 1) * 128], rhs=wd_t[dh][:, f, :],
                                start=(f == 0), stop=(f == 15)), reads=[R_hid, R_wd[dh]], writes=[R_pb[pa]])
                        if cb % 2 == 0:
                            op("vector", lambda e, pa=pa, cb=cb, dh=dh, x2=x2: e.tensor_scalar(
                                out=eo_t[:, cb, dh * 512:(dh + 1) * 512], in0=pb[pa][:], scalar1=gate_t[x2][:, cb:cb + 1],
                                scalar2=None, op0=ALU.mult), reads=[R_pb[pa], R_gate[x2]], writes=[R_eo])
                        else:
                            op("scalar", lambda e, pa=pa, cb=cb, dh=dh, x2=x2: e.activation(
                                out=eo_t[:, cb, dh * 512:(dh + 1) * 512], in_=pb[pa][:], func=AF.Copy,
                                scale=gate_t[x2][:, cb:cb + 1]), reads=[R_pb[pa], R_gate[x2]], writes=[R_eo])
                gather_expert.pbi = pbi
                if ee == 15:
                    for cb in range(8):
                        acc_scatter(15, cb)
            P.barrier()
            P.emit(glob)
    _phase5()
    if phases <= 5:
        return finish(nc, P, glob, out)

    def _phase6():
        with ExitStack() as ph:
            gfin_t = sb(ph, "gfin_t", [128, D])
            xa = [sb(ph, f"xa{i}", [128, D]) for i in range(3)]
            ya = [sb(ph, f"ya{i}", [128, D]) for i in range(3)]
            junk = sb(ph, "junkF", [128, D])
            sm = sb(ph, "smF", [128, 4])
            R_xa = [Res(f"xa{i}") for i in range(3)]
            R_ya = [Res(f"ya{i}") for i in range(3)]
            R_junk, R_sm, R_g = Res("junkF"), Res("smF"), Res("gfin")
            R_out = Res("out", acc=True)
            op("sync", lambda e: e.dma_start(out=gfin_t[:], in_=gfin), writes=[R_g], dma="gfin")
            def loadF(b):
                op("sync", lambda e, b=b: e.dma_start(out=xa[b % 3][:], in_=X2[b * 128:(b + 1) * 128, :]),
                   writes=[R_xa[b % 3]], dma=f"xa{b % 3}")

            loadF(0)
            loadF(1)
            for b in range(64):
                b3 = b % 3
                if b + 2 < 64:
                    loadF(b + 2)
                op("scalar", lambda e, b3=b3: e.activation(out=junk[:], in_=xa[b3][:], func=AF.Square, accum_out=sm[:, 0:1]),
                   reads=[R_xa[b3]], writes=[R_junk, R_sm])
                op("scalar", lambda e: e.activation(out=sm[:, 1:2], in_=sm[:, 0:1], func=AF.Sqrt, bias=eps_c[:, 0:1], scale=1.0 / D),
                   reads=RC, writes=[R_sm])
                op("vector", lambda e: e.reciprocal(out=sm[:, 2:3], in_=sm[:, 1:2]), writes=[R_sm])
                op("vector", lambda e, b3=b3: e.scalar_tensor_tensor(
                    out=ya[b3][:], in0=xa[b3][:], scalar=sm[:, 2:3], in1=gfin_t[:], op0=ALU.mult, op1=ALU.mult),
                   reads=[R_xa[b3], R_sm, R_g], writes=[R_ya[b3]])
                op("sync", lambda e, b3=b3, b=b: e.dma_start(out=out[b * 128:(b + 1) * 128, :], in_=ya[b3][:]),
                   reads=[R_ya[b3]], writes=[R_out], dma=f"ya{b3}")
            P.barrier()
            P.emit(glob)
    _phase6()
    return finish(nc, P, glob, out)


def finish(nc, P, glob, out):
    P.barrier()
    P.emit(glob)
    glob.close()
    return nc


def _const_tables():
    ident = np.eye(128, dtype=np.float32)
    ut = np.triu(np.ones((128, 128), np.float32), 1)
    i = np.arange(128)[:, None]
    c = np.arange(256)[None, :]
    c = np.arange(384)[None, :]
    mask = ((c >= i + 64) & (c <= i + 192)).astype(np.float32)
    tokb = (np.arange(64)[None, :] * 128 + np.arange(128)[:, None]).astype(np.float32)
    cst = np.concatenate([ident, ut, mask, tokb], axis=1)
    pos = np.arange(S, dtype=np.float32)
    inv = (np.float32(500000.0) ** (-np.arange(0, 32, 2, dtype=np.float32) / np.float32(32))).astype(np.float32)
    ang = (pos[None, :] * inv[:, None]).astype(np.float32)
    cos, sin = np.cos(ang).astype(np.float32), np.sin(ang).astype(np.float32)
    one = np.ones((16, S), np.float32)
    zero = np.zeros((16, S), np.float32)
    cosT = np.concatenate([cos, one, cos, one], axis=0)
    sinT = np.concatenate([sin, zero, -sin, zero], axis=0)
    return cst, np.ascontiguousarray(cosT), np.ascontiguousarray(sinT)


HEAD_PERM = np.array(list(range(16)) + list(range(32, 48)) + list(range(16, 32)) + list(range(48, 128)))


def prep_shared(inp):
    f = lambda a: np.ascontiguousarray(np.asarray(a, dtype=np.float32))
    w_in = f(inp["w_in"])[0]
    b_in = f(inp["b_in"])[0]
    colperm = np.arange(8704)
    for c in range(16, 40):
        colperm[c * 128:(c + 1) * 128] = c * 128 + HEAD_PERM
    w_in = w_in[:, colperm]
    b_in = b_in[colperm]
    cst, cosT, sinT = _const_tables()
    sh = {
        "w_in": f(w_in.reshape(8, 128, NCH, 128).transpose(2, 1, 0, 3)),
        "b_in": f(b_in.reshape(NCH, 128).T),
        "gmix": f(inp["norm_mix"][0].reshape(8, 128).T),
        "convw": f(np.asarray(inp["conv_w"])[0].reshape(4, 8, 128).transpose(2, 0, 1)),
        "convb": f(np.asarray(inp["conv_b"])[0].reshape(8, 128).T),
        "rgw": f(np.asarray(inp["rg_w"])[0].reshape(4, 8, 128, 128).transpose(1, 2, 0, 3)),
        "rgb": f(np.asarray(inp["rg_b"])[0].reshape(4, 8, 128).transpose(2, 0, 1)),
        "lam": f(np.asarray(inp["rg_lambda"])[0].reshape(2, 8, 128).transpose(2, 0, 1)),
        "prnn": f(np.asarray(inp["p_rnn"])[0].reshape(8, 128, D).transpose(1, 0, 2)),
        "pattn": f(np.asarray(inp["p_attn"])[0].reshape(4, 128, D).transpose(1, 0, 2)),
        "wout": f(np.asarray(inp["w_out"])[0].reshape(8, 128, D).transpose(1, 0, 2)),
        "gffn": f(np.broadcast_to(np.asarray(inp["norm_ffn"])[0][None, :], (128, D))),
        "gfin": f(np.broadcast_to(np.asarray(inp["norm_final"])[None, :], (128, D))),
        "wr": f(np.asarray(inp["w_router"])[0].reshape(8, 128, 16).transpose(1, 0, 2)),
        "br": f(np.broadcast_to(np.asarray(inp["b_router"])[0][None, :], (128, 16))),
        "wg": f(np.asarray(inp["w_gate"])[0].reshape(16, 8, 128, 16, 128).transpose(0, 3, 2, 1, 4)),
        "wu": f(np.asarray(inp["w_up"])[0].reshape(16, 8, 128, 16, 128).transpose(0, 3, 2, 1, 4)),
        "wd": f(np.asarray(inp["w_down"])[0].reshape(16, 16, 128, 2, 512).transpose(0, 3, 2, 1, 4)),
        "cosT": cosT, "sinT": sinT, "cst": cst,
    }
    return sh


_NC_CACHE = {}


def kernel(**inputs):
    x = np.asarray(inputs["x"], dtype=np.float32)
    B = x.shape[0]
    sh = prep_shared(inputs)
    if "nc" not in _NC_CACHE:
        _NC_CACHE["nc"] = build()
    nc = _NC_CACHE["nc"]
    in_maps = []
    for b in range(B):
        m = dict(sh)
        m["xtok"] = np.ascontiguousarray(x[b])
        m["xT"] = np.ascontiguousarray(x[b].T)
        in_maps.append(m)
    res = run_bass_kernel_spmd(nc, in_maps, core_ids=list(range(B)))
    return np.stack([np.asarray(r["out"], dtype=np.float32) for r in res.results], axis=0)
```

```python
import numpy as np
from contextlib import ExitStack
import concourse.bass as bass
import concourse.mybir as mybir
from concourse.bass_utils import run_bass_kernel_spmd

F32 = mybir.dt.float32
F32R = mybir.dt.float32r
BF16 = mybir.dt.bfloat16
I32 = mybir.dt.int32
AF = mybir.ActivationFunctionType
ALU = mybir.AluOpType
AX = mybir.AxisListType

ENGS = ("sync", "scalar", "vector", "gpsimd", "tensor")
EPOCH = 30000
S = 8192
D = 1024
NCH = 68
XW = 1048


class Res:
    __slots__ = ("name", "w", "r", "acc")

    def __init__(self, name, acc=False):
        self.name = name
        self.w = {}
        self.r = {}
        self.acc = acc


class Prog:
    def __init__(self, nc, same_engine_sync=("scalar", "vector", "gpsimd")):
        self.nc = nc
        self.ops = {e: [] for e in ENGS}
        self.cnt = {e: 0 for e in ENGS}
        self.seen = {e: {} for e in ENGS}
        self.dma_vals = {}
        self.same = set(same_engine_sync)
        self.sem_keys = []
        self.final = {}

    def _key(self, k):
        if k not in self.final:
            self.sem_keys.append(k)
        return k

    def op(self, eng, fn, reads=(), writes=(), dma=None):
        need = {}
        for r in reads:
            for k, v in r.w.items():
                if need.get(k, 0) < v:
                    need[k] = v
        for w in writes:
            if not w.acc:
                for k, v in w.w.items():
                    if need.get(k, 0) < v:
                        need[k] = v
            for k, v in w.r.items():
                if need.get(k, 0) < v:
                    need[k] = v
        waits = []
        seen = self.seen[eng]
        for k, v in need.items():
            if seen.get(k, 0) >= v:
                continue
            if k[0] == eng and eng not in self.same:
                continue
            waits.append((k, v))
            seen[k] = v
        if dma is None:
            c = self.cnt[eng]
            self.cnt[eng] = c + 1
            key = self._key((eng, c // EPOCH))
            ev = (key, c % EPOCH + 1)
            inc = 1
        else:
            key = self._key(("dma", dma))
            val = self.dma_vals.get(key, 0) + 16
            self.dma_vals[key] = val
            ev = (key, val)
            inc = 16
        self.final[key] = ev[1]
        for r in reads:
            if r.r.get(ev[0], 0) < ev[1]:
                r.r[ev[0]] = ev[1]
        for w in writes:
            if w.acc:
                if w.w.get(ev[0], 0) < ev[1]:
                    w.w[ev[0]] = ev[1]
            else:
                w.w = {ev[0]: ev[1]}
                w.r = {}
        self.ops[eng].append((waits, fn, key, inc))

    def wait_all(self, eng, own=False):
        waits = []
        for k, v in self.final.items():
            if self.seen[eng].get(k, 0) < v and (own or k[0] != eng):
                waits.append((k, v))
                self.seen[eng][k] = v
        if waits:
            self.ops[eng].append((waits, None, None, 0))

    def barrier(self):
        for e in ENGS:
            self.wait_all(e, own=(e in self.same))

    def emit(self, st):
        nc = self.nc
        if not hasattr(self, "sems"):
            self.sems = {}
        sems = self.sems
        for k in self.sem_keys:
            if k not in sems:
                sems[k] = st.enter_context(nc.semaphore("s_" + "_".join(str(x) for x in k)))
        self.nblk = getattr(self, "nblk", 0) + 1
        with nc.named_scope(f"ph{self.nblk}"), nc.Block() as block:
            for e in ENGS:
                ops = self.ops[e]
                if not ops:
                    continue

                def body(eng, ops=ops):
                    for waits, fn, key, inc in ops:
                        for k, v in waits:
                            eng.wait_ge(sems[k], v)
                        if fn is not None:
                            fn(eng).then_inc(sems[key], inc)
                getattr(block, e)(body)
        self.ops = {e: [] for e in ENGS}


def chunk_kind(c):
    if c < 8:
        return "xr"
    if c < 16:
        return "gr"
    if c < 28:
        return "q"
    if c < 40:
        return "k"
    if c < 52:
        return "v"
    return "gate"


DIL = (1, 4, 16)


def build(phases=6, debug=False):
    nc = bass.Bass("TRN2", target_bir_lowering=False)

    def din(name, shape, dt=F32):
        return nc.dram_tensor(name, list(shape), dt, kind="ExternalInput").ap()

    def dscr(name, shape, dt=F32):
        kind = "ExternalOutput" if debug else "Internal"
        return nc.dram_tensor(name, list(shape), dt, kind=kind).ap()

    xT = din("xT", [D, S])
    xtok = din("xtok", [S, D])
    w_in = din("w_in", [NCH, 128, 8, 128])
    b_in = din("b_in", [128, NCH])
    gmix = din("gmix", [128, 8])
    convw = din("convw", [128, 4, 8])
    convb = din("convb", [128, 8])
    rgw = din("rgw", [8, 128, 4, 128])
    rgb = din("rgb", [128, 4, 8])
    lam = din("lam", [128, 2, 8])
    prnn = din("prnn", [128, 8, D])
    pattn = din("pattn", [128, 4, D])
    wout = din("wout", [128, 8, D])
    gffn = din("gffn", [128, D])
    gfin = din("gfin", [128, D])
    wr = din("wr", [128, 8, 16])
    br = din("br", [128, 16])
    wg = din("wg", [16, 16, 128, 8, 128])
    wu = din("wu", [16, 16, 128, 8, 128])
    wd = din("wd", [16, 2, 128, 16, 512])
    cosT = din("cosT", [64, S])
    sinT = din("sinT", [64, S])
    cst = din("cst", [128, 128 + 128 + 384 + 64])

    out = nc.dram_tensor("out", [S, D], F32, kind="ExternalOutput").ap()

    XR = dscr("XR", [8, 128, S])
    GG = dscr("GG", [8, 128, S])
    QKV = dscr("QKV", [36, 128, S], BF16)
    SG = dscr("SG", [16, 128, S])
    YT = dscr("YT", [8, 128, S])
    OT = dscr("OT", [4, 128, S])
    X2 = dscr("X2", [S, D])
    H2X = dscr("H2X", [S, XW])
    XG = [dscr(f"XG{e}", [1024, XW]) for e in range(16)]

    P = Prog(nc)
    op = P.op
    R2 = lambda n: [Res(n + "0"), Res(n + "1")]
    bregs = {}

    def bc_reg(e, val):
        if val not in bregs:
            r = e.alloc_register(f"bc{val}")
            e.reg_mov(r, val)
            bregs[val] = r
        return bregs[val]
    glob = ExitStack()

    def sb(st, name, shape, dt=F32):
        return st.enter_context(nc.sbuf_tensor(name, list(shape), dt))

    ident = sb(glob, "ident", [128, 128])
    ident_bf = sb(glob, "ident_bf", [128, 128], BF16)
    ut_bf = sb(glob, "ut_bf", [128, 128], BF16)
    ones_bf = sb(glob, "ones_bf", [128, 128], BF16)
    ones_r = sb(glob, "ones_r", [128, 128], F32R)
    pmask = [sb(glob, f"pmask{i}", [128, 512], BF16) for i in range(3)]
    cst_t = sb(glob, "cst_t", [128, 704])
    one_c = sb(glob, "one_c", [128, 1])
    eps_c = sb(glob, "eps_c", [128, 1])
    AFFt = sb(glob, "AFFt", [128, 64, 16])
    R_const = Res("const")
    R_aff = Res("aff")
    op("sync", lambda e: e.dma_start(out=cst_t[:], in_=cst), writes=[R_const], dma="cst")
    op("vector", lambda e: e.tensor_copy(out=ident[:], in_=cst_t[:, 0:128]), reads=[R_const], writes=[R_const])
    op("vector", lambda e: e.tensor_copy(out=ident_bf[:], in_=cst_t[:, 0:128]), reads=[R_const], writes=[R_const])
    op("vector", lambda e: e.tensor_copy(out=ut_bf[:], in_=cst_t[:, 128:256]), reads=[R_const], writes=[R_const])
    for i_, (o0_, o1_) in enumerate(((64, 64), (128, 64), (64, 0))):
        op("vector", lambda e, i_=i_, o0_=o0_: e.tensor_copy(out=pmask[i_][:, 0:256], in_=cst_t[:, 256 + o0_:512 + o0_]),
           reads=[R_const], writes=[R_const])
        op("vector", lambda e, i_=i_, o1_=o1_: e.tensor_copy(out=pmask[i_][:, 256:512], in_=cst_t[:, 256 + o1_:512 + o1_]),
           reads=[R_const], writes=[R_const])
    op("vector", lambda e: e.memset(ones_bf[:], 1.0), writes=[R_const])
    op("vector", lambda e: e.memset(ones_r[:].bitcast(F32), 1.0), writes=[R_const])
    op("vector", lambda e: e.memset(one_c[:], 1.0), writes=[R_const])
    op("vector", lambda e: e.memset(eps_c[:], 1e-6), writes=[R_const])
    RC = [R_const]

    def _phase1():
        with ExitStack() as ph:
            hT = sb(ph, "hT", [128, 8, 2048], F32R)
            xs = [sb(ph, f"xs{i}", [128, 8, 512]) for i in range(2)]
            rstd2 = [sb(ph, f"rstd{i}", [128, 512]) for i in range(2)]
            R_rstd2 = [Res("rstd0"), Res("rstd1")]
            wch = [sb(ph, f"wch{i}", [128, 8, 128], F32R) for i in range(3)]
            stg = [sb(ph, f"stg{i}", [128, 2048]) for i in range(4)]
            stgb = [sb(ph, f"stgb{i}", [128, 2048], BF16) for i in range(4)]
            rtmp = [sb(ph, f"rtmp{i}", [64, 2048]) for i in range(2)]
            cos_t = sb(ph, "cos_t", [64, 2048])
            sin_t = sb(ph, "sin_t", [64, 2048])
            bin_t = sb(ph, "bin_t", [128, NCH])
            binq_t = sb(ph, "binq_t", [128, NCH])
            gmix_t = sb(ph, "gmix_t", [128, 8])
            pb = [ph.enter_context(nc.psum_tensor(f"pbA{i}", [128, 512], F32)) for i in range(8)]
            R_pb = [Res(f"pbA{i}") for i in range(8)]
            R_hTt = [Res(f"hT{j}") for j in range(4)]
            R_xs = [Res("xs0"), Res("xs1")]
            R_wch = [Res(f"wch{i}") for i in range(3)]
            R_stg = [Res(f"stg{i}") for i in range(4)]
            R_stgb = [Res(f"stgb{i}") for i in range(4)]
            R_rtmp = [Res("rtmp0"), Res("rtmp1")]
            R_cs = Res("cossin")
            R_scrA = Res("scrA", acc=True)
            QSC = 128.0 ** -0.5

            op("sync", lambda e: e.dma_start(out=bin_t[:], in_=b_in), writes=RC, dma="cst")
            op("sync", lambda e: e.dma_start(out=gmix_t[:], in_=gmix), writes=RC, dma="cst")
            op("vector", lambda e: e.tensor_scalar(out=binq_t[:], in0=bin_t[:], scalar1=QSC, scalar2=None, op0=ALU.mult),
               reads=RC, writes=RC)
            xT_v = xT.rearrange("(k p) t -> p k t", p=128)

            def load_x(st_, j_):
                tt_ = st_ * 2048 + j_ * 512
                op("sync", lambda e, j_=j_, tt_=tt_: e.dma_start(out=xs[j_ % 2][:], in_=xT_v[:, :, tt_:tt_ + 512]),
                   writes=[R_xs[j_ % 2]], dma=f"xs{j_ % 2}")

            def issue_w(idx):
                cc = idx % NCH
                op("gpsimd", lambda e, idx=idx, cc=cc: e.dma_start(out=wch[idx % 3][:], in_=w_in[cc]),
                   writes=[R_wch[idx % 3]], dma=f"wch{idx % 3}")
            pbi = 0
            wi = 0
            si = 0
            pending = []
            for st_i in range(4):
                t0 = st_i * 2048
                op("sync", lambda e, t0=t0: e.dma_start(out=cos_t[:], in_=cosT[:, t0:t0 + 2048]), writes=[R_cs], dma="cs")
                op("sync", lambda e, t0=t0: e.dma_start(out=sin_t[:], in_=sinT[:, t0:t0 + 2048]), writes=[R_cs], dma="cs")
                if st_i == 0:
                    load_x(0, 0)
                    load_x(0, 1)
                for j in range(4):
                    xb = xs[j % 2]
                    Rx = R_xs[j % 2]
                    rs = rstd2[j % 2]
                    Rrs = R_rstd2[j % 2]
                    js_ = slice(j * 512, (j + 1) * 512)
                    op("scalar", lambda e, xb=xb, js_=js_: e.activation(out=hT[:, :, js_], in_=xb[:], func=AF.Square),
                       reads=[Rx], writes=[R_hTt[j]])
                    pbk = pbi % 8
                    pbi += 1
                    for k in range(8):
                        op("tensor", lambda e, k=k, pbk=pbk, js_=js_: e.matmul(pb[pbk][:], lhsT=ones_r[:], rhs=hT[:, k, js_],
                                                                              start=(k == 0), stop=(k == 7)),
                           reads=[R_hTt[j]] + RC, writes=[R_pb[pbk]])
                    op("scalar", lambda e, pbk=pbk, rs=rs: e.activation(out=rs[:], in_=pb[pbk][:], func=AF.Sqrt,
                                                                        bias=eps_c[:, 0:1], scale=1.0 / D),
                       reads=[R_pb[pbk]] + RC, writes=[Rrs])
                    op("vector", lambda e, rs=rs: e.reciprocal(out=rs[:], in_=rs[:]), reads=[Rrs], writes=[Rrs])
                    for k in range(8):
                        op("vector", lambda e, k=k, xb=xb, js_=js_, rs=rs: e.scalar_tensor_tensor(
                            out=hT[:, k, js_], in0=xb[:, k, :], scalar=gmix_t[:, k:k + 1],
                            in1=rs[:], op0=ALU.mult, op1=ALU.mult),
                           reads=[Rx, Rrs] + RC, writes=[R_hTt[j]])
                    if j + 2 < 4:
                        load_x(st_i, j + 2)
                for c in range(NCH):
                    kind = chunk_kind(c)
                    wb = wch[wi % 3]
                    Rw = R_wch[wi % 3]
                    if wi == 0:
                        issue_w(0)
                        issue_w(1)
                    if wi + 2 < 4 * NCH:
                        issue_w(wi + 2)
                    wi += 1
                    if c == 56 and st_i + 1 < 4:
                        load_x(st_i + 1, 0)
                        load_x(st_i + 1, 1)
                    sgi = si % 4
                    si += 1
                    use_b = kind in ("q", "k", "v")
                    for j in range(4):
                        pbk = pbi % 8
                        pbi += 1
                        for k in range(8):
                            op("tensor", lambda e, k=k, pbk=pbk, wb=wb, j=j: e.matmul(
                                pb[pbk][:], lhsT=wb[:, k, :], rhs=hT[:, k, j * 512:(j + 1) * 512],
                                start=(k == 0), stop=(k == 7)),
                               reads=[Rw, R_hTt[j]], writes=[R_pb[pbk]])
                        js = slice(j * 512, (j + 1) * 512)
                        if kind == "xr":
                            fn = lambda e, pbk=pbk, js=js, c=c, sgi=sgi: e.activation(
                                out=stg[sgi][:, js], in_=pb[pbk][:], func=AF.Identity, bias=bin_t[:, c:c + 1], scale=1.0)
                            wr_ = [R_stg[sgi]]
                        elif kind == "gr":
                            fn = lambda e, pbk=pbk, js=js, c=c, sgi=sgi: e.activation(
                                out=stg[sgi][:, js], in_=pb[pbk][:], func=AF.Gelu, bias=bin_t[:, c:c + 1], scale=1.0)
                            wr_ = [R_stg[sgi]]
                        elif kind == "gate":
                            fn = lambda e, pbk=pbk, js=js, c=c, sgi=sgi: e.activation(
                                out=stg[sgi][:, js], in_=pb[pbk][:], func=AF.Sigmoid, bias=bin_t[:, c:c + 1], scale=1.0)
                            wr_ = [R_stg[sgi]]
                        elif kind == "q":
                            fn = lambda e, pbk=pbk, js=js, c=c, sgi=sgi: e.activation(
                                out=stg[sgi][:, js], in_=pb[pbk][:], func=AF.Identity, bias=binq_t[:, c:c + 1], scale=QSC)
                            wr_ = [R_stg[sgi]]
                        elif kind == "k":
                            fn = lambda e, pbk=pbk, js=js, c=c, sgi=sgi: e.activation(
                                out=stg[sgi][:, js], in_=pb[pbk][:], func=AF.Identity, bias=bin_t[:, c:c + 1], scale=1.0)
                            wr_ = [R_stg[sgi]]
                        else:
                            dv = DIL[((c - 16) % 12) // 4]
                            nj = 512 // dv
                            fn = lambda e, pbk=pbk, j=j, c=c, sgi=sgi, dv=dv, nj=nj: e.activation(
                                out=stgb[sgi][:].rearrange("p (r l) -> p r l", r=dv)[:, :, j * nj:(j + 1) * nj],
                                in_=pb[pbk][:].rearrange("p (l r) -> p r l", r=dv),
                                func=AF.Identity, bias=bin_t[:, c:c + 1], scale=1.0)
                            wr_ = [R_stgb[sgi]]
                        op("scalar", fn, reads=[R_pb[pbk]] + RC, writes=wr_)
                    tail_ops = []
                    if use_b:
                        g = ((c - 16) % 12) // 4
                        d = DIL[g]
                        sgt = stg[sgi]
                        sbt = stgb[sgi]
                        if kind in ("q", "k"):
                            op("vector", lambda e, sgt=sgt: e.tensor_tensor(
                                out=rtmp[0][0:32, :], in0=sgt[32:64, :], in1=sin_t[32:64, :], op=ALU.mult),
                               reads=[R_stg[sgi], R_cs], writes=[R_rtmp[0]])
                            op("vector", lambda e, sgt=sgt: e.tensor_tensor(
                                out=rtmp[0][32:64, :], in0=sgt[0:32, :], in1=sin_t[0:32, :], op=ALU.mult),
                               reads=[R_stg[sgi], R_cs], writes=[R_rtmp[0]])
                            op("vector", lambda e, sgt=sgt: e.tensor_tensor(
                                out=rtmp[1][:, :], in0=sgt[0:64, :], in1=cos_t[:, :], op=ALU.mult),
                               reads=[R_cs, R_stg[sgi]], writes=[R_rtmp[1]])
                            op("vector", lambda e, sgt=sgt: e.tensor_tensor(
                                out=sgt[0:64, :], in0=rtmp[0][:, :], in1=rtmp[1][:, :], op=ALU.add),
                               reads=[R_rtmp[0], R_rtmp[1]], writes=[R_stg[sgi]])
                            tail_ops.append(("scalar", lambda e, sgt=sgt, sbt=sbt, d=d: e.activation(
                                out=sbt[:].rearrange("p (r j) -> p r j", r=d),
                                in_=sgt[:].rearrange("p (j r) -> p r j", r=d), func=AF.Copy),
                                [R_stg[sgi]], [R_stgb[sgi]], None))
                        qi = c - 16
                        j0 = t0 // d
                        nj = 2048 // d
                        dst = QKV[qi].rearrange("p (r l) -> p r l", r=d)[:, :, j0:j0 + nj]
                        src = sbt[:].rearrange("p (r j) -> p r j", r=d)
                        tail_ops.append(("sync", lambda e, dst=dst, src=src: e.dma_start(out=dst, in_=src),
                                         [R_stgb[sgi]], [R_scrA], f"stgb{sgi}"))
                    else:
                        if kind == "xr":
                            dst = XR[c][:, t0:t0 + 2048]
                        elif kind == "gr":
                            dst = GG[c - 8][:, t0:t0 + 2048]
                        else:
                            dst = SG[c - 52][:, t0:t0 + 2048]
                        tail_ops.append(("sync", lambda e, dst=dst, sgi=sgi: e.dma_start(out=dst, in_=stg[sgi][:]),
                                         [R_stg[sgi]], [R_scrA], f"stg{sgi}"))
                    for (en_, fn_, rd_, wr2_, dm_) in pending:
                        op(en_, fn_, reads=rd_, writes=wr2_, dma=dm_)
                    pending[:] = tail_ops
            for (en_, fn_, rd_, wr2_, dm_) in pending:
                op(en_, fn_, reads=rd_, writes=wr2_, dma=dm_)
            P.barrier()
            P.emit(glob)
    _phase1()
    if phases <= 1:
        return finish(nc, P, glob, out)

    SEG = 1024
    NSEG = S // SEG
    def _phase2():
        with ExitStack() as ph:
            xc2 = [sb(ph, f"xc{i}", [128, S], F32R) for i in range(2)]
            hf = sb(ph, "hf", [128, S])
            raw = [sb(ph, f"raw{i}", [128, SEG + 3]) for i in range(2)]
            xcr = [sb(ph, f"xcr{i}", [128, SEG]) for i in range(2)]
            r_ = [sb(ph, f"r_{i}", [128, SEG]) for i in range(2)]
            i_ = [sb(ph, f"i_{i}", [128, SEG]) for i in range(2)]
            a2_ = [sb(ph, f"a2_{i}", [128, SEG]) for i in range(2)]
            hb_ = [sb(ph, f"hb_{i}", [128, SEG]) for i in range(2)]
            gg_ = [sb(ph, f"gg_{i}", [128, SEG]) for i in range(2)]
            ys_ = [sb(ph, f"ys_{i}", [128, SEG]) for i in range(2)]
            rgw_t = [sb(ph, f"rgw_t{i}", [128, 4, 128], F32R) for i in range(2)]
            rgb_t = sb(ph, "rgb_t", [128, 4, 8])
            kap = sb(ph, "kap", [128, 2, 8])
            kap2 = sb(ph, "kap2", [128, 2, 8])
            hrgb_t = sb(ph, "hrgb_t", [128, 4, 8])
            quarter_c = sb(ph, "quarter_c", [128, 1])
            cw_t = sb(ph, "cw_t", [128, 4, 8])
            cb_t = sb(ph, "cb_t", [128, 8])
            pb = [ph.enter_context(nc.psum_tensor(f"pbB{i}", [128, 512], F32)) for i in range(8)]
            R_pb = [Res(f"pbB{i}") for i in range(8)]
            R_xc2, R_hf = R2("xc"), Res("hf")
            R_raw, R_xcr, R_r, R_i, R_a2, R_hb, R_gg, R_ys, R_rgw = (R2("raw"), R2("xcr"), R2("r"), R2("i"), R2("a2"),
                                                                     R2("hb"), R2("gg"), R2("ys"), R2("rgw"))
            R_scrB = Res("scrB", acc=True)
            op("sync", lambda e: e.dma_start(out=rgb_t[:], in_=rgb), writes=RC, dma="cst")
            op("sync", lambda e: e.dma_start(out=kap[:], in_=lam), writes=RC, dma="cst")
            op("sync", lambda e: e.dma_start(out=cw_t[:], in_=convw), writes=RC, dma="cst")
            op("sync", lambda e: e.dma_start(out=cb_t[:], in_=convb), writes=RC, dma="cst")
            op("vector", lambda e: e.tensor_scalar(out=hrgb_t[:], in0=rgb_t[:], scalar1=0.5, scalar2=None, op0=ALU.mult),
               reads=RC, writes=RC)
            op("vector", lambda e: e.memset(quarter_c[:], 0.25), writes=RC)
            op("scalar", lambda e: e.activation(out=kap[:], in_=kap[:], func=AF.Exp, scale=-1.0), reads=RC, writes=RC)
            op("scalar", lambda e: e.activation(out=kap[:], in_=kap[:], func=AF.Ln, bias=one_c[:, 0:1], scale=1.0),
               reads=RC, writes=RC)
            op("vector", lambda e: e.tensor_scalar(out=kap2[:], in0=kap[:], scalar1=-4.0, scalar2=None, op0=ALU.mult),
               reads=RC, writes=RC)
            op("vector", lambda e: e.tensor_scalar(out=kap[:], in0=kap[:], scalar1=-8.0, scalar2=None, op0=ALU.mult),
               reads=RC, writes=RC)
            steps = []
            for c in range(8):
                for dirn in range(2):
                    for sgm in (range(NSEG) if dirn == 0 else range(NSEG - 1, -1, -1)):
                        steps.append((c, dirn, sgm))
            pbs = [0]

            def load_w(c):
                op("gpsimd", lambda e, c=c: e.dma_start(out=rgw_t[c % 2][:], in_=rgw[c]), writes=[R_rgw[c % 2]], dma=f"rgw{c % 2}")

            def stageX(n):
                c, dirn, sgm = steps[n]
                b2 = n % 2
                xc, R_xc = xc2[c % 2], R_xc2[c % 2]
                wt, Rwt = rgw_t[c % 2], R_rgw[c % 2]
                t0 = sgm * SEG
                ts_ = slice(t0, t0 + SEG)
                if dirn == 0 and sgm == 0 and c + 1 < 8:
                    load_w(c + 1)
                if dirn == 0:
                    rw = raw[b2]
                    lo = max(t0 - 2, 0)
                    hi = min(t0 + SEG + 1, S)
                    if sgm == 0:
                        op("vector", lambda e, rw=rw: e.memset(rw[:, 0:2], 0.0), writes=[R_raw[b2]])
                    if sgm == NSEG - 1:
                        op("vector", lambda e, rw=rw: e.memset(rw[:, SEG + 2:SEG + 3], 0.0), writes=[R_raw[b2]])
                    o0 = lo - (t0 - 2)
                    op("sync", lambda e, rw=rw, c=c, lo=lo, hi=hi, o0=o0: e.dma_start(
                        out=rw[:, o0:o0 + hi - lo], in_=XR[c][:, lo:hi]), writes=[R_raw[b2]], dma=f"raw{b2}")
                    ct = xcr[b2]
                    op("vector", lambda e, rw=rw, c=c, ct=ct: e.tensor_scalar(
                        out=ct[:], in0=rw[:, 0:SEG], scalar1=cw_t[:, 0, c:c + 1], scalar2=cb_t[:, c:c + 1],
                        op0=ALU.mult, op1=ALU.add), reads=[R_raw[b2]] + RC, writes=[R_xcr[b2]])
                    for tap in range(1, 4):
                        op("vector", lambda e, rw=rw, c=c, ts_=ts_, tap=tap, xc=xc, ct=ct: e.scalar_tensor_tensor(
                            out=(xc[:, ts_] if tap == 3 else ct[:]), in0=rw[:, tap:tap + SEG],
                            scalar=cw_t[:, tap, c:c + 1], in1=ct[:], op0=ALU.mult, op1=ALU.add),
                           reads=[R_raw[b2], R_xcr[b2]] + RC, writes=([R_xc] if tap == 3 else [R_xcr[b2]]))
                else:
                    op("sync", lambda e, b2=b2, c=c, ts_=ts_: e.dma_start(out=gg_[b2][:], in_=GG[c][:, ts_]),
                       writes=[R_gg[b2]], dma=f"gg{b2}")
                for j in range(SEG // 512):
                    js = slice(j * 512, (j + 1) * 512)
                    for gt, dstt, Rd in ((0, r_[b2], R_r[b2]), (1, i_[b2], R_i[b2])):
                        pbk = pbs[0] % 8
                        pbs[0] += 1
                        gi = dirn * 2 + gt
                        op("tensor", lambda e, pbk=pbk, wt=wt, gi=gi, xc=xc, j=j, t0=t0: e.matmul(
                            pb[pbk][:], lhsT=wt[:, gi, :], rhs=xc[:, t0 + j * 512:t0 + (j + 1) * 512], start=True, stop=True),
                           reads=[Rwt, R_xc], writes=[R_pb[pbk]])
                        op("scalar", lambda e, pbk=pbk, dstt=dstt, js=js, gi=gi, c=c: e.activation(
                            out=dstt[:, js], in_=pb[pbk][:], func=AF.Tanh, bias=hrgb_t[:, gi, c:c + 1], scale=0.5),
                           reads=[R_pb[pbk]] + RC, writes=[Rd])
                rr, aa = r_[b2], a2_[b2]
                op("scalar", lambda e, rr=rr, aa=aa, dirn=dirn, c=c: e.activation(
                    out=aa[:], in_=rr[:], func=AF.Exp, scale=kap[:, dirn, c:c + 1], bias=kap[:, dirn, c:c + 1]),
                   reads=[R_r[b2]] + RC, writes=[R_a2[b2]])
                op("scalar", lambda e, rr=rr, dirn=dirn, c=c: e.activation(
                    out=rr[:], in_=rr[:], func=AF.Exp, scale=kap2[:, dirn, c:c + 1], bias=kap2[:, dirn, c:c + 1]),
                   reads=RC, writes=[R_r[b2]])
                op("scalar", lambda e, aa=aa: e.activation(
                    out=aa[:], in_=aa[:], func=AF.Sqrt, bias=quarter_c[:, 0:1], scale=-0.25),
                   reads=RC, writes=[R_a2[b2]])

            def stageY(n):
                c, dirn, sgm = steps[n]
                b2 = n % 2
                xc, R_xc = xc2[c % 2], R_xc2[c % 2]
                t0 = sgm * SEG
                ts_ = slice(t0, t0 + SEG)
                rr, ii, aa = r_[b2], i_[b2], a2_[b2]
                op("vector", lambda e, aa=aa, ts_=ts_, xc=xc: e.tensor_tensor(
                    out=aa[:], in0=aa[:], in1=xc[:, ts_].bitcast(F32), op=ALU.mult),
                   reads=[R_xc], writes=[R_a2[b2]])
                op("vector", lambda e, ii=ii, aa=aa: e.scalar_tensor_tensor(
                    out=ii[:], in0=ii[:], scalar=1.0, in1=aa[:], op0=ALU.add, op1=ALU.mult),
                   reads=[R_a2[b2]], writes=[R_i[b2]])
                if dirn == 0:
                    init = 0.0 if sgm == 0 else hf[:, t0 - 1:t0]
                    op("vector", lambda e, rr=rr, ii=ii, ts_=ts_, init=init: e.tensor_tensor_scan(
                        out=hf[:, ts_], data0=rr[:], data1=ii[:], initial=init, op0=ALU.mult, op1=ALU.add),
                       reads=[R_r[b2], R_i[b2]], writes=[R_hf])
                else:
                    hb = hb_[b2]
                    if sgm == NSEG - 1:
                        init = 0.0
                        rd_extra = []
                    else:
                        init = hb_[1 - b2][:, 0:1]
                        rd_extra = [R_hb[1 - b2]]
                    op("vector", lambda e, rr=rr, ii=ii, hb=hb, init=init: e.tensor_tensor_scan(
                        out=hb[:, ::-1], data0=rr[:, ::-1], data1=ii[:, ::-1], initial=init,
                        op0=ALU.mult, op1=ALU.add),
                       reads=[R_r[b2], R_i[b2]] + rd_extra, writes=[R_hb[b2]])
                    gg = gg_[b2]
                    ys = ys_[b2]
                    op("vector", lambda e, ys=ys, hb=hb, ts_=ts_: e.tensor_tensor(
                        out=ys[:], in0=hb[:], in1=hf[:, ts_], op=ALU.add),
                       reads=[R_hb[b2], R_hf], writes=[R_ys[b2]])
                    op("vector", lambda e, ys=ys, gg=gg: e.tensor_tensor(out=ys[:], in0=ys[:], in1=gg[:], op=ALU.mult),
                       reads=[R_gg[b2]], writes=[R_ys[b2]])
                    op("sync", lambda e, ys=ys, c=c, ts_=ts_: e.dma_start(out=YT[c][:, ts_], in_=ys[:]),
                       reads=[R_ys[b2]], writes=[R_scrB], dma=f"ys{b2}")

            load_w(0)
            stageX(0)
            for n in range(len(steps)):
                if n + 1 < len(steps):
                    stageX(n + 1)
                stageY(n)
            P.barrier()
            P.emit(glob)
    _phase2()
    if phases <= 2:
        return finish(nc, P, glob, out)

    def _phase3():
        with ExitStack() as ph:
            Qd = [sb(ph, f"Qd{i}", [128, S], BF16) for i in range(2)]
            Kd = [sb(ph, f"Kd{i}", [128, S], BF16) for i in range(2)]
            Vd = sb(ph, "Vd", [128, S], BF16)
            Vt = sb(ph, "Vt", [128, 64, 128], BF16)
            NUM = sb(ph, "NUM", [128, S])
            DEN = sb(ph, "DEN", [128, S])
            pT = [sb(ph, f"pT{i}", [128, 512], BF16) for i in range(4)]
            ostg = [sb(ph, f"ostg{i}", [128, 1024]) for i in range(2)]
            ps_s = [ph.enter_context(nc.psum_tensor(f"ps_s{i}", [128, 512], F32)) for i in range(3)]
            ps_n = [ph.enter_context(nc.psum_tensor(f"ps_n{i}", [128, 512], F32)) for i in range(2)]
            ps_d = [ph.enter_context(nc.psum_tensor(f"ps_d{i}", [128, 512], F32)) for i in range(2)]
            ps_t = [ph.enter_context(nc.psum_tensor(f"ps_t{i}", [128, 512], BF16)) for i in range(1)]
            R_Qd, R_Kd = R2("Qd"), R2("Kd")
            R_Vd, R_Vt, R_NUM, R_DEN = Res("Vd"), Res("Vt"), Res("NUM"), Res("DEN")
            R_pT = [Res(f"pT{i}") for i in range(4)]
            R_ostg = R2("ostg")
            R_pss, R_psn, R_psd, R_pst = R2("pss"), R2("psn"), R2("psd"), R2("pst")
            R_pss4 = [Res(f"pss4{i}") for i in range(4)]
            R_scrC = Res("scrC", acc=True)
            ucnt = 0
            pcnt = 0
            for h in range(4):
                for g in range(3):
                    d = DIL[g]
                    L = S // d
                    u2 = ucnt % 2
                    ucnt += 1
                    qd, kd = Qd[u2], Kd[u2]
                    op("sync", lambda e, qd=qd, g=g, h=h: e.dma_start(out=qd[:], in_=QKV[g * 4 + h]),
                       writes=[R_Qd[u2]], dma=f"Qd{u2}")
                    op("sync", lambda e, kd=kd, g=g, h=h: e.dma_start(out=kd[:], in_=QKV[12 + g * 4 + h]),
                       writes=[R_Kd[u2]], dma=f"Kd{u2}")
                    op("sync", lambda e, g=g, h=h: e.dma_start(out=Vd[:], in_=QKV[24 + g * 4 + h]),
                       writes=[R_Vd], dma="Vd")
                    tviews = [(ps_t[0][:], R_pst[0])] + [(ps_s[i_][:].bitcast(BF16)[:, 0:512], R_pss4[i_]) for i_ in range(3)]
                    for m4 in range(16):
                        tv, Rtv = tviews[m4 % 4]
                        for q in range(4):
                            m = m4 * 4 + q
                            op("tensor", lambda e, tv=tv, q=q, m=m: e.transpose(
                                out=tv[:, q * 128:(q + 1) * 128], in_=Vd[:, m * 128:(m + 1) * 128], identity=ident_bf[:]),
                               reads=[R_Vd] + RC, writes=[Rtv])
                        if m4 % 2 == 0:
                            op("vector", lambda e, tv=tv, m4=m4: e.tensor_copy(
                                out=Vt[:, m4 * 4:(m4 + 1) * 4, :].rearrange("p a b -> p (a b)"), in_=tv),
                               reads=[Rtv], writes=[R_Vt])
                        else:
                            op("scalar", lambda e, tv=tv, m4=m4: e.activation(
                                out=Vt[:, m4 * 4:(m4 + 1) * 4, :].rearrange("p a b -> p (a b)"), in_=tv, func=AF.Copy),
                               reads=[Rtv], writes=[R_Vt])
                    contribs = []
                    for m in range(64):
                        p0 = m * 128
                        r = p0 // L
                        lo, hi = r * L, (r + 1) * L
                        if p0 == lo:
                            qs, var = p0, 1
                        elif p0 + 128 == hi:
                            qs, var = p0 - 128, 2
                        else:
                            qs, var = p0 - 64, 0
                        qe = qs + 256
                        parts = []
                        sb0 = (qs // 512) * 512
                        while sb0 < qe:
                            a, b = max(qs, sb0), min(qe, sb0 + 512)
                            parts.append((sb0 // 512, a, b))
                            sb0 += 512
                        contribs.append((qs, qe, var, parts))
                    last_of = {}
                    for m, (_, _, _, parts) in enumerate(contribs):
                        for (sbi, a, b) in parts:
                            last_of[sbi] = m
                    started = set()
                    LOOK = 2

                    def front(pi):
                        s3 = pi % 3
                        pt = pT[pi % 4]
                        Rp = R_pT[pi % 4]
                        for hf_ in range(2):
                            m = 2 * pi + hf_
                            qs, qe, var, parts = contribs[m]
                            op("tensor", lambda e, s3=s3, hf_=hf_, m=m, qs=qs, qe=qe, kd=kd, qd=qd: e.matmul(
                                ps_s[s3][:, hf_ * 256:(hf_ + 1) * 256], lhsT=kd[:, m * 128:(m + 1) * 128], rhs=qd[:, qs:qe],
                                start=True, stop=True),
                               reads=[R_Qd[u2], R_Kd[u2]], writes=[R_pss4[s3]])
                        v0, v1 = contribs[2 * pi][2], contribs[2 * pi + 1][2]
                        pm = pmask[{(0, 0): 0, (1, 0): 1, (0, 2): 2}[(v0, v1)]]
                        op("scalar", lambda e, s3=s3, pt=pt: e.activation(out=pt[:], in_=ps_s[s3][:], func=AF.Exp),
                           reads=[R_pss4[s3]], writes=[Rp])
                        op("vector", lambda e, pt=pt, pm=pm: e.tensor_tensor(out=pt[:], in0=pt[:], in1=pm[:], op=ALU.mult),
                           reads=RC, writes=[Rp])

                    def back(pi):
                        pt = pT[pi % 4]
                        Rp = R_pT[pi % 4]
                        for hf_ in range(2):
                            m = 2 * pi + hf_
                            qs, qe, var, parts = contribs[m]
                            for (sbi, a, b) in parts:
                                n2 = sbi % 2
                                first = sbi not in started
                                started.add(sbi)
                                c0, c1 = a - sbi * 512, b - sbi * 512
                                r0, r1 = hf_ * 256 + a - qs, hf_ * 256 + b - qs
                                op("tensor", lambda e, n2=n2, m=m, pt=pt, r0=r0, r1=r1, c0=c0, c1=c1, first=first: e.matmul(
                                    ps_n[n2][:, c0:c1], lhsT=Vt[:, m, :], rhs=pt[:, r0:r1], start=first, stop=False,
                                    skip_group_check=True),
                                   reads=[R_Vt, Rp], writes=[R_psn[n2]])
                                op("tensor", lambda e, n2=n2, pt=pt, r0=r0, r1=r1, c0=c0, c1=c1, first=first: e.matmul(
                                    ps_d[n2][:, c0:c1], lhsT=ones_bf[:], rhs=pt[:, r0:r1], start=first, stop=False,
                                    skip_group_check=True),
                                   reads=[Rp] + RC, writes=[R_psd[n2]])
                                if last_of[sbi] == m:
                                    P0 = sbi * 512
                                    r = P0 // L
                                    j0 = P0 - r * L
                                    nat = slice(r + d * j0, r + d * (j0 + 511) + 1, d) if d > 1 else slice(P0, P0 + 512)
                                    if g == 0:
                                        op("vector", lambda e, n2=n2, nat=nat: e.tensor_copy(out=NUM[:, nat], in_=ps_n[n2][:]),
                                           reads=[R_psn[n2]], writes=[R_NUM])
                                        op("scalar", lambda e, n2=n2, nat=nat: e.activation(out=DEN[:, nat], in_=ps_d[n2][:], func=AF.Copy),
                                           reads=[R_psd[n2]], writes=[R_DEN])
                                    else:
                                        op("vector", lambda e, n2=n2, nat=nat: e.tensor_tensor(
                                            out=NUM[:, nat], in0=ps_n[n2][:], in1=NUM[:, nat], op=ALU.add),
                                           reads=[R_psn[n2]], writes=[R_NUM])
                                        op("vector", lambda e, n2=n2, nat=nat: e.tensor_tensor(
                                            out=DEN[:, nat], in0=ps_d[n2][:], in1=DEN[:, nat], op=ALU.add),
                                           reads=[R_psd[n2]], writes=[R_DEN])

                    for mm in range(32 + LOOK):
                        if mm < 32:
                            front(mm)
                        if mm >= LOOK:
                            back(mm - LOOK)
                for pc in range(8):
                    o2 = pc % 2
                    cs = slice(pc * 1024, (pc + 1) * 1024)
                    op("vector", lambda e, cs=cs: e.reciprocal(out=DEN[:, cs], in_=DEN[:, cs]), writes=[R_DEN])
                    op("vector", lambda e, cs=cs, o2=o2: e.tensor_tensor(out=ostg[o2][:], in0=NUM[:, cs], in1=DEN[:, cs], op=ALU.mult),
                       reads=[R_NUM, R_DEN], writes=[R_ostg[o2]])
                    op("sync", lambda e, cs=cs, o2=o2, h=h: e.dma_start(out=OT[h][:, cs], in_=ostg[o2][:]),
                       reads=[R_ostg[o2]], writes=[R_scrC], dma=f"ostg{o2}")
            P.barrier()
            P.emit(glob)
    _phase3()
    if phases <= 3:
        return finish(nc, P, glob, out)

    TD = 256
    def _phase4():
        with ExitStack() as ph:
            prnn_t = sb(ph, "prnn_t", [128, 8, D], F32R)
            pattn_t = sb(ph, "pattn_t", [128, 4, D], F32R)
            wout_t = sb(ph, "wout_t", [128, 8, D], F32R)
            wr_t = sb(ph, "wr_t", [128, 8, 16])
            br_t = sb(ph, "br_t", [128, 16])
            gffn_t = sb(ph, "gffn_t", [128, D])
            yt = [sb(ph, f"yt{i}", [128, 8, TD], F32R) for i in range(2)]
            ot = [sb(ph, f"ot{i}", [128, 4, TD], F32R) for i in range(2)]
            sga = [sb(ph, f"sga{i}", [128, 8, TD]) for i in range(2)]
            sgb = [sb(ph, f"sgb{i}", [128, 8, TD]) for i in range(2)]
            mg = sb(ph, "mg", [128, 8, TD], F32R)
            mtmp = [sb(ph, f"mtmp{i}", [128, TD]) for i in range(2)]
            mtmp2 = [sb(ph, f"mtmpb{i}", [128, TD]) for i in range(2)]
            R_mtmp2 = R2("mtmp2")
            xt_ = [sb(ph, f"xt_{i}", [128, D]) for i in range(4)]
            x2_ = [sb(ph, f"x2_{i}", [128, D]) for i in range(2)]
            h2_ = [sb(ph, f"h2_{i}", [128, XW]) for i in range(3)]
            h2T = sb(ph, "h2T", [128, 8, 128])
            sm = sb(ph, "sm", [128, 8])
            lg = sb(ph, "lg", [128, 16])
            pb = [ph.enter_context(nc.psum_tensor(f"pbD{i}", [128, 512], F32)) for i in range(8)]
            R_pb = [Res(f"pbD{i}") for i in range(8)]
            R_wD = Res("wD")
            R_yt, R_ot, R_sga, R_sgb, R_mtmp, R_xt, R_x2, R_h2 = (R2("yt"), R2("ot"), R2("sga"), R2("sgb"), R2("mtmp"),
                                                                  R2("xt"), R2("x2"), R2("h2"))
            R_mg, R_h2T, R_sm, R_junk, R_lg = Res("mg"), Res("h2T"), Res("sm"), Res("junk"), Res("lg")
            R_xt = [Res(f"xt{i}") for i in range(4)]
            R_h2 = [Res(f"h2{i}") for i in range(3)]
            R_scrD = Res("scrD", acc=True)
            op("gpsimd", lambda e: e.dma_start(out=prnn_t[:], in_=prnn, max_dma_last_dim=4096), writes=[R_wD], dma="wD")
            op("gpsimd", lambda e: e.dma_start(out=pattn_t[:], in_=pattn, max_dma_last_dim=4096), writes=[R_wD], dma="wD")
            op("gpsimd", lambda e: e.dma_start(out=wout_t[:], in_=wout, max_dma_last_dim=4096), writes=[R_wD], dma="wD")
            op("sync", lambda e: e.dma_start(out=wr_t[:], in_=wr), writes=[R_wD], dma="wD")
            op("sync", lambda e: e.dma_start(out=br_t[:], in_=br), writes=[R_wD], dma="wD")
            op("sync", lambda e: e.dma_start(out=gffn_t[:], in_=gffn), writes=[R_wD], dma="wD")
            pbi = 0
            bcnt = 0
            YT_v = YT.rearrange("c p t -> p c t")
            OT_v = OT.rearrange("c p t -> p c t")
            SG_v = SG.rearrange("c p t -> p c t")
            def loadsD(ti):
                t0 = ti * TD
                b2 = ti % 2
                op("gpsimd", lambda e, b2=b2, t0=t0: e.dma_start(out=yt[b2][:], in_=YT_v[:, :, t0:t0 + TD]),
                   writes=[R_yt[b2]], dma=f"yt{b2}")
                op("gpsimd", lambda e, b2=b2, t0=t0: e.dma_start(out=ot[b2][:], in_=OT_v[:, :, t0:t0 + TD]),
                   writes=[R_ot[b2]], dma=f"ot{b2}")
                op("sync", lambda e, b2=b2, t0=t0: e.dma_start(out=sga[b2][:], in_=SG_v[:, 0:8, t0:t0 + TD]),
                   writes=[R_sga[b2]], dma=f"sga{b2}")
                op("sync", lambda e, b2=b2, t0=t0: e.dma_start(out=sgb[b2][:], in_=SG_v[:, 8:16, t0:t0 + TD]),
                   writes=[R_sgb[b2]], dma=f"sgb{b2}")
                for bb in range(TD // 128):
                    q4 = (ti % 2) * 2 + bb
                    tb = (ti * (TD // 128) + bb) * 128
                    op("sync", lambda e, q4=q4, tb=tb: e.dma_start(out=xt_[q4][:], in_=xtok[tb:tb + 128, :]),
                       writes=[R_xt[q4]], dma=f"xt{q4}")

            def router_q1(q2, blk, tb):
                nonlocal pbi
                pa = pbi % 8
                pbi += 1
                pa2 = pbi % 8
                pbi += 1
                for k in range(8):
                    pp = pa if k < 4 else pa2
                    op("tensor", lambda e, pp=pp, k=k, q2=q2: e.transpose(
                        out=pb[pp][:, (k % 4) * 128:(k % 4 + 1) * 128], in_=h2_[q2][:, k * 128:(k + 1) * 128], identity=ident[:]),
                       reads=[R_h2[q2]] + RC, writes=[R_pb[pp]])
                op("vector", lambda e, pa=pa: e.tensor_copy(out=h2T[:, 0:4, :].rearrange("p a b -> p (a b)"), in_=pb[pa][:]),
                   reads=[R_pb[pa]], writes=[R_h2T])
                op("scalar", lambda e, pa2=pa2: e.activation(out=h2T[:, 4:8, :].rearrange("p a b -> p (a b)"), in_=pb[pa2][:], func=AF.Copy),
                   reads=[R_pb[pa2]], writes=[R_h2T])

            def router_q2(q2, blk, tb):
                nonlocal pbi
                pa = pbi % 8
                pbi += 1
                for k in range(8):
                    op("tensor", lambda e, pa=pa, k=k: e.matmul(
                        pb[pa][:, 0:16], lhsT=h2T[:, k, :], rhs=wr_t[:, k, :], start=(k == 0), stop=(k == 7)),
                       reads=[R_h2T, R_wD], writes=[R_pb[pa]])
                op("vector", lambda e, pa=pa: e.tensor_tensor(out=lg[:], in0=pb[pa][:, 0:16], in1=br_t[:], op=ALU.add),
                   reads=[R_pb[pa], R_wD], writes=[R_lg])
                op("vector", lambda e: e.reduce_max(out=sm[:, 3:4], in_=lg[:], axis=AX.X), reads=[R_lg], writes=[R_sm])
                op("vector", lambda e: e.tensor_scalar(out=sm[:, 3:4], in0=sm[:, 3:4], scalar1=-1.0, scalar2=None, op0=ALU.mult),
                   writes=[R_sm])
                op("scalar", lambda e: e.activation(out=lg[:], in_=lg[:], func=AF.Exp, bias=sm[:, 3:4], scale=1.0,
                                                    accum_out=sm[:, 4:5]), reads=[R_sm], writes=[R_lg, R_sm])
                op("vector", lambda e: e.reciprocal(out=sm[:, 5:6], in_=sm[:, 4:5]), writes=[R_sm])
                op("vector", lambda e, blk=blk: e.tensor_scalar(
                    out=AFFt[:, blk, :], in0=lg[:], scalar1=sm[:, 5:6], scalar2=None, op0=ALU.mult),
                   reads=[R_lg, R_sm], writes=[R_aff])
                op("gpsimd", lambda e, q2=q2, blk=blk: e.tensor_copy(out=h2_[q2][:, D:D + 16], in_=AFFt[:, blk, :]),
                   reads=[R_aff], writes=[R_h2[q2]])
                op("gpsimd", lambda e, q2=q2, blk=blk: e.tensor_copy(out=h2_[q2][:, D + 16:D + 24],
                                                                      in_=cst_t[:, 640 + blk:641 + blk].to_broadcast([128, 8])),
                   reads=RC, writes=[R_h2[q2]])
                op("sync", lambda e, q2=q2, tb=tb: e.dma_start(out=H2X[tb:tb + 128, :], in_=h2_[q2][:]),
                   reads=[R_h2[q2]], writes=[R_scrD], dma=f"h2{q2}")

            pendingD = []
            loadsD(0)
            for ti in range(S // TD):
                t0 = ti * TD
                b2 = ti % 2
                if ti + 1 < S // TD:
                    loadsD(ti + 1)
                for dc in range(8):
                    pa = pbi % 8
                    pbi += 1
                    for k in range(8):
                        op("tensor", lambda e, pa=pa, k=k, dc=dc, b2=b2: e.matmul(
                            pb[pa][:, 0:TD], lhsT=prnn_t[:, k, dc * 128:(dc + 1) * 128], rhs=yt[b2][:, k, :],
                            start=(k == 0), stop=(k == 7)), reads=[R_wD, R_yt[b2]], writes=[R_pb[pa]])
                    for k in range(4):
                        op("tensor", lambda e, pa=pa, k=k, dc=dc, b2=b2: e.matmul(
                            pb[pa][:, TD:2 * TD], lhsT=pattn_t[:, k, dc * 128:(dc + 1) * 128], rhs=ot[b2][:, k, :],
                            start=(k == 0), stop=(k == 3)), reads=[R_wD, R_ot[b2]], writes=[R_pb[pa]])
                    m2 = dc % 2
                    op("vector", lambda e, pa=pa, dc=dc, b2=b2, m2=m2: e.tensor_tensor(
                        out=mtmp[m2][:], in0=pb[pa][:, 0:TD], in1=sga[b2][:, dc, :], op=ALU.mult),
                       reads=[R_pb[pa], R_sga[b2]], writes=[R_mtmp[m2]])
                    op("vector", lambda e, pa=pa, dc=dc, b2=b2, m2=m2: e.tensor_tensor(
                        out=mtmp2[m2][:], in0=pb[pa][:, TD:2 * TD], in1=sgb[b2][:, dc, :], op=ALU.mult),
                       reads=[R_pb[pa], R_sgb[b2]], writes=[R_mtmp2[m2]])
                    op("vector", lambda e, dc=dc, m2=m2: e.tensor_tensor(
                        out=mg[:, dc, :], in0=mtmp2[m2][:], in1=mtmp[m2][:], op=ALU.add),
                       reads=[R_mtmp[m2], R_mtmp2[m2]], writes=[R_mg])
                for bb in range(TD // 128):
                    blk = ti * (TD // 128) + bb
                    tb = blk * 128
                    q2 = bcnt % 2
                    q3 = bcnt % 3
                    bcnt += 1
                    q4 = (ti % 2) * 2 + bb
                    if len(pendingD) == 2:
                        router_q1(*pendingD[0])
                    pas = []
                    for hh in range(2):
                        pa = pbi % 8
                        pbi += 1
                        pas.append(pa)
                        for k in range(8):
                            op("tensor", lambda e, pa=pa, k=k, bb=bb, hh=hh: e.matmul(
                                pb[pa][:], lhsT=mg[:, k, bb * 128:(bb + 1) * 128], rhs=wout_t[:, k, hh * 512:(hh + 1) * 512],
                                start=(k == 0), stop=(k == 7)), reads=[R_mg, R_wD], writes=[R_pb[pa]])
                    if len(pendingD) == 2:
                        router_q2(*pendingD.pop(0))
                    for hh in range(2):
                        pa = pas[hh]
                        op("vector", lambda e, pa=pa, q2=q2, hh=hh, q4=q4: e.tensor_tensor(
                            out=x2_[q2][:, hh * 512:(hh + 1) * 512], in0=pb[pa][:], in1=xt_[q4][:, hh * 512:(hh + 1) * 512],
                            op=ALU.add), reads=[R_pb[pa], R_xt[q4]], writes=[R_x2[q2]])
                    op("sync", lambda e, q2=q2, tb=tb: e.dma_start(out=X2[tb:tb + 128, :], in_=x2_[q2][:]),
                       reads=[R_x2[q2]], writes=[R_scrD], dma=f"x2{q2}")
                    op("scalar", lambda e, q2=q2, q3=q3: e.activation(out=h2_[q3][:, 0:D], in_=x2_[q2][:], func=AF.Square, accum_out=sm[:, 0:1]),
                       reads=[R_x2[q2]], writes=[R_h2[q3], R_sm])
                    op("scalar", lambda e: e.activation(out=sm[:, 1:2], in_=sm[:, 0:1], func=AF.Sqrt, bias=eps_c[:, 0:1], scale=1.0 / D),
                       reads=RC, writes=[R_sm])
                    op("vector", lambda e: e.reciprocal(out=sm[:, 2:3], in_=sm[:, 1:2]), writes=[R_sm])
                    op("vector", lambda e, q2=q2, q3=q3: e.scalar_tensor_tensor(
                        out=h2_[q3][:, 0:D], in0=x2_[q2][:], scalar=sm[:, 2:3], in1=gffn_t[:], op0=ALU.mult, op1=ALU.mult),
                       reads=[R_x2[q2], R_sm, R_wD], writes=[R_h2[q3]])
                    pendingD.append((q3, blk, tb))
            while pendingD:
                router_q1(*pendingD[0])
                router_q2(*pendingD.pop(0))
            P.barrier()
            P.emit(glob)
    _phase4()
    if phases <= 4:
        return finish(nc, P, glob, out)

    def _phase5():
        with ExitStack() as ph:
            lo_t = sb(ph, "lo_t", [128, 16])
            hi_t = sb(ph, "hi_t", [128, 16])
            mid_t = sb(ph, "mid_t", [128, 16])
            cnt_t = sb(ph, "cnt_t", [128, 16])
            flg_t = sb(ph, "flg_t", [128, 16])
            t1_t = sb(ph, "t1_t", [128, 16])
            cmp_bf = sb(ph, "cmp_bf", [128, 1024], BF16)
            pos_f = sb(ph, "pos_f", [128, 1024])
            tot_f = sb(ph, "tot_f", [128, 1024])
            cum_f = sb(ph, "cum_f", [128, 1024])
            zero_f = sb(ph, "zero_f", [128, 64])
            off_i = sb(ph, "off_i", [128, 1024], I32)
            hrow = [sb(ph, f"hrow{i}", [128, XW]) for i in range(3)]
            xgr = [sb(ph, f"xgr{i}", [128, XW]) for i in range(2)]
            xgT = [sb(ph, f"xgT{i}", [128, 8, 1024], BF16) for i in range(2)]
            gate_t = [sb(ph, f"gate_t{i}", [128, 8]) for i in range(2)]
            tok_i = [sb(ph, f"tok_i{i}", [128, 8], I32) for i in range(2)]
            hidT = sb(ph, "hidT", [128, 16, 1024], BF16)
            wg_t = [sb(ph, f"wg_t{i}", [128, 8, 128], BF16) for i in range(3)]
            wu_t = [sb(ph, f"wu_t{i}", [128, 8, 128], BF16) for i in range(3)]
            wd_t = [sb(ph, f"wd_t{i}", [128, 16, 512], BF16) for i in range(2)]
            sg_t = [sb(ph, f"sg_t{i}", [128, 512]) for i in range(2)]
            eo_t = sb(ph, "eo_t", [128, 8, D])
            pb = [ph.enter_context(nc.psum_tensor(f"pbE{i}", [128, 512], F32)) for i in range(8)]
            R_pb = [Res(f"pbE{i}") for i in range(8)]
            R_bis, R_cmp, R_pos, R_off = Res("bis"), Res("cmp"), Res("pos"), Res("off")
            R_hrow = [Res(f"hrow{i}") for i in range(3)]
            R_xgr, R_xgT, R_gate = R2("xgr"), R2("xgT"), R2("gate")
            R_hid, R_eo = Res("hid"), Res("eo")
            R_wg = [Res(f"wg{i}") for i in range(3)]
            R_wu = [Res(f"wu{i}") for i in range(3)]
            R_wd, R_sg = R2("wd"), R2("sg")
            R_XG = [Res(f"XG{e}", acc=True) for e in range(16)]
            R_x2acc = Res("x2acc")
            aff_flat = AFFt[:].rearrange("p b e -> p (b e)")
            bc = lambda t: t[:].unsqueeze(1).to_broadcast([128, 64, 16])
            aff3 = AFFt[:]
            cmp3 = cmp_bf[:].rearrange("p (b e) -> p b e", e=16)
            op("vector", lambda e: e.memset(lo_t[:], 0.0), writes=[R_bis])
            op("vector", lambda e: e.memset(hi_t[:], 1.0), writes=[R_bis])
            op("vector", lambda e: e.memset(zero_f[:], 0.0), writes=[R_bis])
            for it in range(32):
                op("vector", lambda e: e.tensor_tensor(out=mid_t[:], in0=lo_t[:], in1=hi_t[:], op=ALU.add), writes=[R_bis])
                op("vector", lambda e: e.tensor_scalar(out=mid_t[:], in0=mid_t[:], scalar1=0.5, scalar2=None, op0=ALU.mult),
                   writes=[R_bis])
                op("vector", lambda e: e.tensor_tensor(out=cmp3, in0=aff3, in1=bc(mid_t), op=ALU.is_gt),
                   reads=[R_aff, R_bis], writes=[R_cmp])
                pa, pa2 = (2 * it) % 8, (2 * it + 1) % 8
                op("tensor", lambda e, pa=pa: e.matmul(pb[pa][:], lhsT=ones_bf[:], rhs=cmp_bf[:, 0:512], start=True, stop=True),
                   reads=[R_cmp] + RC, writes=[R_pb[pa]])
                op("tensor", lambda e, pa2=pa2: e.matmul(pb[pa2][:], lhsT=ones_bf[:], rhs=cmp_bf[:, 512:1024], start=True, stop=True),
                   reads=[R_cmp] + RC, writes=[R_pb[pa2]])
                op("vector", lambda e, pa=pa: e.tensor_reduce(
                    out=cnt_t[:], in_=pb[pa][:].rearrange("p (b e) -> p e b", e=16), axis=AX.X, op=ALU.add),
                   reads=[R_pb[pa]], writes=[R_bis])
                op("vector", lambda e, pa2=pa2: e.tensor_reduce(
                    out=t1_t[:], in_=pb[pa2][:].rearrange("p (b e) -> p e b", e=16), axis=AX.X, op=ALU.add),
                   reads=[R_pb[pa2]], writes=[R_bis])
                op("vector", lambda e: e.tensor_tensor(out=cnt_t[:], in0=cnt_t[:], in1=t1_t[:], op=ALU.add), writes=[R_bis])
                op("vector", lambda e: e.tensor_scalar(out=flg_t[:], in0=cnt_t[:], scalar1=1023.5, scalar2=None, op0=ALU.is_ge),
                   writes=[R_bis])
                op("vector", lambda e: e.tensor_tensor(out=t1_t[:], in0=flg_t[:], in1=mid_t[:], op=ALU.mult), writes=[R_bis])
                op("vector", lambda e: e.tensor_tensor(out=lo_t[:], in0=lo_t[:], in1=t1_t[:], op=ALU.max), writes=[R_bis])
                op("vector", lambda e: e.scalar_tensor_tensor(out=t1_t[:], in0=flg_t[:], scalar=2.0, in1=mid_t[:],
                                                              op0=ALU.mult, op1=ALU.add), writes=[R_bis])
                op("vector", lambda e: e.tensor_tensor(out=hi_t[:], in0=hi_t[:], in1=t1_t[:], op=ALU.min), writes=[R_bis])
            op("vector", lambda e: e.tensor_tensor(out=cmp3, in0=aff3, in1=bc(lo_t), op=ALU.is_gt),
               reads=[R_aff, R_bis], writes=[R_cmp])
            for hh in range(2):
                cs = slice(hh * 512, (hh + 1) * 512)
                op("tensor", lambda e, hh=hh, cs=cs: e.matmul(pb[hh][:], lhsT=ut_bf[:], rhs=cmp_bf[:, cs], start=True, stop=True),
                   reads=[R_cmp] + RC, writes=[R_pb[hh]])
                op("tensor", lambda e, hh=hh, cs=cs: e.matmul(pb[2 + hh][:], lhsT=ones_bf[:], rhs=cmp_bf[:, cs], start=True, stop=True),
                   reads=[R_cmp] + RC, writes=[R_pb[2 + hh]])
                op("vector", lambda e, hh=hh, cs=cs: e.tensor_copy(out=pos_f[:, cs], in_=pb[hh][:]), reads=[R_pb[hh]], writes=[R_pos])
                op("vector", lambda e, hh=hh, cs=cs: e.tensor_copy(out=tot_f[:, cs], in_=pb[2 + hh][:]), reads=[R_pb[2 + hh]], writes=[R_pos])
            tot3 = tot_f[:].rearrange("p (b e) -> p e b", e=16)
            cum3 = cum_f[:].rearrange("p (b e) -> p e b", e=16)
            for ee in range(16):
                op("vector", lambda e, ee=ee: e.tensor_tensor_scan(
                    out=cum3[:, ee, :], data0=tot3[:, ee, :], data1=zero_f[:], initial=0.0, op0=ALU.add, op1=ALU.add),
                   reads=[R_bis], writes=[R_pos])
            op("vector", lambda e: e.tensor_tensor(out=pos_f[:], in0=pos_f[:], in1=cum_f[:], op=ALU.add), writes=[R_pos])
            op("vector", lambda e: e.tensor_tensor(out=pos_f[:], in0=pos_f[:], in1=tot_f[:], op=ALU.subtract), writes=[R_pos])
            op("vector", lambda e: e.tensor_scalar(out=tot_f[:], in0=cmp_bf[:], scalar1=-1048576.0, scalar2=1048576.0,
                                                   op0=ALU.mult, op1=ALU.add), reads=[R_cmp], writes=[R_pos])
            op("vector", lambda e: e.tensor_tensor(out=pos_f[:], in0=pos_f[:], in1=tot_f[:], op=ALU.add), writes=[R_pos])
            op("vector", lambda e: e.tensor_copy(out=off_i[:], in_=pos_f[:]), reads=[R_pos], writes=[R_off])
            dcnt = [0]

            def seal_group(gn):
                for ee in range(4 * gn, 4 * gn + 4):
                    for h in range(3):
                        k = ("dma", f"sc{gn}_{h}")
                        if k in P.dma_vals:
                            R_XG[ee].w[k] = P.dma_vals[k]

            def dispatch_block(b, experts):
                gn_ = experts[0] // 4
                h3 = dcnt[0] % 3
                dcnt[0] += 1
                op("sync", lambda e, h3=h3, b=b: e.dma_start(out=hrow[h3][:], in_=H2X[b * 128:(b + 1) * 128, :]),
                   writes=[R_hrow[h3]], dma=f"hrow{h3}")
                for ee in experts:
                    col = b * 16 + ee
                    op("gpsimd", lambda e, h3=h3, ee=ee, col=col: e.indirect_dma_start(
                        out=XG[ee], out_offset=bass.IndirectOffsetOnAxis(ap=off_i[:, col:col + 1], axis=0),
                        in_=hrow[h3][:], in_offset=None, bounds_check=bc_reg(e, 1023), oob_is_err=False),
                       reads=[R_hrow[h3], R_off], writes=[R_XG[ee]], dma=f"sc{gn_}_{h3}")

            for b in range(64):
                dispatch_block(b, range(0, 4))
            seal_group(0)
            pbi = 0
            wci = 0
            sgi = 0

            def gather_expert(ee):
                x2 = ee % 2
                for sbk in range(8):
                    g2 = (ee * 8 + sbk) % 2
                    op("sync", lambda e, g2=g2, ee=ee, sbk=sbk: e.dma_start(out=xgr[g2][:], in_=XG[ee][sbk * 128:(sbk + 1) * 128, :]),
                       reads=[R_XG[ee]], writes=[R_xgr[g2]], dma=f"xgr{g2}")
                    nonlocal_pb = []
                    for half in range(2):
                        pa = gather_expert.pbi % 8
                        gather_expert.pbi += 1
                        for q in range(4):
                            k = half * 4 + q
                            op("tensor", lambda e, pa=pa, q=q, k=k, g2=g2: e.transpose(
                                out=pb[pa][:, q * 128:(q + 1) * 128], in_=xgr[g2][:, k * 128:(k + 1) * 128], identity=ident[:]),
                               reads=[R_xgr[g2]] + RC, writes=[R_pb[pa]])
                        dst = xgT[x2][:, half * 4:(half + 1) * 4, sbk * 128:(sbk + 1) * 128]
                        src = pb[pa][:].rearrange("p (a b) -> p a b", a=4)
                        if half == 0:
                            op("vector", lambda e, dst=dst, src=src: e.tensor_copy(out=dst, in_=src),
                               reads=[R_pb[pa]], writes=[R_xgT[x2]])
                        else:
                            op("scalar", lambda e, dst=dst, src=src: e.activation(out=dst, in_=src, func=AF.Copy),
                               reads=[R_pb[pa]], writes=[R_xgT[x2]])
                    op("gpsimd", lambda e, g2=g2, x2=x2, sbk=sbk, ee=ee: e.tensor_copy(
                        out=gate_t[x2][:, sbk:sbk + 1], in_=xgr[g2][:, D + ee:D + ee + 1]),
                       reads=[R_xgr[g2]], writes=[R_gate[x2]])
                    op("gpsimd", lambda e, g2=g2, x2=x2, sbk=sbk: e.tensor_copy(
                        out=tok_i[x2][:, sbk:sbk + 1], in_=xgr[g2][:, D + 16:D + 17]),
                       reads=[R_xgr[g2]], writes=[R_gate[x2]])
            gather_expert.pbi = 0

            R_accbar = [Res(f"accbar{e}", acc=True) for e in range(16)]

            def acc_scatter(ex, cb):
                x2_ = ex % 2
                rd = [R_eo, R_gate[x2_]] + ([R_accbar[ex - 1]] if ex > 0 else [])
                op("gpsimd", lambda e, cb=cb, x2_=x2_: e.indirect_dma_start(
                    out=X2, out_offset=bass.IndirectOffsetOnAxis(ap=tok_i[x2_][:, cb:cb + 1], axis=0),
                    in_=eo_t[:, cb, :], in_offset=None, bounds_check=bc_reg(e, S - 1), oob_is_err=True, compute_op=ALU.add),
                   reads=rd, writes=[R_accbar[ex]], dma="acc")

            def load_wd(ex, dh):
                op("gpsimd", lambda e, dh=dh, ex=ex: e.dma_start(out=wd_t[dh][:], in_=wd[ex, dh], max_dma_last_dim=4096),
                   writes=[R_wd[dh]], dma=f"wd{dh}")

            gather_expert(0)
            for ee in range(16):
                x2 = ee % 2
                pbi = gather_expert.pbi
                for f in range(16):
                    w3 = wci % 3
                    wci += 1
                    op("gpsimd", lambda e, w3=w3, ee=ee, f=f: e.dma_start(out=wg_t[w3][:], in_=wg[ee, f]),
                       writes=[R_wg[w3]], dma=f"wg{w3}")
                    op("gpsimd", lambda e, w3=w3, ee=ee, f=f: e.dma_start(out=wu_t[w3][:], in_=wu[ee, f]),
                       writes=[R_wu[w3]], dma=f"wu{w3}")
                    if ee > 0 and 4 <= f < 12:
                        acc_scatter(ee - 1, f - 4)
                    if f == 2:
                        load_wd(ee, 1)
                        if ee == 0:
                            load_wd(0, 0)
                    if ee // 4 + 1 < 4:
                        gn = ee // 4 + 1
                        dispatch_block((ee % 4) * 16 + f, range(4 * gn, 4 * gn + 4))
                        if ee % 4 == 3 and f == 15:
                            seal_group(gn)
                    for half in range(2):
                        hs = slice(half * 512, (half + 1) * 512)
                        pg = pbi % 8
                        pbi += 1
                        pu = pbi % 8
                        pbi += 1
                        for k in range(8):
                            op("tensor", lambda e, pg=pg, k=k, w3=w3, x2=x2, hs=hs: e.matmul(
                                pb[pg][:], lhsT=wg_t[w3][:, k, :], rhs=xgT[x2][:, k, hs], start=(k == 0), stop=(k == 7)),
                               reads=[R_wg[w3], R_xgT[x2]], writes=[R_pb[pg]])
                        for k in range(8):
                            op("tensor", lambda e, pu=pu, k=k, w3=w3, x2=x2, hs=hs: e.matmul(
                                pb[pu][:], lhsT=wu_t[w3][:, k, :], rhs=xgT[x2][:, k, hs], start=(k == 0), stop=(k == 7)),
                               reads=[R_wu[w3], R_xgT[x2]], writes=[R_pb[pu]])
                        s2 = sgi % 2
                        sgi += 1
                        op("scalar", lambda e, pg=pg, s2=s2: e.activation(out=sg_t[s2][:], in_=pb[pg][:], func=AF.Silu),
                           reads=[R_pb[pg]], writes=[R_sg[s2]])
                        op("vector", lambda e, pu=pu, s2=s2, f=f, hs=hs: e.tensor_tensor(
                            out=hidT[:, f, hs], in0=pb[pu][:], in1=sg_t[s2][:], op=ALU.mult),
                           reads=[R_pb[pu], R_sg[s2]], writes=[R_hid])
                gather_expert.pbi = pbi
                if ee + 1 < 16:
                    gather_expert(ee + 1)
                pbi = gather_expert.pbi
                for dh in range(2):
                    if dh == 1 and ee + 1 < 16:
                        load_wd(ee + 1, 0)
                    for cb in range(8):
                        pa = pbi % 8
                        pbi += 1
                        for f in range(16):
                            op("tensor", lambda e, pa=pa, f=f, cb=cb, dh=dh: e.matmul(
                                pb[pa][:], lhsT=hidT[:, f, cb * 128:(cb + 1) * 128], rhs=wd_t[dh][:, f, :],
                                start=(f == 0), stop=(f == 15)), reads=[R_hid, R_wd[dh]], writes=[R_pb[pa]])
                        if cb % 2 == 0:
                            op("vector", lambda e, pa=pa, cb=cb, dh=dh, x2=x2: e.tensor_scalar(
                                out=eo_t[:, cb, dh * 512:(dh + 1) * 512], in0=pb[pa][:], scalar1=gate_t[x2][:, cb:cb + 1],
                                scalar2=None, op0=ALU.mult), reads=[R_pb[pa], R_gate[x2]], writes=[R_eo])
                        else:
                            op("scalar", lambda e, pa=pa, cb=cb, dh=dh, x2=x2: e.activation(
                                out=eo_t[:, cb, dh * 512:(dh + 1) * 512], in_=pb[pa][:], func=AF.Copy,
                                scale=gate_t[x2][:, cb:cb + 1]), reads=[R_pb[pa], R_gate[x2]], writes=[R_eo])
                gather_expert.pbi = pbi
                if ee == 15:
                    for cb in range(8):
                        acc_scatter(15, cb)
            P.barrier()
            P.emit(glob)
    _phase5()
    if phases <= 5:
        return finish(nc, P, glob, out)

    def _phase6():
        with ExitStack() as ph:
            gfin_t = sb(ph, "gfin_t", [128, D])
            xa = [sb(ph, f"xa{i}", [128, D]) for i in range(3)]
            ya = [sb(ph, f"ya{i}", [128, D]) for i in range(3)]
            junk = sb(ph, "junkF", [128, D])
            sm = sb(ph, "smF", [128, 4])
            R_xa = [Res(f"xa{i}") for i in range(3)]
            R_ya = [Res(f"ya{i}") for i in range(3)]
            R_junk, R_sm, R_g = Res("junkF"), Res("smF"), Res("gfin")
            R_out = Res("out", acc=True)
            op("sync", lambda e: e.dma_start(out=gfin_t[:], in_=gfin), writes=[R_g], dma="gfin")
            def loadF(b):
                op("sync", lambda e, b=b: e.dma_start(out=xa[b % 3][:], in_=X2[b * 128:(b + 1) * 128, :]),
                   writes=[R_xa[b % 3]], dma=f"xa{b % 3}")

            loadF(0)
            loadF(1)
            for b in range(64):
                b3 = b % 3
                if b + 2 < 64:
                    loadF(b + 2)
                op("scalar", lambda e, b3=b3: e.activation(out=junk[:], in_=xa[b3][:], func=AF.Square, accum_out=sm[:, 0:1]),
                   reads=[R_xa[b3]], writes=[R_junk, R_sm])
                op("scalar", lambda e: e.activation(out=sm[:, 1:2], in_=sm[:, 0:1], func=AF.Sqrt, bias=eps_c[:, 0:1], scale=1.0 / D),
                   reads=RC, writes=[R_sm])
                op("vector", lambda e: e.reciprocal(out=sm[:, 2:3], in_=sm[:, 1:2]), writes=[R_sm])
                op("vector", lambda e, b3=b3: e.scalar_tensor_tensor(
                    out=ya[b3][:], in0=xa[b3][:], scalar=sm[:, 2:3], in1=gfin_t[:], op0=ALU.mult, op1=ALU.mult),
                   reads=[R_xa[b3], R_sm, R_g], writes=[R_ya[b3]])
                op("sync", lambda e, b3=b3, b=b: e.dma_start(out=out[b * 128:(b + 1) * 128, :], in_=ya[b3][:]),
                   reads=[R_ya[b3]], writes=[R_out], dma=f"ya{b3}")
            P.barrier()
            P.emit(glob)
    _phase6()
    return finish(nc, P, glob, out)


def finish(nc, P, glob, out):
    P.barrier()
    P.emit(glob)
    glob.close()
    return nc


def _const_tables():
    ident = np.eye(128, dtype=np.float32)
    ut = np.triu(np.ones((128, 128), np.float32), 1)
    i = np.arange(128)[:, None]
    c = np.arange(256)[None, :]
    c = np.arange(384)[None, :]
    mask = ((c >= i + 64) & (c <= i + 192)).astype(np.float32)
    tokb = (np.arange(64)[None, :] * 128 + np.arange(128)[:, None]).astype(np.float32)
    cst = np.concatenate([ident, ut, mask, tokb], axis=1)
    pos = np.arange(S, dtype=np.float32)
    inv = (np.float32(500000.0) ** (-np.arange(0, 32, 2, dtype=np.float32) / np.float32(32))).astype(np.float32)
    ang = (pos[None, :] * inv[:, None]).astype(np.float32)
    cos, sin = np.cos(ang).astype(np.float32), np.sin(ang).astype(np.float32)
    one = np.ones((16, S), np.float32)
    zero = np.zeros((16, S), np.float32)
    cosT = np.concatenate([cos, one, cos, one], axis=0)
    sinT = np.concatenate([sin, zero, -sin, zero], axis=0)
    return cst, np.ascontiguousarray(cosT), np.ascontiguousarray(sinT)


HEAD_PERM = np.array(list(range(16)) + list(range(32, 48)) + list(range(16, 32)) + list(range(48, 128)))


def prep_shared(inp):
    f = lambda a: np.ascontiguousarray(np.asarray(a, dtype=np.float32))
    w_in = f(inp["w_in"])[0]
    b_in = f(inp["b_in"])[0]
    colperm = np.arange(8704)
    for c in range(16, 40):
        colperm[c * 128:(c + 1) * 128] = c * 128 + HEAD_PERM
    w_in = w_in[:, colperm]
    b_in = b_in[colperm]
    cst, cosT, sinT = _const_tables()
    sh = {
        "w_in": f(w_in.reshape(8, 128, NCH, 128).transpose(2, 1, 0, 3)),
        "b_in": f(b_in.reshape(NCH, 128).T),
        "gmix": f(inp["norm_mix"][0].reshape(8, 128).T),
        "convw": f(np.asarray(inp["conv_w"])[0].reshape(4, 8, 128).transpose(2, 0, 1)),
        "convb": f(np.asarray(inp["conv_b"])[0].reshape(8, 128).T),
        "rgw": f(np.asarray(inp["rg_w"])[0].reshape(4, 8, 128, 128).transpose(1, 2, 0, 3)),
        "rgb": f(np.asarray(inp["rg_b"])[0].reshape(4, 8, 128).transpose(2, 0, 1)),
        "lam": f(np.asarray(inp["rg_lambda"])[0].reshape(2, 8, 128).transpose(2, 0, 1)),
        "prnn": f(np.asarray(inp["p_rnn"])[0].reshape(8, 128, D).transpose(1, 0, 2)),
        "pattn": f(np.asarray(inp["p_attn"])[0].reshape(4, 128, D).transpose(1, 0, 2)),
        "wout": f(np.asarray(inp["w_out"])[0].reshape(8, 128, D).transpose(1, 0, 2)),
        "gffn": f(np.broadcast_to(np.asarray(inp["norm_ffn"])[0][None, :], (128, D))),
        "gfin": f(np.broadcast_to(np.asarray(inp["norm_final"])[None, :], (128, D))),
        "wr": f(np.asarray(inp["w_router"])[0].reshape(8, 128, 16).transpose(1, 0, 2)),
        "br": f(np.broadcast_to(np.asarray(inp["b_router"])[0][None, :], (128, 16))),
        "wg": f(np.asarray(inp["w_gate"])[0].reshape(16, 8, 128, 16, 128).transpose(0, 3, 2, 1, 4)),
        "wu": f(np.asarray(inp["w_up"])[0].reshape(16, 8, 128, 16, 128).transpose(0, 3, 2, 1, 4)),
        "wd": f(np.asarray(inp["w_down"])[0].reshape(16, 16, 128, 2, 512).transpose(0, 3, 2, 1, 4)),
        "cosT": cosT, "sinT": sinT, "cst": cst,
    }
    return sh


_NC_CACHE = {}


def kernel(**inputs):
    x = np.asarray(inputs["x"], dtype=np.float32)
    B = x.shape[0]
    sh = prep_shared(inputs)
    if "nc" not in _NC_CACHE:
        _NC_CACHE["nc"] = build()
    nc = _NC_CACHE["nc"]
    in_maps = []
    for b in range(B):
        m = dict(sh)
        m["xtok"] = np.ascontiguousarray(x[b])
        m["xT"] = np.ascontiguousarray(x[b].T)
        in_maps.append(m)
    res = run_bass_kernel_spmd(nc, in_maps, core_ids=list(range(B)))
    return np.stack([np.asarray(r["out"], dtype=np.float32) for r in res.results], axis=0)
```

```python
import numpy as np
from contextlib import ExitStack
import concourse.bass as bass
import concourse.mybir as mybir
from concourse.bass_utils import run_bass_kernel_spmd

F32 = mybir.dt.float32
F32R = mybir.dt.float32r
BF16 = mybir.dt.bfloat16
I32 = mybir.dt.int32
AF = mybir.ActivationFunctionType
ALU = mybir.AluOpType
AX = mybir.AxisListType

ENGS = ("sync", "scalar", "vector", "gpsimd", "tensor")
EPOCH = 30000
S = 8192
D = 1024
NCH = 68
XW = 1048


class Res:
    __slots__ = ("name", "w", "r", "acc")

    def __init__(self, name, acc=False):
        self.name = name
        self.w = {}
        self.r = {}
        self.acc = acc


class Prog:
    def __init__(self, nc, same_engine_sync=("scalar", "vector", "gpsimd")):
        self.nc = nc
        self.ops = {e: [] for e in ENGS}
        self.cnt = {e: 0 for e in ENGS}
        self.seen = {e: {} for e in ENGS}
        self.dma_vals = {}
        self.same = set(same_engine_sync)
        self.sem_keys = []
        self.final = {}

    def _key(self, k):
        if k not in self.final:
            self.sem_keys.append(k)
        return k

    def op(self, eng, fn, reads=(), writes=(), dma=None):
        need = {}
        for r in reads:
            for k, v in r.w.items():
                if need.get(k, 0) < v:
                    need[k] = v
        for w in writes:
            if not w.acc:
                for k, v in w.w.items():
                    if need.get(k, 0) < v:
                        need[k] = v
            for k, v in w.r.items():
                if need.get(k, 0) < v:
                    need[k] = v
        waits = []
        seen = self.seen[eng]
        for k, v in need.items():
            if seen.get(k, 0) >= v:
                continue
            if k[0] == eng and eng not in self.same:
                continue
            waits.append((k, v))
            seen[k] = v
        if dma is None:
            c = self.cnt[eng]
            self.cnt[eng] = c + 1
            key = self._key((eng, c // EPOCH))
            ev = (key, c % EPOCH + 1)
            inc = 1
        else:
            key = self._key(("dma", dma))
            val = self.dma_vals.get(key, 0) + 16
            self.dma_vals[key] = val
            ev = (key, val)
            inc = 16
        self.final[key] = ev[1]
        for r in reads:
            if r.r.get(ev[0], 0) < ev[1]:
                r.r[ev[0]] = ev[1]
        for w in writes:
            if w.acc:
                if w.w.get(ev[0], 0) < ev[1]:
                    w.w[ev[0]] = ev[1]
            else:
                w.w = {ev[0]: ev[1]}
                w.r = {}
        self.ops[eng].append((waits, fn, key, inc))

    def wait_all(self, eng, own=False):
        waits = []
        for k, v in self.final.items():
            if self.seen[eng].get(k, 0) < v and (own or k[0] != eng):
                waits.append((k, v))
                self.seen[eng][k] = v
        if waits:
            self.ops[eng].append((waits, None, None, 0))

    def barrier(self):
        for e in ENGS:
            self.wait_all(e, own=(e in self.same))

    def emit(self, st):
        nc = self.nc
        if not hasattr(self, "sems"):
            self.sems = {}
        sems = self.sems
        for k in self.sem_keys:
            if k not in sems:
                sems[k] = st.enter_context(nc.semaphore("s_" + "_".join(str(x) for x in k)))
        self.nblk = getattr(self, "nblk", 0) + 1
        with nc.named_scope(f"ph{self.nblk}"), nc.Block() as block:
            for e in ENGS:
                ops = self.ops[e]
                if not ops:
                    continue

                def body(eng, ops=ops):
                    for waits, fn, key, inc in ops:
                        for k, v in waits:
                            eng.wait_ge(sems[k], v)
                        if fn is not None:
                            fn(eng).then_inc(sems[key], inc)
                getattr(block, e)(body)
        self.ops = {e: [] for e in ENGS}


def chunk_kind(c):
    if c < 8:
        return "xr"
    if c < 16:
        return "gr"
    if c < 28:
        return "q"
    if c < 40:
        return "k"
    if c < 52:
        return "v"
    return "gate"


DIL = (1, 4, 16)


def build(phases=6, debug=False):
    nc = bass.Bass("TRN2", target_bir_lowering=False)

    def din(name, shape, dt=F32):
        return nc.dram_tensor(name, list(shape), dt, kind="ExternalInput").ap()

    def dscr(name, shape, dt=F32):
        kind = "ExternalOutput" if debug else "Internal"
        return nc.dram_tensor(name, list(shape), dt, kind=kind).ap()

    xT = din("xT", [D, S])
    xtok = din("xtok", [S, D])
    w_in = din("w_in", [NCH, 128, 8, 128])
    b_in = din("b_in", [128, NCH])
    gmix = din("gmix", [128, 8])
    convw = din("convw", [128, 4, 8])
    convb = din("convb", [128, 8])
    rgw = din("rgw", [8, 128, 4, 128])
    rgb = din("rgb", [128, 4, 8])
    lam = din("lam", [128, 2, 8])
    prnn = din("prnn", [128, 8, D])
    pattn = din("pattn", [128, 4, D])
    wout = din("wout", [128, 8, D])
    gffn = din("gffn", [128, D])
    gfin = din("gfin", [128, D])
    wr = din("wr", [128, 8, 16])
    br = din("br", [128, 16])
    wg = din("wg", [16, 16, 128, 8, 128])
    wu = din("wu", [16, 16, 128, 8, 128])
    wd = din("wd", [16, 2, 128, 16, 512])
    cosT = din("cosT", [64, S])
    sinT = din("sinT", [64, S])
    cst = din("cst", [128, 128 + 128 + 384 + 64])

    out = nc.dram_tensor("out", [S, D], F32, kind="ExternalOutput").ap()

    XR = dscr("XR", [8, 128, S])
    GG = dscr("GG", [8, 128, S])
    QKV = dscr("QKV", [36, 128, S], BF16)
    SG = dscr("SG", [16, 128, S])
    YT = dscr("YT", [8, 128, S])
    OT = dscr("OT", [4, 128, S])
    X2 = dscr("X2", [S, D])
    H2X = dscr("H2X", [S, XW])
    XG = [dscr(f"XG{e}", [1024, XW]) for e in range(16)]

    P = Prog(nc)
    op = P.op
    R2 = lambda n: [Res(n + "0"), Res(n + "1")]
    bregs = {}

    def bc_reg(e, val):
        if val not in bregs:
            r = e.alloc_register(f"bc{val}")
            e.reg_mov(r, val)
            bregs[val] = r
        return bregs[val]
    glob = ExitStack()

    def sb(st, name, shape, dt=F32):
        return st.enter_context(nc.sbuf_tensor(name, list(shape), dt))

    ident = sb(glob, "ident", [128, 128])
    ident_bf = sb(glob, "ident_bf", [128, 128], BF16)
    ut_bf = sb(glob, "ut_bf", [128, 128], BF16)
    ones_bf = sb(glob, "ones_bf", [128, 128], BF16)
    ones_r = sb(glob, "ones_r", [128, 128], F32R)
    pmask = [sb(glob, f"pmask{i}", [128, 512], BF16) for i in range(3)]
    cst_t = sb(glob, "cst_t", [128, 704])
    one_c = sb(glob, "one_c", [128, 1])
    eps_c = sb(glob, "eps_c", [128, 1])
    AFFt = sb(glob, "AFFt", [128, 64, 16])
    R_const = Res("const")
    R_aff = Res("aff")
    op("sync", lambda e: e.dma_start(out=cst_t[:], in_=cst), writes=[R_const], dma="cst")
    op("vector", lambda e: e.tensor_copy(out=ident[:], in_=cst_t[:, 0:128]), reads=[R_const], writes=[R_const])
    op("vector", lambda e: e.tensor_copy(out=ident_bf[:], in_=cst_t[:, 0:128]), reads=[R_const], writes=[R_const])
    op("vector", lambda e: e.tensor_copy(out=ut_bf[:], in_=cst_t[:, 128:256]), reads=[R_const], writes=[R_const])
    for i_, (o0_, o1_) in enumerate(((64, 64), (128, 64), (64, 0))):
        op("vector", lambda e, i_=i_, o0_=o0_: e.tensor_copy(out=pmask[i_][:, 0:256], in_=cst_t[:, 256 + o0_:512 + o0_]),
           reads=[R_const], writes=[R_const])
        op("vector", lambda e, i_=i_, o1_=o1_: e.tensor_copy(out=pmask[i_][:, 256:512], in_=cst_t[:, 256 + o1_:512 + o1_]),
           reads=[R_const], writes=[R_const])
    op("vector", lambda e: e.memset(ones_bf[:], 1.0), writes=[R_const])
    op("vector", lambda e: e.memset(ones_r[:].bitcast(F32), 1.0), writes=[R_const])
    op("vector", lambda e: e.memset(one_c[:], 1.0), writes=[R_const])
    op("vector", lambda e: e.memset(eps_c[:], 1e-6), writes=[R_const])
    RC = [R_const]

    def _phase1():
        with ExitStack() as ph:
            hT = sb(ph, "hT", [128, 8, 2048], F32R)
            xs = [sb(ph, f"xs{i}", [128, 8, 512]) for i in range(2)]
            rstd2 = [sb(ph, f"rstd{i}", [128, 512]) for i in range(2)]
            R_rstd2 = [Res("rstd0"), Res("rstd1")]
            wch = [sb(ph, f"wch{i}", [128, 8, 128], F32R) for i in range(3)]
            stg = [sb(ph, f"stg{i}", [128, 2048]) for i in range(4)]
            stgb = [sb(ph, f"stgb{i}", [128, 2048], BF16) for i in range(4)]
            rtmp = [sb(ph, f"rtmp{i}", [64, 2048]) for i in range(2)]
            cos_t = sb(ph, "cos_t", [64, 2048])
            sin_t = sb(ph, "sin_t", [64, 2048])
            bin_t = sb(ph, "bin_t", [128, NCH])
            binq_t = sb(ph, "binq_t", [128, NCH])
            gmix_t = sb(ph, "gmix_t", [128, 8])
            pb = [ph.enter_context(nc.psum_tensor(f"pbA{i}", [128, 512], F32)) for i in range(8)]
            R_pb = [Res(f"pbA{i}") for i in range(8)]
            R_hTt = [Res(f"hT{j}") for j in range(4)]
            R_xs = [Res("xs0"), Res("xs1")]
            R_wch = [Res(f"wch{i}") for i in range(3)]
            R_stg = [Res(f"stg{i}") for i in range(4)]
            R_stgb = [Res(f"stgb{i}") for i in range(4)]
            R_rtmp = [Res("rtmp0"), Res("rtmp1")]
            R_cs = Res("cossin")
            R_scrA = Res("scrA", acc=True)
            QSC = 128.0 ** -0.5

            op("sync", lambda e: e.dma_start(out=bin_t[:], in_=b_in), writes=RC, dma="cst")
            op("sync", lambda e: e.dma_start(out=gmix_t[:], in_=gmix), writes=RC, dma="cst")
            op("vector", lambda e: e.tensor_scalar(out=binq_t[:], in0=bin_t[:], scalar1=QSC, scalar2=None, op0=ALU.mult),
               reads=RC, writes=RC)
            xT_v = xT.rearrange("(k p) t -> p k t", p=128)

            def load_x(st_, j_):
                tt_ = st_ * 2048 + j_ * 512
                op("sync", lambda e, j_=j_, tt_=tt_: e.dma_start(out=xs[j_ % 2][:], in_=xT_v[:, :, tt_:tt_ + 512]),
                   writes=[R_xs[j_ % 2]], dma=f"xs{j_ % 2}")

            def issue_w(idx):
                cc = idx % NCH
                op("gpsimd", lambda e, idx=idx, cc=cc: e.dma_start(out=wch[idx % 3][:], in_=w_in[cc]),
                   writes=[R_wch[idx % 3]], dma=f"wch{idx % 3}")
            pbi = 0
            wi = 0
            si = 0
            pending = []
            for st_i in range(4):
                t0 = st_i * 2048
                op("sync", lambda e, t0=t0: e.dma_start(out=cos_t[:], in_=cosT[:, t0:t0 + 2048]), writes=[R_cs], dma="cs")
                op("sync", lambda e, t0=t0: e.dma_start(out=sin_t[:], in_=sinT[:, t0:t0 + 2048]), writes=[R_cs], dma="cs")
                if st_i == 0:
                    load_x(0, 0)
                    load_x(0, 1)
                for j in range(4):
                    xb = xs[j % 2]
                    Rx = R_xs[j % 2]
                    rs = rstd2[j % 2]
                    Rrs = R_rstd2[j % 2]
                    js_ = slice(j * 512, (j + 1) * 512)
                    op("scalar", lambda e, xb=xb, js_=js_: e.activation(out=hT[:, :, js_], in_=xb[:], func=AF.Square),
                       reads=[Rx], writes=[R_hTt[j]])
                    pbk = pbi % 8
                    pbi += 1
                    for k in range(8):
                        op("tensor", lambda e, k=k, pbk=pbk, js_=js_: e.matmul(pb[pbk][:], lhsT=ones_r[:], rhs=hT[:, k, js_],
                                                                              start=(k == 0), stop=(k == 7)),
                           reads=[R_hTt[j]] + RC, writes=[R_pb[pbk]])
                    op("scalar", lambda e, pbk=pbk, rs=rs: e.activation(out=rs[:], in_=pb[pbk][:], func=AF.Sqrt,
                                                                        bias=eps_c[:, 0:1], scale=1.0 / D),
                       reads=[R_pb[pbk]] + RC, writes=[Rrs])
                    op("vector", lambda e, rs=rs: e.reciprocal(out=rs[:], in_=rs[:]), reads=[Rrs], writes=[Rrs])
                    for k in range(8):
                        op("vector", lambda e, k=k, xb=xb, js_=js_, rs=rs: e.scalar_tensor_tensor(
                            out=hT[:, k, js_], in0=xb[:, k, :], scalar=gmix_t[:, k:k + 1],
                            in1=rs[:], op0=ALU.mult, op1=ALU.mult),
                           reads=[Rx, Rrs] + RC, writes=[R_hTt[j]])
                    if j + 2 < 4:
                        load_x(st_i, j + 2)
                for c in range(NCH):
                    kind = chunk_kind(c)
                    wb = wch[wi % 3]
                    Rw = R_wch[wi % 3]
                    if wi == 0:
                        issue_w(0)
                        issue_w(1)
                    if wi + 2 < 4 * NCH:
                        issue_w(wi + 2)
                    wi += 1
                    if c == 56 and st_i + 1 < 4:
                        load_x(st_i + 1, 0)
                        load_x(st_i + 1, 1)
                    sgi = si % 4
                    si += 1
                    use_b = kind in ("q", "k", "v")
                    for j in range(4):
                        pbk = pbi % 8
                        pbi += 1
                        for k in range(8):
                            op("tensor", lambda e, k=k, pbk=pbk, wb=wb, j=j: e.matmul(
                                pb[pbk][:], lhsT=wb[:, k, :], rhs=hT[:, k, j * 512:(j + 1) * 512],
                                start=(k == 0), stop=(k == 7)),
                               reads=[Rw, R_hTt[j]], writes=[R_pb[pbk]])
                        js = slice(j * 512, (j + 1) * 512)
                        if kind == "xr":
                            fn = lambda e, pbk=pbk, js=js, c=c, sgi=sgi: e.activation(
                                out=stg[sgi][:, js], in_=pb[pbk][:], func=AF.Identity, bias=bin_t[:, c:c + 1], scale=1.0)
                            wr_ = [R_stg[sgi]]
                        elif kind == "gr":
                            fn = lambda e, pbk=pbk, js=js, c=c, sgi=sgi: e.activation(
                                out=stg[sgi][:, js], in_=pb[pbk][:], func=AF.Gelu, bias=bin_t[:, c:c + 1], scale=1.0)
                            wr_ = [R_stg[sgi]]
                        elif kind == "gate":
                            fn = lambda e, pbk=pbk, js=js, c=c, sgi=sgi: e.activation(
                                out=stg[sgi][:, js], in_=pb[pbk][:], func=AF.Sigmoid, bias=bin_t[:, c:c + 1], scale=1.0)
                            wr_ = [R_stg[sgi]]
                        elif kind == "q":
                            fn = lambda e, pbk=pbk, js=js, c=c, sgi=sgi: e.activation(
                                out=stg[sgi][:, js], in_=pb[pbk][:], func=AF.Identity, bias=binq_t[:, c:c + 1], scale=QSC)
                            wr_ = [R_stg[sgi]]
                        elif kind == "k":
                            fn = lambda e, pbk=pbk, js=js, c=c, sgi=sgi: e.activation(
                                out=stg[sgi][:, js], in_=pb[pbk][:], func=AF.Identity, bias=bin_t[:, c:c + 1], scale=1.0)
                            wr_ = [R_stg[sgi]]
                        else:
                            dv = DIL[((c - 16) % 12) // 4]
                            nj = 512 // dv
                            fn = lambda e, pbk=pbk, j=j, c=c, sgi=sgi, dv=dv, nj=nj: e.activation(
                                out=stgb[sgi][:].rearrange("p (r l) -> p r l", r=dv)[:, :, j * nj:(j + 1) * nj],
                                in_=pb[pbk][:].rearrange("p (l r) -> p r l", r=dv),
                                func=AF.Identity, bias=bin_t[:, c:c + 1], scale=1.0)
                            wr_ = [R_stgb[sgi]]
                        op("scalar", fn, reads=[R_pb[pbk]] + RC, writes=wr_)
                    tail_ops = []
                    if use_b:
                        g = ((c - 16) % 12) // 4
                        d = DIL[g]
                        sgt = stg[sgi]
                        sbt = stgb[sgi]
                        if kind in ("q", "k"):
                            op("vector", lambda e, sgt=sgt: e.tensor_tensor(
                                out=rtmp[0][0:32, :], in0=sgt[32:64, :], in1=sin_t[32:64, :], op=ALU.mult),
                               reads=[R_stg[sgi], R_cs], writes=[R_rtmp[0]])
                            op("vector", lambda e, sgt=sgt: e.tensor_tensor(
                                out=rtmp[0][32:64, :], in0=sgt[0:32, :], in1=sin_t[0:32, :], op=ALU.mult),
                               reads=[R_stg[sgi], R_cs], writes=[R_rtmp[0]])
                            op("vector", lambda e, sgt=sgt: e.tensor_tensor(
                                out=rtmp[1][:, :], in0=sgt[0:64, :], in1=cos_t[:, :], op=ALU.mult),
                               reads=[R_cs, R_stg[sgi]], writes=[R_rtmp[1]])
                            op("vector", lambda e, sgt=sgt: e.tensor_tensor(
                                out=sgt[0:64, :], in0=rtmp[0][:, :], in1=rtmp[1][:, :], op=ALU.add),
                               reads=[R_rtmp[0], R_rtmp[1]], writes=[R_stg[sgi]])
                            tail_ops.append(("scalar", lambda e, sgt=sgt, sbt=sbt, d=d: e.activation(
                                out=sbt[:].rearrange("p (r j) -> p r j", r=d),
                                in_=sgt[:].rearrange("p (j r) -> p r j", r=d), func=AF.Copy),
                                [R_stg[sgi]], [R_stgb[sgi]], None))
                        qi = c - 16
                        j0 = t0 // d
                        nj = 2048 // d
                        dst = QKV[qi].rearrange("p (r l) -> p r l", r=d)[:, :, j0:j0 + nj]
                        src = sbt[:].rearrange("p (r j) -> p r j", r=d)
                        tail_ops.append(("sync", lambda e, dst=dst, src=src: e.dma_start(out=dst, in_=src),
                                         [R_stgb[sgi]], [R_scrA], f"stgb{sgi}"))
                    else:
                        if kind == "xr":
                            dst = XR[c][:, t0:t0 + 2048]
                        elif kind == "gr":
                            dst = GG[c - 8][:, t0:t0 + 2048]
                        else:
                            dst = SG[c - 52][:, t0:t0 + 2048]
                        tail_ops.append(("sync", lambda e, dst=dst, sgi=sgi: e.dma_start(out=dst, in_=stg[sgi][:]),
                                         [R_stg[sgi]], [R_scrA], f"stg{sgi}"))
                    for (en_, fn_, rd_, wr2_, dm_) in pending:
                        op(en_, fn_, reads=rd_, writes=wr2_, dma=dm_)
                    pending[:] = tail_ops
            for (en_, fn_, rd_, wr2_, dm_) in pending:
                op(en_, fn_, reads=rd_, writes=wr2_, dma=dm_)
            P.barrier()
            P.emit(glob)
    _phase1()
    if phases <= 1:
        return finish(nc, P, glob, out)

    SEG = 1024
    NSEG = S // SEG
    def _phase2():
        with ExitStack() as ph:
            xc2 = [sb(ph, f"xc{i}", [128, S], F32R) for i in range(2)]
            hf = sb(ph, "hf", [128, S])
            raw = [sb(ph, f"raw{i}", [128, SEG + 3]) for i in range(2)]
            xcr = [sb(ph, f"xcr{i}", [128, SEG]) for i in range(2)]
            r_ = [sb(ph, f"r_{i}", [128, SEG]) for i in range(2)]
            i_ = [sb(ph, f"i_{i}", [128, SEG]) for i in range(2)]
            a2_ = [sb(ph, f"a2_{i}", [128, SEG]) for i in range(2)]
            hb_ = [sb(ph, f"hb_{i}", [128, SEG]) for i in range(2)]
            gg_ = [sb(ph, f"gg_{i}", [128, SEG]) for i in range(2)]
            ys_ = [sb(ph, f"ys_{i}", [128, SEG]) for i in range(2)]
            rgw_t = [sb(ph, f"rgw_t{i}", [128, 4, 128], F32R) for i in range(2)]
            rgb_t = sb(ph, "rgb_t", [128, 4, 8])
            kap = sb(ph, "kap", [128, 2, 8])
            kap2 = sb(ph, "kap2", [128, 2, 8])
            hrgb_t = sb(ph, "hrgb_t", [128, 4, 8])
            quarter_c = sb(ph, "quarter_c", [128, 1])
            cw_t = sb(ph, "cw_t", [128, 4, 8])
            cb_t = sb(ph, "cb_t", [128, 8])
            pb = [ph.enter_context(nc.psum_tensor(f"pbB{i}", [128, 512], F32)) for i in range(8)]
            R_pb = [Res(f"pbB{i}") for i in range(8)]
            R_xc2, R_hf = R2("xc"), Res("hf")
            R_raw, R_xcr, R_r, R_i, R_a2, R_hb, R_gg, R_ys, R_rgw = (R2("raw"), R2("xcr"), R2("r"), R2("i"), R2("a2"),
                                                                     R2("hb"), R2("gg"), R2("ys"), R2("rgw"))
            R_scrB = Res("scrB", acc=True)
            op("sync", lambda e: e.dma_start(out=rgb_t[:], in_=rgb), writes=RC, dma="cst")
            op("sync", lambda e: e.dma_start(out=kap[:], in_=lam), writes=RC, dma="cst")
            op("sync", lambda e: e.dma_start(out=cw_t[:], in_=convw), writes=RC, dma="cst")
            op("sync", lambda e: e.dma_start(out=cb_t[:], in_=convb), writes=RC, dma="cst")
            op("vector", lambda e: e.tensor_scalar(out=hrgb_t[:], in0=rgb_t[:], scalar1=0.5, scalar2=None, op0=ALU.mult),
               reads=RC, writes=RC)
            op("vector", lambda e: e.memset(quarter_c[:], 0.25), writes=RC)
            op("scalar", lambda e: e.activation(out=kap[:], in_=kap[:], func=AF.Exp, scale=-1.0), reads=RC, writes=RC)
            op("scalar", lambda e: e.activation(out=kap[:], in_=kap[:], func=AF.Ln, bias=one_c[:, 0:1], scale=1.0),
               reads=RC, writes=RC)
            op("vector", lambda e: e.tensor_scalar(out=kap2[:], in0=kap[:], scalar1=-4.0, scalar2=None, op0=ALU.mult),
               reads=RC, writes=RC)
            op("vector", lambda e: e.tensor_scalar(out=kap[:], in0=kap[:], scalar1=-8.0, scalar2=None, op0=ALU.mult),
               reads=RC, writes=RC)
            steps = []
            for c in range(8):
                for dirn in range(2):
                    for sgm in (range(NSEG) if dirn == 0 else range(NSEG - 1, -1, -1)):
                        steps.append((c, dirn, sgm))
            pbs = [0]

            def load_w(c):
                op("gpsimd", lambda e, c=c: e.dma_start(out=rgw_t[c % 2][:], in_=rgw[c]), writes=[R_rgw[c % 2]], dma=f"rgw{c % 2}")

            def stageX(n):
                c, dirn, sgm = steps[n]
                b2 = n % 2
                xc, R_xc = xc2[c % 2], R_xc2[c % 2]
                wt, Rwt = rgw_t[c % 2], R_rgw[c % 2]
                t0 = sgm * SEG
                ts_ = slice(t0, t0 + SEG)
                if dirn == 0 and sgm == 0 and c + 1 < 8:
                    load_w(c + 1)
                if dirn == 0:
                    rw = raw[b2]
                    lo = max(t0 - 2, 0)
                    hi = min(t0 + SEG + 1, S)
                    if sgm == 0:
                        op("vector", lambda e, rw=rw: e.memset(rw[:, 0:2], 0.0), writes=[R_raw[b2]])
                    if sgm == NSEG - 1:
                        op("vector", lambda e, rw=rw: e.memset(rw[:, SEG + 2:SEG + 3], 0.0), writes=[R_raw[b2]])
                    o0 = lo - (t0 - 2)
                    op("sync", lambda e, rw=rw, c=c, lo=lo, hi=hi, o0=o0: e.dma_start(
                        out=rw[:, o0:o0 + hi - lo], in_=XR[c][:, lo:hi]), writes=[R_raw[b2]], dma=f"raw{b2}")
                    ct = xcr[b2]
                    op("vector", lambda e, rw=rw, c=c, ct=ct: e.tensor_scalar(
                        out=ct[:], in0=rw[:, 0:SEG], scalar1=cw_t[:, 0, c:c + 1], scalar2=cb_t[:, c:c + 1],
                        op0=ALU.mult, op1=ALU.add), reads=[R_raw[b2]] + RC, writes=[R_xcr[b2]])
                    for tap in range(1, 4):
                        op("vector", lambda e, rw=rw, c=c, ts_=ts_, tap=tap, xc=xc, ct=ct: e.scalar_tensor_tensor(
                            out=(xc[:, ts_] if tap == 3 else ct[:]), in0=rw[:, tap:tap + SEG],
                            scalar=cw_t[:, tap, c:c + 1], in1=ct[:], op0=ALU.mult, op1=ALU.add),
                           reads=[R_raw[b2], R_xcr[b2]] + RC, writes=([R_xc] if tap == 3 else [R_xcr[b2]]))
                else:
                    op("sync", lambda e, b2=b2, c=c, ts_=ts_: e.dma_start(out=gg_[b2][:], in_=GG[c][:, ts_]),
                       writes=[R_gg[b2]], dma=f"gg{b2}")
                for j in range(SEG // 512):
                    js = slice(j * 512, (j + 1) * 512)
                    for gt, dstt, Rd in ((0, r_[b2], R_r[b2]), (1, i_[b2], R_i[b2])):
                        pbk = pbs[0] % 8
                        pbs[0] += 1
                        gi = dirn * 2 + gt
                        op("tensor", lambda e, pbk=pbk, wt=wt, gi=gi, xc=xc, j=j, t0=t0: e.matmul(
                            pb[pbk][:], lhsT=wt[:, gi, :], rhs=xc[:, t0 + j * 512:t0 + (j + 1) * 512], start=True, stop=True),
                           reads=[Rwt, R_xc], writes=[R_pb[pbk]])
                        op("scalar", lambda e, pbk=pbk, dstt=dstt, js=js, gi=gi, c=c: e.activation(
                            out=dstt[:, js], in_=pb[pbk][:], func=AF.Tanh, bias=hrgb_t[:, gi, c:c + 1], scale=0.5),
                           reads=[R_pb[pbk]] + RC, writes=[Rd])
                rr, aa = r_[b2], a2_[b2]
                op("scalar", lambda e, rr=rr, aa=aa, dirn=dirn, c=c: e.activation(
                    out=aa[:], in_=rr[:], func=AF.Exp, scale=kap[:, dirn, c:c + 1], bias=kap[:, dirn, c:c + 1]),
                   reads=[R_r[b2]] + RC, writes=[R_a2[b2]])
                op("scalar", lambda e, rr=rr, dirn=dirn, c=c: e.activation(
                    out=rr[:], in_=rr[:], func=AF.Exp, scale=kap2[:, dirn, c:c + 1], bias=kap2[:, dirn, c:c + 1]),
                   reads=RC, writes=[R_r[b2]])
                op("scalar", lambda e, aa=aa: e.activation(
                    out=aa[:], in_=aa[:], func=AF.Sqrt, bias=quarter_c[:, 0:1], scale=-0.25),
                   reads=RC, writes=[R_a2[b2]])

            def stageY(n):
                c, dirn, sgm = steps[n]
                b2 = n % 2
                xc, R_xc = xc2[c % 2], R_xc2[c % 2]
                t0 = sgm * SEG
                ts_ = slice(t0, t0 + SEG)
                rr, ii, aa = r_[b2], i_[b2], a2_[b2]
                op("vector", lambda e, aa=aa, ts_=ts_, xc=xc: e.tensor_tensor(
                    out=aa[:], in0=aa[:], in1=xc[:, ts_].bitcast(F32), op=ALU.mult),
                   reads=[R_xc], writes=[R_a2[b2]])
                op("vector", lambda e, ii=ii, aa=aa: e.scalar_tensor_tensor(
                    out=ii[:], in0=ii[:], scalar=1.0, in1=aa[:], op0=ALU.add, op1=ALU.mult),
                   reads=[R_a2[b2]], writes=[R_i[b2]])
                if dirn == 0:
                    init = 0.0 if sgm == 0 else hf[:, t0 - 1:t0]
                    op("vector", lambda e, rr=rr, ii=ii, ts_=ts_, init=init: e.tensor_tensor_scan(
                        out=hf[:, ts_], data0=rr[:], data1=ii[:], initial=init, op0=ALU.mult, op1=ALU.add),
                       reads=[R_r[b2], R_i[b2]], writes=[R_hf])
                else:
                    hb = hb_[b2]
                    if sgm == NSEG - 1:
                        init = 0.0
                        rd_extra = []
                    else:
                        init = hb_[1 - b2][:, 0:1]
                        rd_extra = [R_hb[1 - b2]]
                    op("vector", lambda e, rr=rr, ii=ii, hb=hb, init=init: e.tensor_tensor_scan(
                        out=hb[:, ::-1], data0=rr[:, ::-1], data1=ii[:, ::-1], initial=init,
                        op0=ALU.mult, op1=ALU.add),
                       reads=[R_r[b2], R_i[b2]] + rd_extra, writes=[R_hb[b2]])
                    gg = gg_[b2]
                    ys = ys_[b2]
                    op("vector", lambda e, ys=ys, hb=hb, ts_=ts_: e.tensor_tensor(
                        out=ys[:], in0=hb[:], in1=hf[:, ts_], op=ALU.add),
                       reads=[R_hb[b2], R_hf], writes=[R_ys[b2]])
                    op("vector", lambda e, ys=ys, gg=gg: e.tensor_tensor(out=ys[:], in0=ys[:], in1=gg[:], op=ALU.mult),
                       reads=[R_gg[b2]], writes=[R_ys[b2]])
                    op("sync", lambda e, ys=ys, c=c, ts_=ts_: e.dma_start(out=YT[c][:, ts_], in_=ys[:]),
                       reads=[R_ys[b2]], writes=[R_scrB], dma=f"ys{b2}")

            load_w(0)
            stageX(0)
            for n in range(len(steps)):
                if n + 1 < len(steps):
                    stageX(n + 1)
                stageY(n)
            P.barrier()
            P.emit(glob)
    _phase2()
    if phases <= 2:
        return finish(nc, P, glob, out)

    def _phase3():
        with ExitStack() as ph:
            Qd = [sb(ph, f"Qd{i}", [128, S], BF16) for i in range(2)]
            Kd = [sb(ph, f"Kd{i}", [128, S], BF16) for i in range(2)]
            Vd = sb(ph, "Vd", [128, S], BF16)
            Vt = sb(ph, "Vt", [128, 64, 128], BF16)
            NUM = sb(ph, "NUM", [128, S])
            DEN = sb(ph, "DEN", [128, S])
            pT = [sb(ph, f"pT{i}", [128, 512], BF16) for i in range(4)]
            ostg = [sb(ph, f"ostg{i}", [128, 1024]) for i in range(2)]
            ps_s = [ph.enter_context(nc.psum_tensor(f"ps_s{i}", [128, 512], F32)) for i in range(3)]
            ps_n = [ph.enter_context(nc.psum_tensor(f"ps_n{i}", [128, 512], F32)) for i in range(2)]
            ps_d = [ph.enter_context(nc.psum_tensor(f"ps_d{i}", [128, 512], F32)) for i in range(2)]
            ps_t = [ph.enter_context(nc.psum_tensor(f"ps_t{i}", [128, 512], BF16)) for i in range(1)]
            R_Qd, R_Kd = R2("Qd"), R2("Kd")
            R_Vd, R_Vt, R_NUM, R_DEN = Res("Vd"), Res("Vt"), Res("NUM"), Res("DEN")
            R_pT = [Res(f"pT{i}") for i in range(4)]
            R_ostg = R2("ostg")
            R_pss, R_psn, R_psd, R_pst = R2("pss"), R2("psn"), R2("psd"), R2("pst")
            R_pss4 = [Res(f"pss4{i}") for i in range(4)]
            R_scrC = Res("scrC", acc=True)
            ucnt = 0
            pcnt = 0
            for h in range(4):
                for g in range(3):
                    d = DIL[g]
                    L = S // d
                    u2 = ucnt % 2
                    ucnt += 1
                    qd, kd = Qd[u2], Kd[u2]
                    op("sync", lambda e, qd=qd, g=g, h=h: e.dma_start(out=qd[:], in_=QKV[g * 4 + h]),
                       writes=[R_Qd[u2]], dma=f"Qd{u2}")
                    op("sync", lambda e, kd=kd, g=g, h=h: e.dma_start(out=kd[:], in_=QKV[12 + g * 4 + h]),
                       writes=[R_Kd[u2]], dma=f"Kd{u2}")
                    op("sync", lambda e, g=g, h=h: e.dma_start(out=Vd[:], in_=QKV[24 + g * 4 + h]),
                       writes=[R_Vd], dma="Vd")
                    tviews = [(ps_t[0][:], R_pst[0])] + [(ps_s[i_][:].bitcast(BF16)[:, 0:512], R_pss4[i_]) for i_ in range(3)]
                    for m4 in range(16):
                        tv, Rtv = tviews[m4 % 4]
                        for q in range(4):
                            m = m4 * 4 + q
                            op("tensor", lambda e, tv=tv, q=q, m=m: e.transpose(
                                out=tv[:, q * 128:(q + 1) * 128], in_=Vd[:, m * 128:(m + 1) * 128], identity=ident_bf[:]),
                               reads=[R_Vd] + RC, writes=[Rtv])
                        if m4 % 2 == 0:
                            op("vector", lambda e, tv=tv, m4=m4: e.tensor_copy(
                                out=Vt[:, m4 * 4:(m4 + 1) * 4, :].rearrange("p a b -> p (a b)"), in_=tv),
                               reads=[Rtv], writes=[R_Vt])
                        else:
                            op("scalar", lambda e, tv=tv, m4=m4: e.activation(
                                out=Vt[:, m4 * 4:(m4 + 1) * 4, :].rearrange("p a b -> p (a b)"), in_=tv, func=AF.Copy),
                               reads=[Rtv], writes=[R_Vt])
                    contribs = []
                    for m in range(64):
                        p0 = m * 128
                        r = p0 // L
                        lo, hi = r * L, (r + 1) * L
                        if p0 == lo:
                            qs, var = p0, 1
                        elif p0 + 128 == hi:
                            qs, var = p0 - 128, 2
                        else:
                            qs, var = p0 - 64, 0
                        qe = qs + 256
                        parts = []
                        sb0 = (qs // 512) * 512
                        while sb0 < qe:
                            a, b = max(qs, sb0), min(qe, sb0 + 512)
                            parts.append((sb0 // 512, a, b))
                            sb0 += 512
                        contribs.append((qs, qe, var, parts))
                    last_of = {}
                    for m, (_, _, _, parts) in enumerate(contribs):
                        for (sbi, a, b) in parts:
                            last_of[sbi] = m
                    started = set()
                    LOOK = 2

                    def front(pi):
                        s3 = pi % 3
                        pt = pT[pi % 4]
                        Rp = R_pT[pi % 4]
                        for hf_ in range(2):
                            m = 2 * pi + hf_
                            qs, qe, var, parts = contribs[m]
                            op("tensor", lambda e, s3=s3, hf_=hf_, m=m, qs=qs, qe=qe, kd=kd, qd=qd: e.matmul(
                                ps_s[s3][:, hf_ * 256:(hf_ + 1) * 256], lhsT=kd[:, m * 128:(m + 1) * 128], rhs=qd[:, qs:qe],
                                start=True, stop=True),
                               reads=[R_Qd[u2], R_Kd[u2]], writes=[R_pss4[s3]])
                        v0, v1 = contribs[2 * pi][2], contribs[2 * pi + 1][2]
                        pm = pmask[{(0, 0): 0, (1, 0): 1, (0, 2): 2}[(v0, v1)]]
                        op("scalar", lambda e, s3=s3, pt=pt: e.activation(out=pt[:], in_=ps_s[s3][:], func=AF.Exp),
                           reads=[R_pss4[s3]], writes=[Rp])
                        op("vector", lambda e, pt=pt, pm=pm: e.tensor_tensor(out=pt[:], in0=pt[:], in1=pm[:], op=ALU.mult),
                           reads=RC, writes=[Rp])

                    def back(pi):
                        pt = pT[pi % 4]
                        Rp = R_pT[pi % 4]
                        for hf_ in range(2):
                            m = 2 * pi + hf_
                            qs, qe, var, parts = contribs[m]
                            for (sbi, a, b) in parts:
                                n2 = sbi % 2
                                first = sbi not in started
                                started.add(sbi)
                                c0, c1 = a - sbi * 512, b - sbi * 512
                                r0, r1 = hf_ * 256 + a - qs, hf_ * 256 + b - qs
                                op("tensor", lambda e, n2=n2, m=m, pt=pt, r0=r0, r1=r1, c0=c0, c1=c1, first=first: e.matmul(
                                    ps_n[n2][:, c0:c1], lhsT=Vt[:, m, :], rhs=pt[:, r0:r1], start=first, stop=False,
                                    skip_group_check=True),
                                   reads=[R_Vt, Rp], writes=[R_psn[n2]])
                                op("tensor", lambda e, n2=n2, pt=pt, r0=r0, r1=r1, c0=c0, c1=c1, first=first: e.matmul(
                                    ps_d[n2][:, c0:c1], lhsT=ones_bf[:], rhs=pt[:, r0:r1], start=first, stop=False,
                                    skip_group_check=True),
                                   reads=[Rp] + RC, writes=[R_psd[n2]])
                                if last_of[sbi] == m:
                                    P0 = sbi * 512
                                    r = P0 // L
                                    j0 = P0 - r * L
                                    nat = slice(r + d * j0, r + d * (j0 + 511) + 1, d) if d > 1 else slice(P0, P0 + 512)
                                    if g == 0:
                                        op("vector", lambda e, n2=n2, nat=nat: e.tensor_copy(out=NUM[:, nat], in_=ps_n[n2][:]),
                                           reads=[R_psn[n2]], writes=[R_NUM])
                                        op("scalar", lambda e, n2=n2, nat=nat: e.activation(out=DEN[:, nat], in_=ps_d[n2][:], func=AF.Copy),
                                           reads=[R_psd[n2]], writes=[R_DEN])
                                    else:
                                        op("vector", lambda e, n2=n2, nat=nat: e.tensor_tensor(
                                            out=NUM[:, nat], in0=ps_n[n2][:], in1=NUM[:, nat], op=ALU.add),
                                           reads=[R_psn[n2]], writes=[R_NUM])
                                        op("vector", lambda e, n2=n2, nat=nat: e.tensor_tensor(
                                            out=DEN[:, nat], in0=ps_d[n2][:], in1=DEN[:, nat], op=ALU.add),
                                           reads=[R_psd[n2]], writes=[R_DEN])

                    for mm in range(32 + LOOK):
                        if mm < 32:
                            front(mm)
                        if mm >= LOOK:
                            back(mm - LOOK)
                for pc in range(8):
                    o2 = pc % 2
                    cs = slice(pc * 1024, (pc + 1) * 1024)
                    op("vector", lambda e, cs=cs: e.reciprocal(out=DEN[:, cs], in_=DEN[:, cs]), writes=[R_DEN])
                    op("vector", lambda e, cs=cs, o2=o2: e.tensor_tensor(out=ostg[o2][:], in0=NUM[:, cs], in1=DEN[:, cs], op=ALU.mult),
                       reads=[R_NUM, R_DEN], writes=[R_ostg[o2]])
                    op("sync", lambda e, cs=cs, o2=o2, h=h: e.dma_start(out=OT[h][:, cs], in_=ostg[o2][:]),
                       reads=[R_ostg[o2]], writes=[R_scrC], dma=f"ostg{o2}")
            P.barrier()
            P.emit(glob)
    _phase3()
    if phases <= 3:
        return finish(nc, P, glob, out)

    TD = 256
    def _phase4():
        with ExitStack() as ph:
            prnn_t = sb(ph, "prnn_t", [128, 8, D], F32R)
            pattn_t = sb(ph, "pattn_t", [128, 4, D], F32R)
            wout_t = sb(ph, "wout_t", [128, 8, D], F32R)
            wr_t = sb(ph, "wr_t", [128, 8, 16])
            br_t = sb(ph, "br_t", [128, 16])
            gffn_t = sb(ph, "gffn_t", [128, D])
            yt = [sb(ph, f"yt{i}", [128, 8, TD], F32R) for i in range(2)]
            ot = [sb(ph, f"ot{i}", [128, 4, TD], F32R) for i in range(2)]
            sga = [sb(ph, f"sga{i}", [128, 8, TD]) for i in range(2)]
            sgb = [sb(ph, f"sgb{i}", [128, 8, TD]) for i in range(2)]
            mg = sb(ph, "mg", [128, 8, TD], F32R)
            mtmp = [sb(ph, f"mtmp{i}", [128, TD]) for i in range(2)]
            mtmp2 = [sb(ph, f"mtmpb{i}", [128, TD]) for i in range(2)]
            R_mtmp2 = R2("mtmp2")
            xt_ = [sb(ph, f"xt_{i}", [128, D]) for i in range(4)]
            x2_ = [sb(ph, f"x2_{i}", [128, D]) for i in range(2)]
            h2_ = [sb(ph, f"h2_{i}", [128, XW]) for i in range(3)]
            h2T = sb(ph, "h2T", [128, 8, 128])
            sm = sb(ph, "sm", [128, 8])
            lg = sb(ph, "lg", [128, 16])
            pb = [ph.enter_context(nc.psum_tensor(f"pbD{i}", [128, 512], F32)) for i in range(8)]
            R_pb = [Res(f"pbD{i}") for i in range(8)]
            R_wD = Res("wD")
            R_yt, R_ot, R_sga, R_sgb, R_mtmp, R_xt, R_x2, R_h2 = (R2("yt"), R2("ot"), R2("sga"), R2("sgb"), R2("mtmp"),
                                                                  R2("xt"), R2("x2"), R2("h2"))
            R_mg, R_h2T, R_sm, R_junk, R_lg = Res("mg"), Res("h2T"), Res("sm"), Res("junk"), Res("lg")
            R_xt = [Res(f"xt{i}") for i in range(4)]
            R_h2 = [Res(f"h2{i}") for i in range(3)]
            R_scrD = Res("scrD", acc=True)
            op("gpsimd", lambda e: e.dma_start(out=prnn_t[:], in_=prnn, max_dma_last_dim=4096), writes=[R_wD], dma="wD")
            op("gpsimd", lambda e: e.dma_start(out=pattn_t[:], in_=pattn, max_dma_last_dim=4096), writes=[R_wD], dma="wD")
            op("gpsimd", lambda e: e.dma_start(out=wout_t[:], in_=wout, max_dma_last_dim=4096), writes=[R_wD], dma="wD")
            op("sync", lambda e: e.dma_start(out=wr_t[:], in_=wr), writes=[R_wD], dma="wD")
            op("sync", lambda e: e.dma_start(out=br_t[:], in_=br), writes=[R_wD], dma="wD")
            op("sync", lambda e: e.dma_start(out=gffn_t[:], in_=gffn), writes=[R_wD], dma="wD")
            pbi = 0
            bcnt = 0
            YT_v = YT.rearrange("c p t -> p c t")
            OT_v = OT.rearrange("c p t -> p c t")
            SG_v = SG.rearrange("c p t -> p c t")
            def loadsD(ti):
                t0 = ti * TD
                b2 = ti % 2
                op("gpsimd", lambda e, b2=b2, t0=t0: e.dma_start(out=yt[b2][:], in_=YT_v[:, :, t0:t0 + TD]),
                   writes=[R_yt[b2]], dma=f"yt{b2}")
                op("gpsimd", lambda e, b2=b2, t0=t0: e.dma_start(out=ot[b2][:], in_=OT_v[:, :, t0:t0 + TD]),
                   writes=[R_ot[b2]], dma=f"ot{b2}")
                op("sync", lambda e, b2=b2, t0=t0: e.dma_start(out=sga[b2][:], in_=SG_v[:, 0:8, t0:t0 + TD]),
                   writes=[R_sga[b2]], dma=f"sga{b2}")
                op("sync", lambda e, b2=b2, t0=t0: e.dma_start(out=sgb[b2][:], in_=SG_v[:, 8:16, t0:t0 + TD]),
                   writes=[R_sgb[b2]], dma=f"sgb{b2}")
                for bb in range(TD // 128):
                    q4 = (ti % 2) * 2 + bb
                    tb = (ti * (TD // 128) + bb) * 128
                    op("sync", lambda e, q4=q4, tb=tb: e.dma_start(out=xt_[q4][:], in_=xtok[tb:tb + 128, :]),
                       writes=[R_xt[q4]], dma=f"xt{q4}")

            def router_q1(q2, blk, tb):
                nonlocal pbi
                pa = pbi % 8
                pbi += 1
                pa2 = pbi % 8
                pbi += 1
                for k in range(8):
                    pp = pa if k < 4 else pa2
                    op("tensor", lambda e, pp=pp, k=k, q2=q2: e.transpose(
                        out=pb[pp][:, (k % 4) * 128:(k % 4 + 1) * 128], in_=h2_[q2][:, k * 128:(k + 1) * 128], identity=ident[:]),
                       reads=[R_h2[q2]] + RC, writes=[R_pb[pp]])
                op("vector", lambda e, pa=pa: e.tensor_copy(out=h2T[:, 0:4, :].rearrange("p a b -> p (a b)"), in_=pb[pa][:]),
                   reads=[R_pb[pa]], writes=[R_h2T])
                op("scalar", lambda e, pa2=pa2: e.activation(out=h2T[:, 4:8, :].rearrange("p a b -> p (a b)"), in_=pb[pa2][:], func=AF.Copy),
                   reads=[R_pb[pa2]], writes=[R_h2T])

            def router_q2(q2, blk, tb):
                nonlocal pbi
                pa = pbi % 8
                pbi += 1
                for k in range(8):
                    op("tensor", lambda e, pa=pa, k=k: e.matmul(
                        pb[pa][:, 0:16], lhsT=h2T[:, k, :], rhs=wr_t[:, k, :], start=(k == 0), stop=(k == 7)),
                       reads=[R_h2T, R_wD], writes=[R_pb[pa]])
                op("vector", lambda e, pa=pa: e.tensor_tensor(out=lg[:], in0=pb[pa][:, 0:16], in1=br_t[:], op=ALU.add),
                   reads=[R_pb[pa], R_wD], writes=[R_lg])
                op("vector", lambda e: e.reduce_max(out=sm[:, 3:4], in_=lg[:], axis=AX.X), reads=[R_lg], writes=[R_sm])
                op("vector", lambda e: e.tensor_scalar(out=sm[:, 3:4], in0=sm[:, 3:4], scalar1=-1.0, scalar2=None, op0=ALU.mult),
                   writes=[R_sm])
                op("scalar", lambda e: e.activation(out=lg[:], in_=lg[:], func=AF.Exp, bias=sm[:, 3:4], scale=1.0,
                                                    accum_out=sm[:, 4:5]), reads=[R_sm], writes=[R_lg, R_sm])
                op("vector", lambda e: e.reciprocal(out=sm[:, 5:6], in_=sm[:, 4:5]), writes=[R_sm])
                op("vector", lambda e, blk=blk: e.tensor_scalar(
                    out=AFFt[:, blk, :], in0=lg[:], scalar1=sm[:, 5:6], scalar2=None, op0=ALU.mult),
                   reads=[R_lg, R_sm], writes=[R_aff])
                op("gpsimd", lambda e, q2=q2, blk=blk: e.tensor_copy(out=h2_[q2][:, D:D + 16], in_=AFFt[:, blk, :]),
                   reads=[R_aff], writes=[R_h2[q2]])
                op("gpsimd", lambda e, q2=q2, blk=blk: e.tensor_copy(out=h2_[q2][:, D + 16:D + 24],
                                                                      in_=cst_t[:, 640 + blk:641 + blk].to_broadcast([128, 8])),
                   reads=RC, writes=[R_h2[q2]])
                op("sync", lambda e, q2=q2, tb=tb: e.dma_start(out=H2X[tb:tb + 128, :], in_=h2_[q2][:]),
                   reads=[R_h2[q2]], writes=[R_scrD], dma=f"h2{q2}")

            pendingD = []
            loadsD(0)
            for ti in range(S // TD):
                t0 = ti * TD
                b2 = ti % 2
                if ti + 1 < S // TD:
                    loadsD(ti + 1)
                for dc in range(8):
                    pa = pbi % 8
                    pbi += 1
                    for k in range(8):
                        op("tensor", lambda e, pa=pa, k=k, dc=dc, b2=b2: e.matmul(
                            pb[pa][:, 0:TD], lhsT=prnn_t[:, k, dc * 128:(dc + 1) * 128], rhs=yt[b2][:, k, :],
                            start=(k == 0), stop=(k == 7)), reads=[R_wD, R_yt[b2]], writes=[R_pb[pa]])
                    for k in range(4):
                        op("tensor", lambda e, pa=pa, k=k, dc=dc, b2=b2: e.matmul(
                            pb[pa][:, TD:2 * TD], lhsT=pattn_t[:, k, dc * 128:(dc + 1) * 128], rhs=ot[b2][:, k, :],
                            start=(k == 0), stop=(k == 3)), reads=[R_wD, R_ot[b2]], writes=[R_pb[pa]])
                    m2 = dc % 2
                    op("vector", lambda e, pa=pa, dc=dc, b2=b2, m2=m2: e.tensor_tensor(
                        out=mtmp[m2][:], in0=pb[pa][:, 0:TD], in1=sga[b2][:, dc, :], op=ALU.mult),
                       reads=[R_pb[pa], R_sga[b2]], writes=[R_mtmp[m2]])
                    op("vector", lambda e, pa=pa, dc=dc, b2=b2, m2=m2: e.tensor_tensor(
                        out=mtmp2[m2][:], in0=pb[pa][:, TD:2 * TD], in1=sgb[b2][:, dc, :], op=ALU.mult),
                       reads=[R_pb[pa], R_sgb[b2]], writes=[R_mtmp2[m2]])
                    op("vector", lambda e, dc=dc, m2=m2: e.tensor_tensor(
                        out=mg[:, dc, :], in0=mtmp2[m2][:], in1=mtmp[m2][:], op=ALU.add),
                       reads=[R_mtmp[m2], R_mtmp2[m2]], writes=[R_mg])
                for bb in range(TD // 128):
                    blk = ti * (TD // 128) + bb
                    tb = blk * 128
                    q2 = bcnt % 2
                    q3 = bcnt % 3
                    bcnt += 1
                    q4 = (ti % 2) * 2 + bb
                    if len(pendingD) == 2:
                        router_q1(*pendingD[0])
                    pas = []
                    for hh in range(2):
                        pa = pbi % 8
                        pbi += 1
                        pas.append(pa)
                        for k in range(8):
                            op("tensor", lambda e, pa=pa, k=k, bb=bb, hh=hh: e.matmul(
                                pb[pa][:], lhsT=mg[:, k, bb * 128:(bb + 1) * 128], rhs=wout_t[:, k, hh * 512:(hh + 1) * 512],
                                start=(k == 0), stop=(k == 7)), reads=[R_mg, R_wD], writes=[R_pb[pa]])
                    if len(pendingD) == 2:
                        router_q2(*pendingD.pop(0))
                    for hh in range(2):
                        pa = pas[hh]
                        op("vector", lambda e, pa=pa, q2=q2, hh=hh, q4=q4: e.tensor_tensor(
                            out=x2_[q2][:, hh * 512:(hh + 1) * 512], in0=pb[pa][:], in1=xt_[q4][:, hh * 512:(hh + 1) * 512],
                            op=ALU.add), reads=[R_pb[pa], R_xt[q4]], writes=[R_x2[q2]])
                    op("sync", lambda e, q2=q2, tb=tb: e.dma_start(out=X2[tb:tb + 128, :], in_=x2_[q2][:]),
                       reads=[R_x2[q2]], writes=[R_scrD], dma=f"x2{q2}")
                    op("scalar", lambda e, q2=q2, q3=q3: e.activation(out=h2_[q3][:, 0:D], in_=x2_[q2][:], func=AF.Square, accum_out=sm[:, 0:1]),
                       reads=[R_x2[q2]], writes=[R_h2[q3], R_sm])
                    op("scalar", lambda e: e.activation(out=sm[:, 1:2], in_=sm[:, 0:1], func=AF.Sqrt, bias=eps_c[:, 0:1], scale=1.0 / D),
                       reads=RC, writes=[R_sm])
                    op("vector", lambda e: e.reciprocal(out=sm[:, 2:3], in_=sm[:, 1:2]), writes=[R_sm])
                    op("vector", lambda e, q2=q2, q3=q3: e.scalar_tensor_tensor(
                        out=h2_[q3][:, 0:D], in0=x2_[q2][:], scalar=sm[:, 2:3], in1=gffn_t[:], op0=ALU.mult, op1=ALU.mult),
                       reads=[R_x2[q2], R_sm, R_wD], writes=[R_h2[q3]])
                    pendingD.append((q3, blk, tb))
            while pendingD:
                router_q1(*pendingD[0])
                router_q2(*pendingD.pop(0))
            P.barrier()
            P.emit(glob)
    _phase4()
    if phases <= 4:
        return finish(nc, P, glob, out)

    def _phase5():
        with ExitStack() as ph:
            lo_t = sb(ph, "lo_t", [128, 16])
            hi_t = sb(ph, "hi_t", [128, 16])
            mid_t = sb(ph, "mid_t", [128, 16])
            cnt_t = sb(ph, "cnt_t", [128, 16])
            flg_t = sb(ph, "flg_t", [128, 16])
            t1_t = sb(ph, "t1_t", [128, 16])
            cmp_bf = sb(ph, "cmp_bf", [128, 1024], BF16)
            pos_f = sb(ph, "pos_f", [128, 1024])
            tot_f = sb(ph, "tot_f", [128, 1024])
            cum_f = sb(ph, "cum_f", [128, 1024])
            zero_f = sb(ph, "zero_f", [128, 64])
            off_i = sb(ph, "off_i", [128, 1024], I32)
            hrow = [sb(ph, f"hrow{i}", [128, XW]) for i in range(3)]
            xgr = [sb(ph, f"xgr{i}", [128, XW]) for i in range(2)]
            xgT = [sb(ph, f"xgT{i}", [128, 8, 1024], BF16) for i in range(2)]
            gate_t = [sb(ph, f"gate_t{i}", [128, 8]) for i in range(2)]
            tok_i = [sb(ph, f"tok_i{i}", [128, 8], I32) for i in range(2)]
            hidT = sb(ph, "hidT", [128, 16, 1024], BF16)
            wg_t = [sb(ph, f"wg_t{i}", [128, 8, 128], BF16) for i in range(3)]
            wu_t = [sb(ph, f"wu_t{i}", [128, 8, 128], BF16) for i in range(3)]
            wd_t = [sb(ph, f"wd_t{i}", [128, 16, 512], BF16) for i in range(2)]
            sg_t = [sb(ph, f"sg_t{i}", [128, 512]) for i in range(2)]
            eo_t = sb(ph, "eo_t", [128, 8, D])
            pb = [ph.enter_context(nc.psum_tensor(f"pbE{i}", [128, 512], F32)) for i in range(8)]
            R_pb = [Res(f"pbE{i}") for i in range(8)]
            R_bis, R_cmp, R_pos, R_off = Res("bis"), Res("cmp"), Res("pos"), Res("off")
            R_hrow = [Res(f"hrow{i}") for i in range(3)]
            R_xgr, R_xgT, R_gate = R2("xgr"), R2("xgT"), R2("gate")
            R_hid, R_eo = Res("hid"), Res("eo")
            R_wg = [Res(f"wg{i}") for i in range(3)]
            R_wu = [Res(f"wu{i}") for i in range(3)]
            R_wd, R_sg = R2("wd"), R2("sg")
            R_XG = [Res(f"XG{e}", acc=True) for e in range(16)]
            R_x2acc = Res("x2acc")
            aff_flat = AFFt[:].rearrange("p b e -> p (b e)")
            bc = lambda t: t[:].unsqueeze(1).to_broadcast([128, 64, 16])
            aff3 = AFFt[:]
            cmp3 = cmp_bf[:].rearrange("p (b e) -> p b e", e=16)
            op("vector", lambda e: e.memset(lo_t[:], 0.0), writes=[R_bis])
            op("vector", lambda e: e.memset(hi_t[:], 1.0), writes=[R_bis])
            op("vector", lambda e: e.memset(zero_f[:], 0.0), writes=[R_bis])
            for it in range(32):
                op("vector", lambda e: e.tensor_tensor(out=mid_t[:], in0=lo_t[:], in1=hi_t[:], op=ALU.add), writes=[R_bis])
                op("vector", lambda e: e.tensor_scalar(out=mid_t[:], in0=mid_t[:], scalar1=0.5, scalar2=None, op0=ALU.mult),
                   writes=[R_bis])
                op("vector", lambda e: e.tensor_tensor(out=cmp3, in0=aff3, in1=bc(mid_t), op=ALU.is_gt),
                   reads=[R_aff, R_bis], writes=[R_cmp])
                pa, pa2 = (2 * it) % 8, (2 * it + 1) % 8
                op("tensor", lambda e, pa=pa: e.matmul(pb[pa][:], lhsT=ones_bf[:], rhs=cmp_bf[:, 0:512], start=True, stop=True),
                   reads=[R_cmp] + RC, writes=[R_pb[pa]])
                op("tensor", lambda e, pa2=pa2: e.matmul(pb[pa2][:], lhsT=ones_bf[:], rhs=cmp_bf[:, 512:1024], start=True, stop=True),
                   reads=[R_cmp] + RC, writes=[R_pb[pa2]])
                op("vector", lambda e, pa=pa: e.tensor_reduce(
                    out=cnt_t[:], in_=pb[pa][:].rearrange("p (b e) -> p e b", e=16), axis=AX.X, op=ALU.add),
                   reads=[R_pb[pa]], writes=[R_bis])
                op("vector", lambda e, pa2=pa2: e.tensor_reduce(
                    out=t1_t[:], in_=pb[pa2][:].rearrange("p (b e) -> p e b", e=16), axis=AX.X, op=ALU.add),
                   reads=[R_pb[pa2]], writes=[R_bis])
                op("vector", lambda e: e.tensor_tensor(out=cnt_t[:], in0=cnt_t[:], in1=t1_t[:], op=ALU.add), writes=[R_bis])
                op("vector", lambda e: e.tensor_scalar(out=flg_t[:], in0=cnt_t[:], scalar1=1023.5, scalar2=None, op0=ALU.is_ge),
                   writes=[R_bis])
                op("vector", lambda e: e.tensor_tensor(out=t1_t[:], in0=flg_t[:], in1=mid_t[:], op=ALU.mult), writes=[R_bis])
                op("vector", lambda e: e.tensor_tensor(out=lo_t[:], in0=lo_t[:], in1=t1_t[:], op=ALU.max), writes=[R_bis])
                op("vector", lambda e: e.scalar_tensor_tensor(out=t1_t[:], in0=flg_t[:], scalar=2.0, in1=mid_t[:],
                                                              op0=ALU.mult, op1=ALU.add), writes=[R_bis])
                op("vector", lambda e: e.tensor_tensor(out=hi_t[:], in0=hi_t[:], in1=t1_t[:], op=ALU.min), writes=[R_bis])
            op("vector", lambda e: e.tensor_tensor(out=cmp3, in0=aff3, in1=bc(lo_t), op=ALU.is_gt),
               reads=[R_aff, R_bis], writes=[R_cmp])
            for hh in range(2):
                cs = slice(hh * 512, (hh + 1) * 512)
                op("tensor", lambda e, hh=hh, cs=cs: e.matmul(pb[hh][:], lhsT=ut_bf[:], rhs=cmp_bf[:, cs], start=True, stop=True),
                   reads=[R_cmp] + RC, writes=[R_pb[hh]])
                op("tensor", lambda e, hh=hh, cs=cs: e.matmul(pb[2 + hh][:], lhsT=ones_bf[:], rhs=cmp_bf[:, cs], start=True, stop=True),
                   reads=[R_cmp] + RC, writes=[R_pb[2 + hh]])
                op("vector", lambda e, hh=hh, cs=cs: e.tensor_copy(out=pos_f[:, cs], in_=pb[hh][:]), reads=[R_pb[hh]], writes=[R_pos])
                op("vector", lambda e, hh=hh, cs=cs: e.tensor_copy(out=tot_f[:, cs], in_=pb[2 + hh][:]), reads=[R_pb[2 + hh]], writes=[R_pos])
            tot3 = tot_f[:].rearrange("p (b e) -> p e b", e=16)
            cum3 = cum_f[:].rearrange("p (b e) -> p e b", e=16)
            for ee in range(16):
                op("vector", lambda e, ee=ee: e.tensor_tensor_scan(
                    out=cum3[:, ee, :], data0=tot3[:, ee, :], data1=zero_f[:], initial=0.0, op0=ALU.add, op1=ALU.add),
                   reads=[R_bis], writes=[R_pos])
            op("vector", lambda e: e.tensor_tensor(out=pos_f[:], in0=pos_f[:], in1=cum_f[:], op=ALU.add), writes=[R_pos])
            op("vector", lambda e: e.tensor_tensor(out=pos_f[:], in0=pos_f[:], in1=tot_f[:], op=ALU.subtract), writes=[R_pos])
            op("vector", lambda e: e.tensor_scalar(out=tot_f[:], in0=cmp_bf[:], scalar1=-1048576.0, scalar2=1048576.0,
                                                   op0=ALU.mult, op1=ALU.add), reads=[R_cmp], writes=[R_pos])
            op("vector", lambda e: e.tensor_tensor(out=pos_f[:], in0=pos_f[:], in1=tot_f[:], op=ALU.add), writes=[R_pos])
            op("vector", lambda e: e.tensor_copy(out=off_i[:], in_=pos_f[:]), reads=[R_pos], writes=[R_off])
            dcnt = [0]

            def seal_group(gn):
                for ee in range(4 * gn, 4 * gn + 4):
                    for h in range(3):
                        k = ("dma", f"sc{gn}_{h}")
                        if k in P.dma_vals:
                            R_XG[ee].w[k] = P.dma_vals[k]

            def dispatch_block(b, experts):
                gn_ = experts[0] // 4
                h3 = dcnt[0] % 3
                dcnt[0] += 1
                op("sync", lambda e, h3=h3, b=b: e.dma_start(out=hrow[h3][:], in_=H2X[b * 128:(b + 1) * 128, :]),
                   writes=[R_hrow[h3]], dma=f"hrow{h3}")
                for ee in experts:
                    col = b * 16 + ee
                    op("gpsimd", lambda e, h3=h3, ee=ee, col=col: e.indirect_dma_start(
                        out=XG[ee], out_offset=bass.IndirectOffsetOnAxis(ap=off_i[:, col:col + 1], axis=0),
                        in_=hrow[h3][:], in_offset=None, bounds_check=bc_reg(e, 1023), oob_is_err=False),
                       reads=[R_hrow[h3], R_off], writes=[R_XG[ee]], dma=f"sc{gn_}_{h3}")

            for b in range(64):
                dispatch_block(b, range(0, 4))
            seal_group(0)
            pbi = 0
            wci = 0
            sgi = 0

            def gather_expert(ee, sbks=range(8)):
                nonlocal pbi
                x2 = ee % 2
                for sbk in sbks:
                    g2 = (ee * 8 + sbk) % 2
                    op("sync", lambda e, g2=g2, ee=ee, sbk=sbk: e.dma_start(out=xgr[g2][:], in_=XG[ee][sbk * 128:(sbk + 1) * 128, :]),
                       reads=[R_XG[ee]], writes=[R_xgr[g2]], dma=f"xgr{g2}")
                    for half in range(2):
                        pa = pbi % 8
                        pbi += 1
                        for q in range(4):
                            k = half * 4 + q
                            op("tensor", lambda e, pa=pa, q=q, k=k, g2=g2: e.transpose(
                                out=pb[pa][:, q * 128:(q + 1) * 128], in_=xgr[g2][:, k * 128:(k + 1) * 128], identity=ident[:]),
                               reads=[R_xgr[g2]] + RC, writes=[R_pb[pa]])
                        dst = xgT[x2][:, half * 4:(half + 1) * 4, sbk * 128:(sbk + 1) * 128]
                        src = pb[pa][:].rearrange("p (a b) -> p a b", a=4)
                        if half == 0:
                            op("vector", lambda e, dst=dst, src=src: e.tensor_copy(out=dst, in_=src),
                               reads=[R_pb[pa]], writes=[R_xgT[x2]])
                        else:
                            op("scalar", lambda e, dst=dst, src=src: e.activation(out=dst, in_=src, func=AF.Copy),
                               reads=[R_pb[pa]], writes=[R_xgT[x2]])
                    op("gpsimd", lambda e, g2=g2, x2=x2, sbk=sbk, ee=ee: e.tensor_copy(
                        out=gate_t[x2][:, sbk:sbk + 1], in_=xgr[g2][:, D + ee:D + ee + 1]),
                       reads=[R_xgr[g2]], writes=[R_gate[x2]])
                    op("gpsimd", lambda e, g2=g2, x2=x2, sbk=sbk: e.tensor_copy(
                        out=tok_i[x2][:, sbk:sbk + 1], in_=xgr[g2][:, D + 16:D + 17]),
                       reads=[R_xgr[g2]], writes=[R_gate[x2]])

            R_accbar = [Res(f"accbar{e}", acc=True) for e in range(16)]

            def acc_scatter(ex, cb):
                x2_ = ex % 2
                rd = [R_eo, R_gate[x2_]] + ([R_accbar[ex - 1]] if ex > 0 else [])
                op("gpsimd", lambda e, cb=cb, x2_=x2_: e.indirect_dma_start(
                    out=X2, out_offset=bass.IndirectOffsetOnAxis(ap=tok_i[x2_][:, cb:cb + 1], axis=0),
                    in_=eo_t[:, cb, :], in_offset=None, bounds_check=bc_reg(e, S - 1), oob_is_err=True, compute_op=ALU.add),
                   reads=rd, writes=[R_accbar[ex]], dma="acc")

            def load_wd(ex, dh):
                op("gpsimd", lambda e, dh=dh, ex=ex: e.dma_start(out=wd_t[dh][:], in_=wd[ex, dh], max_dma_last_dim=4096),
                   writes=[R_wd[dh]], dma=f"wd{dh}")

            gather_expert(0)
            for ee in range(16):
                x2 = ee % 2
                for f in range(16):
                    w3 = wci % 3
                    wci += 1
                    op("gpsimd", lambda e, w3=w3, ee=ee, f=f: e.dma_start(out=wg_t[w3][:], in_=wg[ee, f]),
                       writes=[R_wg[w3]], dma=f"wg{w3}")
                    op("gpsimd", lambda e, w3=w3, ee=ee, f=f: e.dma_start(out=wu_t[w3][:], in_=wu[ee, f]),
                       writes=[R_wu[w3]], dma=f"wu{w3}")
                    if ee > 0 and 4 <= f < 12:
                        acc_scatter(ee - 1, f - 4)
                    if f == 2:
                        load_wd(ee, 1)
                        if ee == 0:
                            load_wd(0, 0)
                    if ee // 4 + 1 < 4:
                        gn = ee // 4 + 1
                        dispatch_block((ee % 4) * 16 + f, range(4 * gn, 4 * gn + 4))
                        if ee % 4 == 3 and f == 15:
                            seal_group(gn)
                    for half in range(2):
                        hs = slice(half * 512, (half + 1) * 512)
                        pg = pbi % 8
                        pbi += 1
                        pu = pbi % 8
                        pbi += 1
                        for k in range(8):
                            op("tensor", lambda e, pg=pg, k=k, w3=w3, x2=x2, hs=hs: e.matmul(
                                pb[pg][:], lhsT=wg_t[w3][:, k, :], rhs=xgT[x2][:, k, hs], start=(k == 0), stop=(k == 7)),
                               reads=[R_wg[w3], R_xgT[x2]], writes=[R_pb[pg]])
                        for k in range(8):
                            op("tensor", lambda e, pu=pu, k=k, w3=w3, x2=x2, hs=hs: e.matmul(
                                pb[pu][:], lhsT=wu_t[w3][:, k, :], rhs=xgT[x2][:, k, hs], start=(k == 0), stop=(k == 7)),
                               reads=[R_wu[w3], R_xgT[x2]], writes=[R_pb[pu]])
                        s2 = sgi % 2
                        sgi += 1
                        op("scalar", lambda e, pg=pg, s2=s2: e.activation(out=sg_t[s2][:], in_=pb[pg][:], func=AF.Silu),
                           reads=[R_pb[pg]], writes=[R_sg[s2]])
                        op("vector", lambda e, pu=pu, s2=s2, f=f, hs=hs: e.tensor_tensor(
                            out=hidT[:, f, hs], in0=pb[pu][:], in1=sg_t[s2][:], op=ALU.mult),
                           reads=[R_pb[pu], R_sg[s2]], writes=[R_hid])
                gidx = 0
                for dh in range(2):
                    if dh == 1 and ee + 1 < 16:
                        load_wd(ee + 1, 0)
                    for cb in range(8):
                        if ee + 1 < 16 and gidx % 2 == 0:
                            gather_expert(ee + 1, [gidx // 2])
                        gidx += 1
                        pa = pbi % 8
                        pbi += 1
                        for f in range(16):
                            op("tensor", lambda e, pa=pa, f=f, cb=cb, dh=dh: e.matmul(
                                pb[pa][:], lhsT=hidT[:, f, cb * 128:(cb + 1) * 128], rhs=wd_t[dh][:, f, :],
                                start=(f == 0), stop=(f == 15)), reads=[R_hid, R_wd[dh]], writes=[R_pb[pa]])
                        if cb % 2 == 0:
                            op("vector", lambda e, pa=pa, cb=cb, dh=dh, x2=x2: e.tensor_scalar(
                                out=eo_t[:, cb, dh * 512:(dh + 1) * 512], in0=pb[pa][:], scalar1=gate_t[x2][:, cb:cb + 1],
                                scalar2=None, op0=ALU.mult), reads=[R_pb[pa], R_gate[x2]], writes=[R_eo])
                        else:
                            op("scalar", lambda e, pa=pa, cb=cb, dh=dh, x2=x2: e.activation(
                                out=eo_t[:, cb, dh * 512:(dh + 1) * 512], in_=pb[pa][:], func=AF.Copy,
                                scale=gate_t[x2][:, cb:cb + 1]), reads=[R_pb[pa], R_gate[x2]], writes=[R_eo])
                if ee == 15:
                    for cb in range(8):
                        acc_scatter(15, cb)
            P.barrier()
            P.emit(glob)
    _phase5()
    if phases <= 5:
        return finish(nc, P, glob, out)

    def _phase6():
        with ExitStack() as ph:
            gfin_t = sb(ph, "gfin_t", [128, D])
            xa = [sb(ph, f"xa{i}", [128, D]) for i in range(3)]
            ya = [sb(ph, f"ya{i}", [128, D]) for i in range(3)]
            junk = sb(ph, "junkF", [128, D])
            sm = sb(ph, "smF", [128, 4])
            R_xa = [Res(f"xa{i}") for i in range(3)]
            R_ya = [Res(f"ya{i}") for i in range(3)]
            R_junk, R_sm, R_g = Res("junkF"), Res("smF"), Res("gfin")
            R_out = Res("out", acc=True)
            op("sync", lambda e: e.dma_start(out=gfin_t[:], in_=gfin), writes=[R_g], dma="gfin")
            def loadF(b):
                op("sync", lambda e, b=b: e.dma_start(out=xa[b % 3][:], in_=X2[b * 128:(b + 1) * 128, :]),
                   writes=[R_xa[b % 3]], dma=f"xa{b % 3}")

            loadF(0)
            loadF(1)
            for b in range(64):
                b3 = b % 3
                if b + 2 < 64:
                    loadF(b + 2)
                op("scalar", lambda e, b3=b3: e.activation(out=junk[:], in_=xa[b3][:], func=AF.Square, accum_out=sm[:, 0:1]),
                   reads=[R_xa[b3]], writes=[R_junk, R_sm])
                op("scalar", lambda e: e.activation(out=sm[:, 1:2], in_=sm[:, 0:1], func=AF.Sqrt, bias=eps_c[:, 0:1], scale=1.0 / D),
                   reads=RC, writes=[R_sm])
                op("vector", lambda e: e.reciprocal(out=sm[:, 2:3], in_=sm[:, 1:2]), writes=[R_sm])
                op("vector", lambda e, b3=b3: e.scalar_tensor_tensor(
                    out=ya[b3][:], in0=xa[b3][:], scalar=sm[:, 2:3], in1=gfin_t[:], op0=ALU.mult, op1=ALU.mult),
                   reads=[R_xa[b3], R_sm, R_g], writes=[R_ya[b3]])
                op("sync", lambda e, b3=b3, b=b: e.dma_start(out=out[b * 128:(b + 1) * 128, :], in_=ya[b3][:]),
                   reads=[R_ya[b3]], writes=[R_out], dma=f"ya{b3}")
            P.barrier()
            P.emit(glob)
    _phase6()
    return finish(nc, P, glob, out)


def finish(nc, P, glob, out):
    P.barrier()
    P.emit(glob)
    glob.close()
    return nc


def _const_tables():
    ident = np.eye(128, dtype=np.float32)
    ut = np.triu(np.ones((128, 128), np.float32), 1)
    i = np.arange(128)[:, None]
    c = np.arange(256)[None, :]
    c = np.arange(384)[None, :]
    mask = ((c >= i + 64) & (c <= i + 192)).astype(np.float32)
    tokb = (np.arange(64)[None, :] * 128 + np.arange(128)[:, None]).astype(np.float32)
    cst = np.concatenate([ident, ut, mask, tokb], axis=1)
    pos = np.arange(S, dtype=np.float32)
    inv = (np.float32(500000.0) ** (-np.arange(0, 32, 2, dtype=np.float32) / np.float32(32))).astype(np.float32)
    ang = (pos[None, :] * inv[:, None]).astype(np.float32)
    cos, sin = np.cos(ang).astype(np.float32), np.sin(ang).astype(np.float32)
    one = np.ones((16, S), np.float32)
    zero = np.zeros((16, S), np.float32)
    cosT = np.concatenate([cos, one, cos, one], axis=0)
    sinT = np.concatenate([sin, zero, -sin, zero], axis=0)
    return cst, np.ascontiguousarray(cosT), np.ascontiguousarray(sinT)


HEAD_PERM = np.array(list(range(16)) + list(range(32, 48)) + list(range(16, 32)) + list(range(48, 128)))


def prep_shared(inp):
    f = lambda a: np.ascontiguousarray(np.asarray(a, dtype=np.float32))
    w_in = f(inp["w_in"])[0]
    b_in = f(inp["b_in"])[0]
    colperm = np.arange(8704)
    for c in range(16, 40):
        colperm[c * 128:(c + 1) * 128] = c * 128 + HEAD_PERM
    w_in = w_in[:, colperm]
    b_in = b_in[colperm]
    cst, cosT, sinT = _const_tables()
    sh = {
        "w_in": f(w_in.reshape(8, 128, NCH, 128).transpose(2, 1, 0, 3)),
        "b_in": f(b_in.reshape(NCH, 128).T),
        "gmix": f(inp["norm_mix"][0].reshape(8, 128).T),
        "convw": f(np.asarray(inp["conv_w"])[0].reshape(4, 8, 128).transpose(2, 0, 1)),
        "convb": f(np.asarray(inp["conv_b"])[0].reshape(8, 128).T),
        "rgw": f(np.asarray(inp["rg_w"])[0].reshape(4, 8, 128, 128).transpose(1, 2, 0, 3)),
        "rgb": f(np.asarray(inp["rg_b"])[0].reshape(4, 8, 128).transpose(2, 0, 1)),
        "lam": f(np.asarray(inp["rg_lambda"])[0].reshape(2, 8, 128).transpose(2, 0, 1)),
        "prnn": f(np.asarray(inp["p_rnn"])[0].reshape(8, 128, D).transpose(1, 0, 2)),
        "pattn": f(np.asarray(inp["p_attn"])[0].reshape(4, 128, D).transpose(1, 0, 2)),
        "wout": f(np.asarray(inp["w_out"])[0].reshape(8, 128, D).transpose(1, 0, 2)),
        "gffn": f(np.broadcast_to(np.asarray(inp["norm_ffn"])[0][None, :], (128, D))),
        "gfin": f(np.broadcast_to(np.asarray(inp["norm_final"])[None, :], (128, D))),
        "wr": f(np.asarray(inp["w_router"])[0].reshape(8, 128, 16).transpose(1, 0, 2)),
        "br": f(np.broadcast_to(np.asarray(inp["b_router"])[0][None, :], (128, 16))),
        "wg": f(np.asarray(inp["w_gate"])[0].reshape(16, 8, 128, 16, 128).transpose(0, 3, 2, 1, 4)),
        "wu": f(np.asarray(inp["w_up"])[0].reshape(16, 8, 128, 16, 128).transpose(0, 3, 2, 1, 4)),
        "wd": f(np.asarray(inp["w_down"])[0].reshape(16, 16, 128, 2, 512).transpose(0, 3, 2, 1, 4)),
        "cosT": cosT, "sinT": sinT, "cst": cst,
    }
    return sh


_NC_CACHE = {}


def kernel(**inputs):
    x = np.asarray(inputs["x"], dtype=np.float32)
    B = x.shape[0]
    sh = prep_shared(inputs)
    if "nc" not in _NC_CACHE:
        _NC_CACHE["nc"] = build()
    nc = _NC_CACHE["nc"]
    in_maps = []
    for b in range(B):
        m = dict(sh)
        m["xtok"] = np.ascontiguousarray(x[b])
        m["xT"] = np.ascontiguousarray(x[b].T)
        in_maps.append(m)
    res = run_bass_kernel_spmd(nc, in_maps, core_ids=list(range(B)))
    return np.stack([np.asarray(r["out"], dtype=np.float32) for r in res.results], axis=0)
```

```python
import numpy as np
from contextlib import ExitStack
import concourse.bass as bass
import concourse.mybir as mybir
from concourse.bass_utils import run_bass_kernel_spmd

F32 = mybir.dt.float32
F32R = mybir.dt.float32r
BF16 = mybir.dt.bfloat16
I32 = mybir.dt.int32
AF = mybir.ActivationFunctionType
ALU = mybir.AluOpType
AX = mybir.AxisListType

ENGS = ("sync", "scalar", "vector", "gpsimd", "tensor")
EPOCH = 30000
S = 8192
D = 1024
NCH = 68
XW = 1048


class Res:
    __slots__ = ("name", "w", "r", "acc")

    def __init__(self, name, acc=False):
        self.name = name
        self.w = {}
        self.r = {}
        self.acc = acc


class Prog:
    def __init__(self, nc, same_engine_sync=("scalar", "vector", "gpsimd")):
        self.nc = nc
        self.ops = {e: [] for e in ENGS}
        self.cnt = {e: 0 for e in ENGS}
        self.seen = {e: {} for e in ENGS}
        self.dma_vals = {}
        self.same = set(same_engine_sync)
        self.sem_keys = []
        self.final = {}

    def _key(self, k):
        if k not in self.final:
            self.sem_keys.append(k)
        return k

    def op(self, eng, fn, reads=(), writes=(), dma=None):
        need = {}
        for r in reads:
            for k, v in r.w.items():
                if need.get(k, 0) < v:
                    need[k] = v
        for w in writes:
            if not w.acc:
                for k, v in w.w.items():
                    if need.get(k, 0) < v:
                        need[k] = v
            for k, v in w.r.items():
                if need.get(k, 0) < v:
                    need[k] = v
        waits = []
        seen = self.seen[eng]
        for k, v in need.items():
            if seen.get(k, 0) >= v:
                continue
            if k[0] == eng and eng not in self.same:
                continue
            waits.append((k, v))
            seen[k] = v
        if dma is None:
            c = self.cnt[eng]
            self.cnt[eng] = c + 1
            key = self._key((eng, c // EPOCH))
            ev = (key, c % EPOCH + 1)
            inc = 1
        else:
            key = self._key(("dma", dma))
            val = self.dma_vals.get(key, 0) + 16
            self.dma_vals[key] = val
            ev = (key, val)
            inc = 16
        self.final[key] = ev[1]
        for r in reads:
            if r.r.get(ev[0], 0) < ev[1]:
                r.r[ev[0]] = ev[1]
        for w in writes:
            if w.acc:
                if w.w.get(ev[0], 0) < ev[1]:
                    w.w[ev[0]] = ev[1]
            else:
                w.w = {ev[0]: ev[1]}
                w.r = {}
        self.ops[eng].append((waits, fn, key, inc))

    def wait_all(self, eng, own=False):
        waits = []
        for k, v in self.final.items():
            if self.seen[eng].get(k, 0) < v and (own or k[0] != eng):
                waits.append((k, v))
                self.seen[eng][k] = v
        if waits:
            self.ops[eng].append((waits, None, None, 0))

    def barrier(self):
        for e in ENGS:
            self.wait_all(e, own=(e in self.same))

    def emit(self, st):
        nc = self.nc
        if not hasattr(self, "sems"):
            self.sems = {}
        sems = self.sems
        for k in self.sem_keys:
            if k not in sems:
                sems[k] = st.enter_context(nc.semaphore("s_" + "_".join(str(x) for x in k)))
        self.nblk = getattr(self, "nblk", 0) + 1
        with nc.named_scope(f"ph{self.nblk}"), nc.Block() as block:
            for e in ENGS:
                ops = self.ops[e]
                if not ops:
                    continue

                def body(eng, ops=ops):
                    for waits, fn, key, inc in ops:
                        for k, v in waits:
                            eng.wait_ge(sems[k], v)
                        if fn is not None:
                            fn(eng).then_inc(sems[key], inc)
                getattr(block, e)(body)
        self.ops = {e: [] for e in ENGS}


def chunk_kind(c):
    if c < 8:
        return "xr"
    if c < 16:
        return "gr"
    if c < 28:
        return "q"
    if c < 40:
        return "k"
    if c < 52:
        return "v"
    return "gate"


DIL = (1, 4, 16)


def build(phases=6, debug=False):
    nc = bass.Bass("TRN2", target_bir_lowering=False)

    def din(name, shape, dt=F32):
        return nc.dram_tensor(name, list(shape), dt, kind="ExternalInput").ap()

    def dscr(name, shape, dt=F32):
        kind = "ExternalOutput" if debug else "Internal"
        return nc.dram_tensor(name, list(shape), dt, kind=kind).ap()

    xT = din("xT", [D, S])
    xtok = din("xtok", [S, D])
    w_in = din("w_in", [NCH, 128, 8, 128])
    b_in = din("b_in", [128, NCH])
    gmix = din("gmix", [128, 8])
    convw = din("convw", [128, 4, 8])
    convb = din("convb", [128, 8])
    rgw = din("rgw", [8, 128, 4, 128])
    rgb = din("rgb", [128, 4, 8])
    lam = din("lam", [128, 2, 8])
    prnn = din("prnn", [128, 8, D])
    pattn = din("pattn", [128, 4, D])
    wout = din("wout", [128, 8, D])
    gffn = din("gffn", [128, D])
    gfin = din("gfin", [128, D])
    wr = din("wr", [128, 8, 16])
    br = din("br", [128, 16])
    wg = din("wg", [16, 16, 128, 8, 128])
    wu = din("wu", [16, 16, 128, 8, 128])
    wd = din("wd", [16, 2, 128, 16, 512])
    cosT = din("cosT", [64, S])
    sinT = din("sinT", [64, S])
    cst = din("cst", [128, 128 + 128 + 384 + 64])

    out = nc.dram_tensor("out", [S, D], F32, kind="ExternalOutput").ap()

    XR = dscr("XR", [8, 128, S])
    GG = dscr("GG", [8, 128, S])
    QKV = dscr("QKV", [36, 128, S], BF16)
    SG = dscr("SG", [16, 128, S])
    YT = dscr("YT", [8, 128, S])
    OT = dscr("OT", [4, 128, S])
    X2 = dscr("X2", [S, D])
    H2X = dscr("H2X", [S, XW])
    XG = [dscr(f"XG{e}", [1024, XW]) for e in range(16)]

    P = Prog(nc)
    op = P.op
    R2 = lambda n: [Res(n + "0"), Res(n + "1")]
    bregs = {}

    def bc_reg(e, val):
        if val not in bregs:
            r = e.alloc_register(f"bc{val}")
            e.reg_mov(r, val)
            bregs[val] = r
        return bregs[val]
    glob = ExitStack()

    def sb(st, name, shape, dt=F32):
        return st.enter_context(nc.sbuf_tensor(name, list(shape), dt))

    ident = sb(glob, "ident", [128, 128])
    ident_bf = sb(glob, "ident_bf", [128, 128], BF16)
    ut_bf = sb(glob, "ut_bf", [128, 128], BF16)
    ones_bf = sb(glob, "ones_bf", [128, 128], BF16)
    ones_r = sb(glob, "ones_r", [128, 128], F32R)
    pmask = [sb(glob, f"pmask{i}", [128, 512], BF16) for i in range(3)]
    cst_t = sb(glob, "cst_t", [128, 704])
    one_c = sb(glob, "one_c", [128, 1])
    eps_c = sb(glob, "eps_c", [128, 1])
    AFFt = sb(glob, "AFFt", [128, 64, 16])
    R_const = Res("const")
    R_aff = Res("aff")
    op("sync", lambda e: e.dma_start(out=cst_t[:], in_=cst), writes=[R_const], dma="cst")
    op("vector", lambda e: e.tensor_copy(out=ident[:], in_=cst_t[:, 0:128]), reads=[R_const], writes=[R_const])
    op("vector", lambda e: e.tensor_copy(out=ident_bf[:], in_=cst_t[:, 0:128]), reads=[R_const], writes=[R_const])
    op("vector", lambda e: e.tensor_copy(out=ut_bf[:], in_=cst_t[:, 128:256]), reads=[R_const], writes=[R_const])
    for i_, (o0_, o1_) in enumerate(((64, 64), (128, 64), (64, 0))):
        op("vector", lambda e, i_=i_, o0_=o0_: e.tensor_copy(out=pmask[i_][:, 0:256], in_=cst_t[:, 256 + o0_:512 + o0_]),
           reads=[R_const], writes=[R_const])
        op("vector", lambda e, i_=i_, o1_=o1_: e.tensor_copy(out=pmask[i_][:, 256:512], in_=cst_t[:, 256 + o1_:512 + o1_]),
           reads=[R_const], writes=[R_const])
    op("vector", lambda e: e.memset(ones_bf[:], 1.0), writes=[R_const])
    op("vector", lambda e: e.memset(ones_r[:].bitcast(F32), 1.0), writes=[R_const])
    op("vector", lambda e: e.memset(one_c[:], 1.0), writes=[R_const])
    op("vector", lambda e: e.memset(eps_c[:], 1e-6), writes=[R_const])
    RC = [R_const]

    def _phase1():
        with ExitStack() as ph:
            hT = sb(ph, "hT", [128, 8, 2048], F32R)
            xs = [sb(ph, f"xs{i}", [128, 8, 512]) for i in range(2)]
            rstd2 = [sb(ph, f"rstd{i}", [128, 512]) for i in range(2)]
            R_rstd2 = [Res("rstd0"), Res("rstd1")]
            wch = [sb(ph, f"wch{i}", [128, 8, 128], F32R) for i in range(3)]
            stg = [sb(ph, f"stg{i}", [128, 2048]) for i in range(4)]
            stgb = [sb(ph, f"stgb{i}", [128, 2048], BF16) for i in range(4)]
            rtmp = [sb(ph, f"rtmp{i}", [64, 2048]) for i in range(2)]
            cos_t = sb(ph, "cos_t", [64, 2048])
            sin_t = sb(ph, "sin_t", [64, 2048])
            bin_t = sb(ph, "bin_t", [128, NCH])
            binq_t = sb(ph, "binq_t", [128, NCH])
            gmix_t = sb(ph, "gmix_t", [128, 8])
            pb = [ph.enter_context(nc.psum_tensor(f"pbA{i}", [128, 512], F32)) for i in range(8)]
            R_pb = [Res(f"pbA{i}") for i in range(8)]
            R_hTt = [Res(f"hT{j}") for j in range(4)]
            R_xs = [Res("xs0"), Res("xs1")]
            R_wch = [Res(f"wch{i}") for i in range(3)]
            R_stg = [Res(f"stg{i}") for i in range(4)]
            R_stgb = [Res(f"stgb{i}") for i in range(4)]
            R_rtmp = [Res("rtmp0"), Res("rtmp1")]
            R_cs = Res("cossin")
            R_scrA = Res("scrA", acc=True)
            QSC = 128.0 ** -0.5

            op("sync", lambda e: e.dma_start(out=bin_t[:], in_=b_in), writes=RC, dma="cst")
            op("sync", lambda e: e.dma_start(out=gmix_t[:], in_=gmix), writes=RC, dma="cst")
            op("vector", lambda e: e.tensor_scalar(out=binq_t[:], in0=bin_t[:], scalar1=QSC, scalar2=None, op0=ALU.mult),
               reads=RC, writes=RC)
            xT_v = xT.rearrange("(k p) t -> p k t", p=128)

            def load_x(st_, j_):
                tt_ = st_ * 2048 + j_ * 512
                op("sync", lambda e, j_=j_, tt_=tt_: e.dma_start(out=xs[j_ % 2][:], in_=xT_v[:, :, tt_:tt_ + 512]),
                   writes=[R_xs[j_ % 2]], dma=f"xs{j_ % 2}")

            def issue_w(idx):
                cc = idx % NCH
                op("gpsimd", lambda e, idx=idx, cc=cc: e.dma_start(out=wch[idx % 3][:], in_=w_in[cc]),
                   writes=[R_wch[idx % 3]], dma=f"wch{idx % 3}")
            pbi = 0
            wi = 0
            si = 0
            pending = []
            for st_i in range(4):
                t0 = st_i * 2048
                op("sync", lambda e, t0=t0: e.dma_start(out=cos_t[:], in_=cosT[:, t0:t0 + 2048]), writes=[R_cs], dma="cs")
                op("sync", lambda e, t0=t0: e.dma_start(out=sin_t[:], in_=sinT[:, t0:t0 + 2048]), writes=[R_cs], dma="cs")
                if st_i == 0:
                    load_x(0, 0)
                    load_x(0, 1)
                for j in range(4):
                    xb = xs[j % 2]
                    Rx = R_xs[j % 2]
                    rs = rstd2[j % 2]
                    Rrs = R_rstd2[j % 2]
                    js_ = slice(j * 512, (j + 1) * 512)
                    op("scalar", lambda e, xb=xb, js_=js_: e.activation(out=hT[:, :, js_], in_=xb[:], func=AF.Square),
                       reads=[Rx], writes=[R_hTt[j]])
                    pbk = pbi % 8
                    pbi += 1
                    for k in range(8):
                        op("tensor", lambda e, k=k, pbk=pbk, js_=js_: e.matmul(pb[pbk][:], lhsT=ones_r[:], rhs=hT[:, k, js_],
                                                                              start=(k == 0), stop=(k == 7)),
                           reads=[R_hTt[j]] + RC, writes=[R_pb[pbk]])
                    op("scalar", lambda e, pbk=pbk, rs=rs: e.activation(out=rs[:], in_=pb[pbk][:], func=AF.Sqrt,
                                                                        bias=eps_c[:, 0:1], scale=1.0 / D),
                       reads=[R_pb[pbk]] + RC, writes=[Rrs])
                    op("vector", lambda e, rs=rs: e.reciprocal(out=rs[:], in_=rs[:]), reads=[Rrs], writes=[Rrs])
                    for k in range(8):
                        op("vector", lambda e, k=k, xb=xb, js_=js_, rs=rs: e.scalar_tensor_tensor(
                            out=hT[:, k, js_], in0=xb[:, k, :], scalar=gmix_t[:, k:k + 1],
                            in1=rs[:], op0=ALU.mult, op1=ALU.mult),
                           reads=[Rx, Rrs] + RC, writes=[R_hTt[j]])
                    if j + 2 < 4:
                        load_x(st_i, j + 2)
                for c in range(NCH):
                    kind = chunk_kind(c)
                    wb = wch[wi % 3]
                    Rw = R_wch[wi % 3]
                    if wi == 0:
                        issue_w(0)
                        issue_w(1)
                    if wi + 2 < 4 * NCH:
                        issue_w(wi + 2)
                    wi += 1
                    if c == 56 and st_i + 1 < 4:
                        load_x(st_i + 1, 0)
                        load_x(st_i + 1, 1)
                    sgi = si % 4
                    si += 1
                    use_b = kind in ("q", "k", "v")
                    for j in range(4):
                        pbk = pbi % 8
                        pbi += 1
                        for k in range(8):
                            op("tensor", lambda e, k=k, pbk=pbk, wb=wb, j=j: e.matmul(
                                pb[pbk][:], lhsT=wb[:, k, :], rhs=hT[:, k, j * 512:(j + 1) * 512],
                                start=(k == 0), stop=(k == 7)),
                               reads=[Rw, R_hTt[j]], writes=[R_pb[pbk]])
                        js = slice(j * 512, (j + 1) * 512)
                        if kind == "xr":
                            fn = lambda e, pbk=pbk, js=js, c=c, sgi=sgi: e.activation(
                                out=stg[sgi][:, js], in_=pb[pbk][:], func=AF.Identity, bias=bin_t[:, c:c + 1], scale=1.0)
                            wr_ = [R_stg[sgi]]
                        elif kind == "gr":
                            fn = lambda e, pbk=pbk, js=js, c=c, sgi=sgi: e.activation(
                                out=stg[sgi][:, js], in_=pb[pbk][:], func=AF.Gelu, bias=bin_t[:, c:c + 1], scale=1.0)
                            wr_ = [R_stg[sgi]]
                        elif kind == "gate":
                            fn = lambda e, pbk=pbk, js=js, c=c, sgi=sgi: e.activation(
                                out=stg[sgi][:, js], in_=pb[pbk][:], func=AF.Sigmoid, bias=bin_t[:, c:c + 1], scale=1.0)
                            wr_ = [R_stg[sgi]]
                        elif kind == "q":
                            fn = lambda e, pbk=pbk, js=js, c=c, sgi=sgi: e.activation(
                                out=stg[sgi][:, js], in_=pb[pbk][:], func=AF.Identity, bias=binq_t[:, c:c + 1], scale=QSC)
                            wr_ = [R_stg[sgi]]
                        elif kind == "k":
                            fn = lambda e, pbk=pbk, js=js, c=c, sgi=sgi: e.activation(
                                out=stg[sgi][:, js], in_=pb[pbk][:], func=AF.Identity, bias=bin_t[:, c:c + 1], scale=1.0)
                            wr_ = [R_stg[sgi]]
                        else:
                            dv = DIL[((c - 16) % 12) // 4]
                            nj = 512 // dv
                            fn = lambda e, pbk=pbk, j=j, c=c, sgi=sgi, dv=dv, nj=nj: e.activation(
                                out=stgb[sgi][:].rearrange("p (r l) -> p r l", r=dv)[:, :, j * nj:(j + 1) * nj],
                                in_=pb[pbk][:].rearrange("p (l r) -> p r l", r=dv),
                                func=AF.Identity, bias=bin_t[:, c:c + 1], scale=1.0)
                            wr_ = [R_stgb[sgi]]
                        op("scalar", fn, reads=[R_pb[pbk]] + RC, writes=wr_)
                    tail_ops = []
                    if use_b:
                        g = ((c - 16) % 12) // 4
                        d = DIL[g]
                        sgt = stg[sgi]
                        sbt = stgb[sgi]
                        if kind in ("q", "k"):
                            op("vector", lambda e, sgt=sgt: e.tensor_tensor(
                                out=rtmp[0][0:32, :], in0=sgt[32:64, :], in1=sin_t[32:64, :], op=ALU.mult),
                               reads=[R_stg[sgi], R_cs], writes=[R_rtmp[0]])
                            op("vector", lambda e, sgt=sgt: e.tensor_tensor(
                                out=rtmp[0][32:64, :], in0=sgt[0:32, :], in1=sin_t[0:32, :], op=ALU.mult),
                               reads=[R_stg[sgi], R_cs], writes=[R_rtmp[0]])
                            op("vector", lambda e, sgt=sgt: e.tensor_tensor(
                                out=rtmp[1][:, :], in0=sgt[0:64, :], in1=cos_t[:, :], op=ALU.mult),
                               reads=[R_cs, R_stg[sgi]], writes=[R_rtmp[1]])
                            op("vector", lambda e, sgt=sgt: e.tensor_tensor(
                                out=sgt[0:64, :], in0=rtmp[0][:, :], in1=rtmp[1][:, :], op=ALU.add),
                               reads=[R_rtmp[0], R_rtmp[1]], writes=[R_stg[sgi]])
                            tail_ops.append(("scalar", lambda e, sgt=sgt, sbt=sbt, d=d: e.activation(
                                out=sbt[:].rearrange("p (r j) -> p r j", r=d),
                                in_=sgt[:].rearrange("p (j r) -> p r j", r=d), func=AF.Copy),
                                [R_stg[sgi]], [R_stgb[sgi]], None))
                        qi = c - 16
                        j0 = t0 // d
                        nj = 2048 // d
                        dst = QKV[qi].rearrange("p (r l) -> p r l", r=d)[:, :, j0:j0 + nj]
                        src = sbt[:].rearrange("p (r j) -> p r j", r=d)
                        tail_ops.append(("sync", lambda e, dst=dst, src=src: e.dma_start(out=dst, in_=src),
                                         [R_stgb[sgi]], [R_scrA], f"stgb{sgi}"))
                    else:
                        if kind == "xr":
                            dst = XR[c][:, t0:t0 + 2048]
                        elif kind == "gr":
                            dst = GG[c - 8][:, t0:t0 + 2048]
                        else:
                            dst = SG[c - 52][:, t0:t0 + 2048]
                        tail_ops.append(("sync", lambda e, dst=dst, sgi=sgi: e.dma_start(out=dst, in_=stg[sgi][:]),
                                         [R_stg[sgi]], [R_scrA], f"stg{sgi}"))
                    for (en_, fn_, rd_, wr2_, dm_) in pending:
                        op(en_, fn_, reads=rd_, writes=wr2_, dma=dm_)
                    pending[:] = tail_ops
            for (en_, fn_, rd_, wr2_, dm_) in pending:
                op(en_, fn_, reads=rd_, writes=wr2_, dma=dm_)
            P.barrier()
            P.emit(glob)
    _phase1()
    if phases <= 1:
        return finish(nc, P, glob, out)

    SEG = 1024
    NSEG = S // SEG
    def _phase2():
        with ExitStack() as ph:
            xc2 = [sb(ph, f"xc{i}", [128, S], F32R) for i in range(2)]
            hf = sb(ph, "hf", [128, S])
            raw = [sb(ph, f"raw{i}", [128, SEG + 3]) for i in range(2)]
            xcr = [sb(ph, f"xcr{i}", [128, SEG]) for i in range(2)]
            r_ = [sb(ph, f"r_{i}", [128, SEG]) for i in range(2)]
            i_ = [sb(ph, f"i_{i}", [128, SEG]) for i in range(2)]
            a2_ = [sb(ph, f"a2_{i}", [128, SEG]) for i in range(2)]
            hb_ = [sb(ph, f"hb_{i}", [128, SEG]) for i in range(2)]
            gg_ = [sb(ph, f"gg_{i}", [128, SEG]) for i in range(2)]
            ys_ = [sb(ph, f"ys_{i}", [128, SEG]) for i in range(2)]
            rgw_t = [sb(ph, f"rgw_t{i}", [128, 4, 128], F32R) for i in range(2)]
            rgb_t = sb(ph, "rgb_t", [128, 4, 8])
            kap = sb(ph, "kap", [128, 2, 8])
            kap2 = sb(ph, "kap2", [128, 2, 8])
            hrgb_t = sb(ph, "hrgb_t", [128, 4, 8])
            quarter_c = sb(ph, "quarter_c", [128, 1])
            cw_t = sb(ph, "cw_t", [128, 4, 8])
            cb_t = sb(ph, "cb_t", [128, 8])
            pb = [ph.enter_context(nc.psum_tensor(f"pbB{i}", [128, 512], F32)) for i in range(8)]
            R_pb = [Res(f"pbB{i}") for i in range(8)]
            R_xc2, R_hf = R2("xc"), Res("hf")
            R_raw, R_xcr, R_r, R_i, R_a2, R_hb, R_gg, R_ys, R_rgw = (R2("raw"), R2("xcr"), R2("r"), R2("i"), R2("a2"),
                                                                     R2("hb"), R2("gg"), R2("ys"), R2("rgw"))
            R_scrB = Res("scrB", acc=True)
            op("sync", lambda e: e.dma_start(out=rgb_t[:], in_=rgb), writes=RC, dma="cst")
            op("sync", lambda e: e.dma_start(out=kap[:], in_=lam), writes=RC, dma="cst")
            op("sync", lambda e: e.dma_start(out=cw_t[:], in_=convw), writes=RC, dma="cst")
            op("sync", lambda e: e.dma_start(out=cb_t[:], in_=convb), writes=RC, dma="cst")
            op("vector", lambda e: e.tensor_scalar(out=hrgb_t[:], in0=rgb_t[:], scalar1=0.5, scalar2=None, op0=ALU.mult),
               reads=RC, writes=RC)
            op("vector", lambda e: e.memset(quarter_c[:], 0.25), writes=RC)
            op("scalar", lambda e: e.activation(out=kap[:], in_=kap[:], func=AF.Exp, scale=-1.0), reads=RC, writes=RC)
            op("scalar", lambda e: e.activation(out=kap[:], in_=kap[:], func=AF.Ln, bias=one_c[:, 0:1], scale=1.0),
               reads=RC, writes=RC)
            op("vector", lambda e: e.tensor_scalar(out=kap2[:], in0=kap[:], scalar1=-4.0, scalar2=None, op0=ALU.mult),
               reads=RC, writes=RC)
            op("vector", lambda e: e.tensor_scalar(out=kap[:], in0=kap[:], scalar1=-8.0, scalar2=None, op0=ALU.mult),
               reads=RC, writes=RC)
            steps = []
            for c in range(8):
                for dirn in range(2):
                    for sgm in (range(NSEG) if dirn == 0 else range(NSEG - 1, -1, -1)):
                        steps.append((c, dirn, sgm))
            pbs = [0]

            def load_w(c):
                op("gpsimd", lambda e, c=c: e.dma_start(out=rgw_t[c % 2][:], in_=rgw[c]), writes=[R_rgw[c % 2]], dma=f"rgw{c % 2}")

            def stageX(n):
                c, dirn, sgm = steps[n]
                b2 = n % 2
                xc, R_xc = xc2[c % 2], R_xc2[c % 2]
                wt, Rwt = rgw_t[c % 2], R_rgw[c % 2]
                t0 = sgm * SEG
                ts_ = slice(t0, t0 + SEG)
                if dirn == 0 and sgm == 0 and c + 1 < 8:
                    load_w(c + 1)
                if dirn == 0:
                    rw = raw[b2]
                    lo = max(t0 - 2, 0)
                    hi = min(t0 + SEG + 1, S)
                    if sgm == 0:
                        op("vector", lambda e, rw=rw: e.memset(rw[:, 0:2], 0.0), writes=[R_raw[b2]])
                    if sgm == NSEG - 1:
                        op("vector", lambda e, rw=rw: e.memset(rw[:, SEG + 2:SEG + 3], 0.0), writes=[R_raw[b2]])
                    o0 = lo - (t0 - 2)
                    op("sync", lambda e, rw=rw, c=c, lo=lo, hi=hi, o0=o0: e.dma_start(
                        out=rw[:, o0:o0 + hi - lo], in_=XR[c][:, lo:hi]), writes=[R_raw[b2]], dma=f"raw{b2}")
                    ct = xcr[b2]
                    op("vector", lambda e, rw=rw, c=c, ct=ct: e.tensor_scalar(
                        out=ct[:], in0=rw[:, 0:SEG], scalar1=cw_t[:, 0, c:c + 1], scalar2=cb_t[:, c:c + 1],
                        op0=ALU.mult, op1=ALU.add), reads=[R_raw[b2]] + RC, writes=[R_xcr[b2]])
                    for tap in range(1, 4):
                        op("vector", lambda e, rw=rw, c=c, ts_=ts_, tap=tap, xc=xc, ct=ct: e.scalar_tensor_tensor(
                            out=(xc[:, ts_] if tap == 3 else ct[:]), in0=rw[:, tap:tap + SEG],
                            scalar=cw_t[:, tap, c:c + 1], in1=ct[:], op0=ALU.mult, op1=ALU.add),
                           reads=[R_raw[b2], R_xcr[b2]] + RC, writes=([R_xc] if tap == 3 else [R_xcr[b2]]))
                else:
                    op("sync", lambda e, b2=b2, c=c, ts_=ts_: e.dma_start(out=gg_[b2][:], in_=GG[c][:, ts_]),
                       writes=[R_gg[b2]], dma=f"gg{b2}")
                for j in range(SEG // 512):
                    js = slice(j * 512, (j + 1) * 512)
                    for gt, dstt, Rd in ((0, r_[b2], R_r[b2]), (1, i_[b2], R_i[b2])):
                        pbk = pbs[0] % 8
                        pbs[0] += 1
                        gi = dirn * 2 + gt
                        op("tensor", lambda e, pbk=pbk, wt=wt, gi=gi, xc=xc, j=j, t0=t0: e.matmul(
                            pb[pbk][:], lhsT=wt[:, gi, :], rhs=xc[:, t0 + j * 512:t0 + (j + 1) * 512], start=True, stop=True),
                           reads=[Rwt, R_xc], writes=[R_pb[pbk]])
                        op("scalar", lambda e, pbk=pbk, dstt=dstt, js=js, gi=gi, c=c: e.activation(
                            out=dstt[:, js], in_=pb[pbk][:], func=AF.Tanh, bias=hrgb_t[:, gi, c:c + 1], scale=0.5),
                           reads=[R_pb[pbk]] + RC, writes=[Rd])
                rr, aa = r_[b2], a2_[b2]
                op("scalar", lambda e, rr=rr, aa=aa, dirn=dirn, c=c: e.activation(
                    out=aa[:], in_=rr[:], func=AF.Exp, scale=kap[:, dirn, c:c + 1], bias=kap[:, dirn, c:c + 1]),
                   reads=[R_r[b2]] + RC, writes=[R_a2[b2]])
                op("scalar", lambda e, rr=rr, dirn=dirn, c=c: e.activation(
                    out=rr[:], in_=rr[:], func=AF.Exp, scale=kap2[:, dirn, c:c + 1], bias=kap2[:, dirn, c:c + 1]),
                   reads=RC, writes=[R_r[b2]])
                op("scalar", lambda e, aa=aa: e.activation(
                    out=aa[:], in_=aa[:], func=AF.Sqrt, bias=quarter_c[:, 0:1], scale=-0.25),
                   reads=RC, writes=[R_a2[b2]])

            def stageY(n):
                c, dirn, sgm = steps[n]
                b2 = n % 2
                xc, R_xc = xc2[c % 2], R_xc2[c % 2]
                t0 = sgm * SEG
                ts_ = slice(t0, t0 + SEG)
                rr, ii, aa = r_[b2], i_[b2], a2_[b2]
                op("vector", lambda e, aa=aa, ts_=ts_, xc=xc: e.tensor_tensor(
                    out=aa[:], in0=aa[:], in1=xc[:, ts_].bitcast(F32), op=ALU.mult),
                   reads=[R_xc], writes=[R_a2[b2]])
                op("vector", lambda e, ii=ii, aa=aa: e.scalar_tensor_tensor(
                    out=ii[:], in0=ii[:], scalar=1.0, in1=aa[:], op0=ALU.add, op1=ALU.mult),
                   reads=[R_a2[b2]], writes=[R_i[b2]])
                if dirn == 0:
                    init = 0.0 if sgm == 0 else hf[:, t0 - 1:t0]
                    op("vector", lambda e, rr=rr, ii=ii, ts_=ts_, init=init: e.tensor_tensor_scan(
                        out=hf[:, ts_], data0=rr[:], data1=ii[:], initial=init, op0=ALU.mult, op1=ALU.add),
                       reads=[R_r[b2], R_i[b2]], writes=[R_hf])
                else:
                    hb = hb_[b2]
                    if sgm == NSEG - 1:
                        init = 0.0
                        rd_extra = []
                    else:
                        init = hb_[1 - b2][:, 0:1]
                        rd_extra = [R_hb[1 - b2]]
                    op("vector", lambda e, rr=rr, ii=ii, hb=hb, init=init: e.tensor_tensor_scan(
                        out=hb[:, ::-1], data0=rr[:, ::-1], data1=ii[:, ::-1], initial=init,
                        op0=ALU.mult, op1=ALU.add),
                       reads=[R_r[b2], R_i[b2]] + rd_extra, writes=[R_hb[b2]])
                    gg = gg_[b2]
                    ys = ys_[b2]
                    op("vector", lambda e, ys=ys, hb=hb, ts_=ts_: e.tensor_tensor(
                        out=ys[:], in0=hb[:], in1=hf[:, ts_], op=ALU.add),
                       reads=[R_hb[b2], R_hf], writes=[R_ys[b2]])
                    op("vector", lambda e, ys=ys, gg=gg: e.tensor_tensor(out=ys[:], in0=ys[:], in1=gg[:], op=ALU.mult),
                       reads=[R_gg[b2]], writes=[R_ys[b2]])
                    op("sync", lambda e, ys=ys, c=c, ts_=ts_: e.dma_start(out=YT[c][:, ts_], in_=ys[:]),
                       reads=[R_ys[b2]], writes=[R_scrB], dma=f"ys{b2}")

            load_w(0)
            stageX(0)
            for n in range(len(steps)):
                if n + 1 < len(steps):
                    stageX(n + 1)
                stageY(n)
            P.barrier()
            P.emit(glob)
    _phase2()
    if phases <= 2:
        return finish(nc, P, glob, out)

    def _phase3():
        with ExitStack() as ph:
            Qd = [sb(ph, f"Qd{i}", [128, S], BF16) for i in range(2)]
            Kd = [sb(ph, f"Kd{i}", [128, S], BF16) for i in range(2)]
            Vd = sb(ph, "Vd", [128, S], BF16)
            Vt = sb(ph, "Vt", [128, 64, 128], BF16)
            NUM = sb(ph, "NUM", [128, S])
            DEN = sb(ph, "DEN", [128, S])
            pT = [sb(ph, f"pT{i}", [128, 512], BF16) for i in range(4)]
            ostg = [sb(ph, f"ostg{i}", [128, 1024]) for i in range(2)]
            ps_s = [ph.enter_context(nc.psum_tensor(f"ps_s{i}", [128, 512], F32)) for i in range(3)]
            ps_n = [ph.enter_context(nc.psum_tensor(f"ps_n{i}", [128, 512], F32)) for i in range(2)]
            ps_d = [ph.enter_context(nc.psum_tensor(f"ps_d{i}", [128, 512], F32)) for i in range(2)]
            ps_t = [ph.enter_context(nc.psum_tensor(f"ps_t{i}", [128, 512], BF16)) for i in range(1)]
            R_Qd, R_Kd = R2("Qd"), R2("Kd")
            R_Vd, R_Vt, R_NUM, R_DEN = Res("Vd"), Res("Vt"), Res("NUM"), Res("DEN")
            R_pT = [Res(f"pT{i}") for i in range(4)]
            R_ostg = R2("ostg")
            R_pss, R_psn, R_psd, R_pst = R2("pss"), R2("psn"), R2("psd"), R2("pst")
            R_pss4 = [Res(f"pss4{i}") for i in range(4)]
            R_scrC = Res("scrC", acc=True)
            ucnt = 0
            pcnt = 0
            for h in range(4):
                for g in range(3):
                    d = DIL[g]
                    L = S // d
                    u2 = ucnt % 2
                    ucnt += 1
                    qd, kd = Qd[u2], Kd[u2]
                    op("sync", lambda e, qd=qd, g=g, h=h: e.dma_start(out=qd[:], in_=QKV[g * 4 + h]),
                       writes=[R_Qd[u2]], dma=f"Qd{u2}")
                    op("sync", lambda e, kd=kd, g=g, h=h: e.dma_start(out=kd[:], in_=QKV[12 + g * 4 + h]),
                       writes=[R_Kd[u2]], dma=f"Kd{u2}")
                    op("sync", lambda e, g=g, h=h: e.dma_start(out=Vd[:], in_=QKV[24 + g * 4 + h]),
                       writes=[R_Vd], dma="Vd")
                    tviews = [(ps_t[0][:], R_pst[0])] + [(ps_s[i_][:].bitcast(BF16)[:, 0:512], R_pss4[i_]) for i_ in range(3)]
                    for m4 in range(16):
                        tv, Rtv = tviews[m4 % 4]
                        for q in range(4):
                            m = m4 * 4 + q
                            op("tensor", lambda e, tv=tv, q=q, m=m: e.transpose(
                                out=tv[:, q * 128:(q + 1) * 128], in_=Vd[:, m * 128:(m + 1) * 128], identity=ident_bf[:]),
                               reads=[R_Vd] + RC, writes=[Rtv])
                        if m4 % 2 == 0:
                            op("vector", lambda e, tv=tv, m4=m4: e.tensor_copy(
                                out=Vt[:, m4 * 4:(m4 + 1) * 4, :].rearrange("p a b -> p (a b)"), in_=tv),
                               reads=[Rtv], writes=[R_Vt])
                        else:
                            op("scalar", lambda e, tv=tv, m4=m4: e.activation(
                                out=Vt[:, m4 * 4:(m4 + 1) * 4, :].rearrange("p a b -> p (a b)"), in_=tv, func=AF.Copy),
                               reads=[Rtv], writes=[R_Vt])
                    contribs = []
                    for m in range(64):
                        p0 = m * 128
                        r = p0 // L
                        lo, hi = r * L, (r + 1) * L
                        if p0 == lo:
                            qs, var = p0, 1
                        elif p0 + 128 == hi:
                            qs, var = p0 - 128, 2
                        else:
                            qs, var = p0 - 64, 0
                        qe = qs + 256
                        parts = []
                        sb0 = (qs // 512) * 512
                        while sb0 < qe:
                            a, b = max(qs, sb0), min(qe, sb0 + 512)
                            parts.append((sb0 // 512, a, b))
                            sb0 += 512
                        contribs.append((qs, qe, var, parts))
                    last_of = {}
                    for m, (_, _, _, parts) in enumerate(contribs):
                        for (sbi, a, b) in parts:
                            last_of[sbi] = m
                    started = set()
                    LOOK = 2

                    def front(pi):
                        s3 = pi % 3
                        pt = pT[pi % 4]
                        Rp = R_pT[pi % 4]
                        for hf_ in range(2):
                            m = 2 * pi + hf_
                            qs, qe, var, parts = contribs[m]
                            op("tensor", lambda e, s3=s3, hf_=hf_, m=m, qs=qs, qe=qe, kd=kd, qd=qd: e.matmul(
                                ps_s[s3][:, hf_ * 256:(hf_ + 1) * 256], lhsT=kd[:, m * 128:(m + 1) * 128], rhs=qd[:, qs:qe],
                                start=True, stop=True),
                               reads=[R_Qd[u2], R_Kd[u2]], writes=[R_pss4[s3]])
                        v0, v1 = contribs[2 * pi][2], contribs[2 * pi + 1][2]
                        pm = pmask[{(0, 0): 0, (1, 0): 1, (0, 2): 2}[(v0, v1)]]
                        op("scalar", lambda e, s3=s3, pt=pt: e.activation(out=pt[:], in_=ps_s[s3][:], func=AF.Exp),
                           reads=[R_pss4[s3]], writes=[Rp])
                        op("vector", lambda e, pt=pt, pm=pm: e.tensor_tensor(out=pt[:], in0=pt[:], in1=pm[:], op=ALU.mult),
                           reads=RC, writes=[Rp])

                    def back(pi):
                        pt = pT[pi % 4]
                        Rp = R_pT[pi % 4]
                        for hf_ in range(2):
                            m = 2 * pi + hf_
                            qs, qe, var, parts = contribs[m]
                            for (sbi, a, b) in parts:
                                n2 = sbi % 2
                                first = sbi not in started
                                started.add(sbi)
                                c0, c1 = a - sbi * 512, b - sbi * 512
                                r0, r1 = hf_ * 256 + a - qs, hf_ * 256 + b - qs
                                op("tensor", lambda e, n2=n2, m=m, pt=pt, r0=r0, r1=r1, c0=c0, c1=c1, first=first: e.matmul(
                                    ps_n[n2][:, c0:c1], lhsT=Vt[:, m, :], rhs=pt[:, r0:r1], start=first, stop=False,
                                    skip_group_check=True),
                                   reads=[R_Vt, Rp], writes=[R_psn[n2]])
                                op("tensor", lambda e, n2=n2, pt=pt, r0=r0, r1=r1, c0=c0, c1=c1, first=first: e.matmul(
                                    ps_d[n2][:, c0:c1], lhsT=ones_bf[:], rhs=pt[:, r0:r1], start=first, stop=False,
                                    skip_group_check=True),
                                   reads=[Rp] + RC, writes=[R_psd[n2]])
                                if last_of[sbi] == m:
                                    P0 = sbi * 512
                                    r = P0 // L
                                    j0 = P0 - r * L
                                    nat = slice(r + d * j0, r + d * (j0 + 511) + 1, d) if d > 1 else slice(P0, P0 + 512)
                                    if g == 0:
                                        op("vector", lambda e, n2=n2, nat=nat: e.tensor_copy(out=NUM[:, nat], in_=ps_n[n2][:]),
                                           reads=[R_psn[n2]], writes=[R_NUM])
                                        op("scalar", lambda e, n2=n2, nat=nat: e.activation(out=DEN[:, nat], in_=ps_d[n2][:], func=AF.Copy),
                                           reads=[R_psd[n2]], writes=[R_DEN])
                                    else:
                                        op("vector", lambda e, n2=n2, nat=nat: e.tensor_tensor(
                                            out=NUM[:, nat], in0=ps_n[n2][:], in1=NUM[:, nat], op=ALU.add),
                                           reads=[R_psn[n2]], writes=[R_NUM])
                                        op("vector", lambda e, n2=n2, nat=nat: e.tensor_tensor(
                                            out=DEN[:, nat], in0=ps_d[n2][:], in1=DEN[:, nat], op=ALU.add),
                                           reads=[R_psd[n2]], writes=[R_DEN])

                    for mm in range(32 + LOOK):
                        if mm < 32:
                            front(mm)
                        if mm >= LOOK:
                            back(mm - LOOK)
                for pc in range(8):
                    o2 = pc % 2
                    cs = slice(pc * 1024, (pc + 1) * 1024)
                    op("vector", lambda e, cs=cs: e.reciprocal(out=DEN[:, cs], in_=DEN[:, cs]), writes=[R_DEN])
                    op("vector", lambda e, cs=cs, o2=o2: e.tensor_tensor(out=ostg[o2][:], in0=NUM[:, cs], in1=DEN[:, cs], op=ALU.mult),
                       reads=[R_NUM, R_DEN], writes=[R_ostg[o2]])
                    op("sync", lambda e, cs=cs, o2=o2, h=h: e.dma_start(out=OT[h][:, cs], in_=ostg[o2][:]),
                       reads=[R_ostg[o2]], writes=[R_scrC], dma=f"ostg{o2}")
            P.barrier()
            P.emit(glob)
    _phase3()
    if phases <= 3:
        return finish(nc, P, glob, out)

    TD = 256
    def _phase4():
        with ExitStack() as ph:
            prnn_t = sb(ph, "prnn_t", [128, 8, D], F32R)
            pattn_t = sb(ph, "pattn_t", [128, 4, D], F32R)
            wout_t = sb(ph, "wout_t", [128, 8, D], F32R)
            wr_t = sb(ph, "wr_t", [128, 8, 16])
            br_t = sb(ph, "br_t", [128, 16])
            gffn_t = sb(ph, "gffn_t", [128, D])
            yt = [sb(ph, f"yt{i}", [128, 8, TD], F32R) for i in range(2)]
            ot = [sb(ph, f"ot{i}", [128, 4, TD], F32R) for i in range(2)]
            sga = [sb(ph, f"sga{i}", [128, 8, TD]) for i in range(2)]
            sgb = [sb(ph, f"sgb{i}", [128, 8, TD]) for i in range(2)]
            mg = sb(ph, "mg", [128, 8, TD], F32R)
            mtmp = [sb(ph, f"mtmp{i}", [128, TD]) for i in range(2)]
            mtmp2 = [sb(ph, f"mtmpb{i}", [128, TD]) for i in range(2)]
            R_mtmp2 = R2("mtmp2")
            xt_ = [sb(ph, f"xt_{i}", [128, D]) for i in range(4)]
            x2_ = [sb(ph, f"x2_{i}", [128, D]) for i in range(2)]
            h2_ = [sb(ph, f"h2_{i}", [128, XW]) for i in range(3)]
            h2T = sb(ph, "h2T", [128, 8, 128])
            sm = sb(ph, "sm", [128, 8])
            lg = sb(ph, "lg", [128, 16])
            pb = [ph.enter_context(nc.psum_tensor(f"pbD{i}", [128, 512], F32)) for i in range(8)]
            R_pb = [Res(f"pbD{i}") for i in range(8)]
            R_wD = Res("wD")
            R_yt, R_ot, R_sga, R_sgb, R_mtmp, R_xt, R_x2, R_h2 = (R2("yt"), R2("ot"), R2("sga"), R2("sgb"), R2("mtmp"),
                                                                  R2("xt"), R2("x2"), R2("h2"))
            R_mg, R_h2T, R_sm, R_junk, R_lg = Res("mg"), Res("h2T"), Res("sm"), Res("junk"), Res("lg")
            R_xt = [Res(f"xt{i}") for i in range(4)]
            R_h2 = [Res(f"h2{i}") for i in range(3)]
            R_scrD = Res("scrD", acc=True)
            op("gpsimd", lambda e: e.dma_start(out=prnn_t[:], in_=prnn, max_dma_last_dim=4096), writes=[R_wD], dma="wD")
            op("gpsimd", lambda e: e.dma_start(out=pattn_t[:], in_=pattn, max_dma_last_dim=4096), writes=[R_wD], dma="wD")
            op("gpsimd", lambda e: e.dma_start(out=wout_t[:], in_=wout, max_dma_last_dim=4096), writes=[R_wD], dma="wD")
            op("sync", lambda e: e.dma_start(out=wr_t[:], in_=wr), writes=[R_wD], dma="wDs")
            op("sync", lambda e: e.dma_start(out=br_t[:], in_=br), writes=[R_wD], dma="wDs")
            op("sync", lambda e: e.dma_start(out=gffn_t[:], in_=gffn), writes=[R_wD], dma="wDs")
            pbi = 0
            bcnt = 0
            YT_v = YT.rearrange("c p t -> p c t")
            OT_v = OT.rearrange("c p t -> p c t")
            SG_v = SG.rearrange("c p t -> p c t")
            def loadsD(ti):
                t0 = ti * TD
                b2 = ti % 2
                op("gpsimd", lambda e, b2=b2, t0=t0: e.dma_start(out=yt[b2][:], in_=YT_v[:, :, t0:t0 + TD]),
                   writes=[R_yt[b2]], dma=f"yt{b2}")
                op("gpsimd", lambda e, b2=b2, t0=t0: e.dma_start(out=ot[b2][:], in_=OT_v[:, :, t0:t0 + TD]),
                   writes=[R_ot[b2]], dma=f"ot{b2}")
                op("sync", lambda e, b2=b2, t0=t0: e.dma_start(out=sga[b2][:], in_=SG_v[:, 0:8, t0:t0 + TD]),
                   writes=[R_sga[b2]], dma=f"sga{b2}")
                op("sync", lambda e, b2=b2, t0=t0: e.dma_start(out=sgb[b2][:], in_=SG_v[:, 8:16, t0:t0 + TD]),
                   writes=[R_sgb[b2]], dma=f"sgb{b2}")
                for bb in range(TD // 128):
                    q4 = (ti % 2) * 2 + bb
                    tb = (ti * (TD // 128) + bb) * 128
                    op("sync", lambda e, q4=q4, tb=tb: e.dma_start(out=xt_[q4][:], in_=xtok[tb:tb + 128, :]),
                       writes=[R_xt[q4]], dma=f"xt{q4}")

            def router_q1(q2, blk, tb):
                nonlocal pbi
                pa = pbi % 8
                pbi += 1
                pa2 = pbi % 8
                pbi += 1
                for k in range(8):
                    pp = pa if k < 4 else pa2
                    op("tensor", lambda e, pp=pp, k=k, q2=q2: e.transpose(
                        out=pb[pp][:, (k % 4) * 128:(k % 4 + 1) * 128], in_=h2_[q2][:, k * 128:(k + 1) * 128], identity=ident[:]),
                       reads=[R_h2[q2]] + RC, writes=[R_pb[pp]])
                op("vector", lambda e, pa=pa: e.tensor_copy(out=h2T[:, 0:4, :].rearrange("p a b -> p (a b)"), in_=pb[pa][:]),
                   reads=[R_pb[pa]], writes=[R_h2T])
                op("scalar", lambda e, pa2=pa2: e.activation(out=h2T[:, 4:8, :].rearrange("p a b -> p (a b)"), in_=pb[pa2][:], func=AF.Copy),
                   reads=[R_pb[pa2]], writes=[R_h2T])

            def router_q2(q2, blk, tb):
                nonlocal pbi
                pa = pbi % 8
                pbi += 1
                for k in range(8):
                    op("tensor", lambda e, pa=pa, k=k: e.matmul(
                        pb[pa][:, 0:16], lhsT=h2T[:, k, :], rhs=wr_t[:, k, :], start=(k == 0), stop=(k == 7)),
                       reads=[R_h2T, R_wD], writes=[R_pb[pa]])
                op("vector", lambda e, pa=pa: e.tensor_tensor(out=lg[:], in0=pb[pa][:, 0:16], in1=br_t[:], op=ALU.add),
                   reads=[R_pb[pa], R_wD], writes=[R_lg])
                op("vector", lambda e: e.reduce_max(out=sm[:, 3:4], in_=lg[:], axis=AX.X), reads=[R_lg], writes=[R_sm])
                op("vector", lambda e: e.tensor_scalar(out=sm[:, 3:4], in0=sm[:, 3:4], scalar1=-1.0, scalar2=None, op0=ALU.mult),
                   writes=[R_sm])
                op("scalar", lambda e: e.activation(out=lg[:], in_=lg[:], func=AF.Exp, bias=sm[:, 3:4], scale=1.0,
                                                    accum_out=sm[:, 4:5]), reads=[R_sm], writes=[R_lg, R_sm])
                op("vector", lambda e: e.reciprocal(out=sm[:, 5:6], in_=sm[:, 4:5]), writes=[R_sm])
                op("vector", lambda e, blk=blk: e.tensor_scalar(
                    out=AFFt[:, blk, :], in0=lg[:], scalar1=sm[:, 5:6], scalar2=None, op0=ALU.mult),
                   reads=[R_lg, R_sm], writes=[R_aff])
                op("gpsimd", lambda e, q2=q2, blk=blk: e.tensor_copy(out=h2_[q2][:, D:D + 16], in_=AFFt[:, blk, :]),
                   reads=[R_aff], writes=[R_h2[q2]])
                op("gpsimd", lambda e, q2=q2, blk=blk: e.tensor_copy(out=h2_[q2][:, D + 16:D + 24],
                                                                      in_=cst_t[:, 640 + blk:641 + blk].to_broadcast([128, 8])),
                   reads=RC, writes=[R_h2[q2]])
                op("sync", lambda e, q2=q2, tb=tb: e.dma_start(out=H2X[tb:tb + 128, :], in_=h2_[q2][:]),
                   reads=[R_h2[q2]], writes=[R_scrD], dma=f"h2{q2}")

            pendingD = []
            loadsD(0)
            for ti in range(S // TD):
                t0 = ti * TD
                b2 = ti % 2
                if ti + 1 < S // TD:
                    loadsD(ti + 1)
                for dc in range(8):
                    pa = pbi % 8
                    pbi += 1
                    for k in range(8):
                        op("tensor", lambda e, pa=pa, k=k, dc=dc, b2=b2: e.matmul(
                            pb[pa][:, 0:TD], lhsT=prnn_t[:, k, dc * 128:(dc + 1) * 128], rhs=yt[b2][:, k, :],
                            start=(k == 0), stop=(k == 7)), reads=[R_wD, R_yt[b2]], writes=[R_pb[pa]])
                    for k in range(4):
                        op("tensor", lambda e, pa=pa, k=k, dc=dc, b2=b2: e.matmul(
                            pb[pa][:, TD:2 * TD], lhsT=pattn_t[:, k, dc * 128:(dc + 1) * 128], rhs=ot[b2][:, k, :],
                            start=(k == 0), stop=(k == 3)), reads=[R_wD, R_ot[b2]], writes=[R_pb[pa]])
                    m2 = dc % 2
                    op("vector", lambda e, pa=pa, dc=dc, b2=b2, m2=m2: e.tensor_tensor(
                        out=mtmp[m2][:], in0=pb[pa][:, 0:TD], in1=sga[b2][:, dc, :], op=ALU.mult),
                       reads=[R_pb[pa], R_sga[b2]], writes=[R_mtmp[m2]])
                    op("vector", lambda e, pa=pa, dc=dc, b2=b2, m2=m2: e.tensor_tensor(
                        out=mtmp2[m2][:], in0=pb[pa][:, TD:2 * TD], in1=sgb[b2][:, dc, :], op=ALU.mult),
                       reads=[R_pb[pa], R_sgb[b2]], writes=[R_mtmp2[m2]])
                    op("vector", lambda e, dc=dc, m2=m2: e.tensor_tensor(
                        out=mg[:, dc, :], in0=mtmp2[m2][:], in1=mtmp[m2][:], op=ALU.add),
                       reads=[R_mtmp[m2], R_mtmp2[m2]], writes=[R_mg])
                for bb in range(TD // 128):
                    blk = ti * (TD // 128) + bb
                    tb = blk * 128
                    q2 = bcnt % 2
                    q3 = bcnt % 3
                    bcnt += 1
                    q4 = (ti % 2) * 2 + bb
                    if len(pendingD) == 2:
                        router_q1(*pendingD[0])
                    pas = []
                    for hh in range(2):
                        pa = pbi % 8
                        pbi += 1
                        pas.append(pa)
                        for k in range(8):
                            op("tensor", lambda e, pa=pa, k=k, bb=bb, hh=hh: e.matmul(
                                pb[pa][:], lhsT=mg[:, k, bb * 128:(bb + 1) * 128], rhs=wout_t[:, k, hh * 512:(hh + 1) * 512],
                                start=(k == 0), stop=(k == 7)), reads=[R_mg, R_wD], writes=[R_pb[pa]])
                    if len(pendingD) == 2:
                        router_q2(*pendingD.pop(0))
                    for hh in range(2):
                        pa = pas[hh]
                        op("vector", lambda e, pa=pa, q2=q2, hh=hh, q4=q4: e.tensor_tensor(
                            out=x2_[q2][:, hh * 512:(hh + 1) * 512], in0=pb[pa][:], in1=xt_[q4][:, hh * 512:(hh + 1) * 512],
                            op=ALU.add), reads=[R_pb[pa], R_xt[q4]], writes=[R_x2[q2]])
                    op("sync", lambda e, q2=q2, tb=tb: e.dma_start(out=X2[tb:tb + 128, :], in_=x2_[q2][:]),
                       reads=[R_x2[q2]], writes=[R_scrD], dma=f"x2{q2}")
                    op("scalar", lambda e, q2=q2, q3=q3: e.activation(out=h2_[q3][:, 0:D], in_=x2_[q2][:], func=AF.Square, accum_out=sm[:, 0:1]),
                       reads=[R_x2[q2]], writes=[R_h2[q3], R_sm])
                    op("scalar", lambda e: e.activation(out=sm[:, 1:2], in_=sm[:, 0:1], func=AF.Sqrt, bias=eps_c[:, 0:1], scale=1.0 / D),
                       reads=RC, writes=[R_sm])
                    op("vector", lambda e: e.reciprocal(out=sm[:, 2:3], in_=sm[:, 1:2]), writes=[R_sm])
                    op("vector", lambda e, q2=q2, q3=q3: e.scalar_tensor_tensor(
                        out=h2_[q3][:, 0:D], in0=x2_[q2][:], scalar=sm[:, 2:3], in1=gffn_t[:], op0=ALU.mult, op1=ALU.mult),
                       reads=[R_x2[q2], R_sm, R_wD], writes=[R_h2[q3]])
                    pendingD.append((q3, blk, tb))
            while pendingD:
                router_q1(*pendingD[0])
                router_q2(*pendingD.pop(0))
            P.barrier()
            P.emit(glob)
    _phase4()
    if phases <= 4:
        return finish(nc, P, glob, out)

    def _phase5():
        with ExitStack() as ph:
            lo_t = sb(ph, "lo_t", [128, 16])
            hi_t = sb(ph, "hi_t", [128, 16])
            mid_t = sb(ph, "mid_t", [128, 16])
            cnt_t = sb(ph, "cnt_t", [128, 16])
            flg_t = sb(ph, "flg_t", [128, 16])
            t1_t = sb(ph, "t1_t", [128, 16])
            cmp_bf = sb(ph, "cmp_bf", [128, 1024], BF16)
            pos_f = sb(ph, "pos_f", [128, 1024])
            tot_f = sb(ph, "tot_f", [128, 1024])
            cum_f = sb(ph, "cum_f", [128, 1024])
            zero_f = sb(ph, "zero_f", [128, 64])
            off_i = sb(ph, "off_i", [128, 1024], I32)
            hrow = [sb(ph, f"hrow{i}", [128, XW]) for i in range(3)]
            xgr = [sb(ph, f"xgr{i}", [128, XW]) for i in range(2)]
            xgT = [sb(ph, f"xgT{i}", [128, 8, 1024], BF16) for i in range(2)]
            gate_t = [sb(ph, f"gate_t{i}", [128, 8]) for i in range(2)]
            tok_i = [sb(ph, f"tok_i{i}", [128, 8], I32) for i in range(2)]
            hidT = sb(ph, "hidT", [128, 16, 1024], BF16)
            wg_t = [sb(ph, f"wg_t{i}", [128, 8, 128], BF16) for i in range(3)]
            wu_t = [sb(ph, f"wu_t{i}", [128, 8, 128], BF16) for i in range(3)]
            wd_t = [sb(ph, f"wd_t{i}", [128, 16, 512], BF16) for i in range(2)]
            sg_t = [sb(ph, f"sg_t{i}", [128, 512]) for i in range(2)]
            eo_t = sb(ph, "eo_t", [128, 8, D])
            pb = [ph.enter_context(nc.psum_tensor(f"pbE{i}", [128, 512], F32)) for i in range(8)]
            R_pb = [Res(f"pbE{i}") for i in range(8)]
            R_bis, R_cmp, R_pos, R_off = Res("bis"), Res("cmp"), Res("pos"), Res("off")
            R_hrow = [Res(f"hrow{i}") for i in range(3)]
            R_xgr, R_xgT, R_gate = R2("xgr"), R2("xgT"), R2("gate")
            R_hid, R_eo = Res("hid"), Res("eo")
            R_wg = [Res(f"wg{i}") for i in range(3)]
            R_wu = [Res(f"wu{i}") for i in range(3)]
            R_wd, R_sg = R2("wd"), R2("sg")
            R_XG = [Res(f"XG{e}", acc=True) for e in range(16)]
            R_x2acc = Res("x2acc")
            aff_flat = AFFt[:].rearrange("p b e -> p (b e)")
            bc = lambda t: t[:].unsqueeze(1).to_broadcast([128, 64, 16])
            aff3 = AFFt[:]
            cmp3 = cmp_bf[:].rearrange("p (b e) -> p b e", e=16)
            op("vector", lambda e: e.memset(lo_t[:], 0.0), writes=[R_bis])
            op("vector", lambda e: e.memset(hi_t[:], 1.0), writes=[R_bis])
            op("vector", lambda e: e.memset(zero_f[:], 0.0), writes=[R_bis])
            for it in range(32):
                op("vector", lambda e: e.tensor_tensor(out=mid_t[:], in0=lo_t[:], in1=hi_t[:], op=ALU.add), writes=[R_bis])
                op("vector", lambda e: e.tensor_scalar(out=mid_t[:], in0=mid_t[:], scalar1=0.5, scalar2=None, op0=ALU.mult),
                   writes=[R_bis])
                op("vector", lambda e: e.tensor_tensor(out=cmp3, in0=aff3, in1=bc(mid_t), op=ALU.is_gt),
                   reads=[R_aff, R_bis], writes=[R_cmp])
                pa, pa2 = (2 * it) % 8, (2 * it + 1) % 8
                op("tensor", lambda e, pa=pa: e.matmul(pb[pa][:], lhsT=ones_bf[:], rhs=cmp_bf[:, 0:512], start=True, stop=True),
                   reads=[R_cmp] + RC, writes=[R_pb[pa]])
                op("tensor", lambda e, pa2=pa2: e.matmul(pb[pa2][:], lhsT=ones_bf[:], rhs=cmp_bf[:, 512:1024], start=True, stop=True),
                   reads=[R_cmp] + RC, writes=[R_pb[pa2]])
                op("vector", lambda e, pa=pa: e.tensor_reduce(
                    out=cnt_t[:], in_=pb[pa][:].rearrange("p (b e) -> p e b", e=16), axis=AX.X, op=ALU.add),
                   reads=[R_pb[pa]], writes=[R_bis])
                op("vector", lambda e, pa2=pa2: e.tensor_reduce(
                    out=t1_t[:], in_=pb[pa2][:].rearrange("p (b e) -> p e b", e=16), axis=AX.X, op=ALU.add),
                   reads=[R_pb[pa2]], writes=[R_bis])
                op("vector", lambda e: e.tensor_tensor(out=cnt_t[:], in0=cnt_t[:], in1=t1_t[:], op=ALU.add), writes=[R_bis])
                op("vector", lambda e: e.tensor_scalar(out=flg_t[:], in0=cnt_t[:], scalar1=1023.5, scalar2=None, op0=ALU.is_ge),
                   writes=[R_bis])
                op("vector", lambda e: e.tensor_tensor(out=t1_t[:], in0=flg_t[:], in1=mid_t[:], op=ALU.mult), writes=[R_bis])
                op("vector", lambda e: e.tensor_tensor(out=lo_t[:], in0=lo_t[:], in1=t1_t[:], op=ALU.max), writes=[R_bis])
                op("vector", lambda e: e.scalar_tensor_tensor(out=t1_t[:], in0=flg_t[:], scalar=2.0, in1=mid_t[:],
                                                              op0=ALU.mult, op1=ALU.add), writes=[R_bis])
                op("vector", lambda e: e.tensor_tensor(out=hi_t[:], in0=hi_t[:], in1=t1_t[:], op=ALU.min), writes=[R_bis])
            op("vector", lambda e: e.tensor_tensor(out=cmp3, in0=aff3, in1=bc(lo_t), op=ALU.is_gt),
               reads=[R_aff, R_bis], writes=[R_cmp])
            for hh in range(2):
                cs = slice(hh * 512, (hh + 1) * 512)
                op("tensor", lambda e, hh=hh, cs=cs: e.matmul(pb[hh][:], lhsT=ut_bf[:], rhs=cmp_bf[:, cs], start=True, stop=True),
                   reads=[R_cmp] + RC, writes=[R_pb[hh]])
                op("tensor", lambda e, hh=hh, cs=cs: e.matmul(pb[2 + hh][:], lhsT=ones_bf[:], rhs=cmp_bf[:, cs], start=True, stop=True),
                   reads=[R_cmp] + RC, writes=[R_pb[2 + hh]])
                op("vector", lambda e, hh=hh, cs=cs: e.tensor_copy(out=pos_f[:, cs], in_=pb[hh][:]), reads=[R_pb[hh]], writes=[R_pos])
                op("vector", lambda e, hh=hh, cs=cs: e.tensor_copy(out=tot_f[:, cs], in_=pb[2 + hh][:]), reads=[R_pb[2 + hh]], writes=[R_pos])
            tot3 = tot_f[:].rearrange("p (b e) -> p e b", e=16)
            cum3 = cum_f[:].rearrange("p (b e) -> p e b", e=16)
            for ee in range(16):
                op("vector", lambda e, ee=ee: e.tensor_tensor_scan(
                    out=cum3[:, ee, :], data0=tot3[:, ee, :], data1=zero_f[:], initial=0.0, op0=ALU.add, op1=ALU.add),
                   reads=[R_bis], writes=[R_pos])
            op("vector", lambda e: e.tensor_tensor(out=pos_f[:], in0=pos_f[:], in1=cum_f[:], op=ALU.add), writes=[R_pos])
            op("vector", lambda e: e.tensor_tensor(out=pos_f[:], in0=pos_f[:], in1=tot_f[:], op=ALU.subtract), writes=[R_pos])
            op("vector", lambda e: e.tensor_scalar(out=tot_f[:], in0=cmp_bf[:], scalar1=-1048576.0, scalar2=1048576.0,
                                                   op0=ALU.mult, op1=ALU.add), reads=[R_cmp], writes=[R_pos])
            op("vector", lambda e: e.tensor_tensor(out=pos_f[:], in0=pos_f[:], in1=tot_f[:], op=ALU.add), writes=[R_pos])
            op("vector", lambda e: e.tensor_copy(out=off_i[:], in_=pos_f[:]), reads=[R_pos], writes=[R_off])
            dcnt = [0]

            def seal_group(gn):
                for ee in range(4 * gn, 4 * gn + 4):
                    for h in range(3):
                        k = ("dma", f"sc{gn}_{h}")
                        if k in P.dma_vals:
                            R_XG[ee].w[k] = P.dma_vals[k]

            def dispatch_block(b, experts):
                gn_ = experts[0] // 4
                h3 = dcnt[0] % 3
                dcnt[0] += 1
                op("sync", lambda e, h3=h3, b=b: e.dma_start(out=hrow[h3][:], in_=H2X[b * 128:(b + 1) * 128, :]),
                   writes=[R_hrow[h3]], dma=f"hrow{h3}")
                for ee in experts:
                    col = b * 16 + ee
                    op("gpsimd", lambda e, h3=h3, ee=ee, col=col: e.indirect_dma_start(
                        out=XG[ee], out_offset=bass.IndirectOffsetOnAxis(ap=off_i[:, col:col + 1], axis=0),
                        in_=hrow[h3][:], in_offset=None, bounds_check=bc_reg(e, 1023), oob_is_err=False),
                       reads=[R_hrow[h3], R_off], writes=[R_XG[ee]], dma=f"sc{gn_}_{h3}")

            for b in range(64):
                dispatch_block(b, range(0, 4))
            seal_group(0)
            pbi = 0
            wci = 0
            sgi = 0

            def gather_expert(ee, sbks=range(8)):
                nonlocal pbi
                x2 = ee % 2
                for sbk in sbks:
                    g2 = (ee * 8 + sbk) % 2
                    op("sync", lambda e, g2=g2, ee=ee, sbk=sbk: e.dma_start(out=xgr[g2][:], in_=XG[ee][sbk * 128:(sbk + 1) * 128, :]),
                       reads=[R_XG[ee]], writes=[R_xgr[g2]], dma=f"xgr{g2}")
                    for half in range(2):
                        pa = pbi % 8
                        pbi += 1
                        for q in range(4):
                            k = half * 4 + q
                            op("tensor", lambda e, pa=pa, q=q, k=k, g2=g2: e.transpose(
                                out=pb[pa][:, q * 128:(q + 1) * 128], in_=xgr[g2][:, k * 128:(k + 1) * 128], identity=ident[:]),
                               reads=[R_xgr[g2]] + RC, writes=[R_pb[pa]])
                        dst = xgT[x2][:, half * 4:(half + 1) * 4, sbk * 128:(sbk + 1) * 128]
                        src = pb[pa][:].rearrange("p (a b) -> p a b", a=4)
                        if half == 0:
                            op("vector", lambda e, dst=dst, src=src: e.tensor_copy(out=dst, in_=src),
                               reads=[R_pb[pa]], writes=[R_xgT[x2]])
                        else:
                            op("scalar", lambda e, dst=dst, src=src: e.activation(out=dst, in_=src, func=AF.Copy),
                               reads=[R_pb[pa]], writes=[R_xgT[x2]])
                    op("gpsimd", lambda e, g2=g2, x2=x2, sbk=sbk, ee=ee: e.tensor_copy(
                        out=gate_t[x2][:, sbk:sbk + 1], in_=xgr[g2][:, D + ee:D + ee + 1]),
                       reads=[R_xgr[g2]], writes=[R_gate[x2]])
                    op("gpsimd", lambda e, g2=g2, x2=x2, sbk=sbk: e.tensor_copy(
                        out=tok_i[x2][:, sbk:sbk + 1], in_=xgr[g2][:, D + 16:D + 17]),
                       reads=[R_xgr[g2]], writes=[R_gate[x2]])

            R_accbar = [Res(f"accbar{e}", acc=True) for e in range(16)]

            def acc_scatter(ex, cb):
                x2_ = ex % 2
                rd = [R_eo, R_gate[x2_]] + ([R_accbar[ex - 1]] if ex > 0 else [])
                op("gpsimd", lambda e, cb=cb, x2_=x2_: e.indirect_dma_start(
                    out=X2, out_offset=bass.IndirectOffsetOnAxis(ap=tok_i[x2_][:, cb:cb + 1], axis=0),
                    in_=eo_t[:, cb, :], in_offset=None, bounds_check=bc_reg(e, S - 1), oob_is_err=True, compute_op=ALU.add),
                   reads=rd, writes=[R_accbar[ex]], dma="acc")

            def load_wd(ex, dh):
                op("gpsimd", lambda e, dh=dh, ex=ex: e.dma_start(out=wd_t[dh][:], in_=wd[ex, dh], max_dma_last_dim=4096),
                   writes=[R_wd[dh]], dma=f"wd{dh}")

            gather_expert(0)
            for ee in range(16):
                x2 = ee % 2
                for f in range(16):
                    w3 = wci % 3
                    wci += 1
                    op("gpsimd", lambda e, w3=w3, ee=ee, f=f: e.dma_start(out=wg_t[w3][:], in_=wg[ee, f]),
                       writes=[R_wg[w3]], dma=f"wg{w3}")
                    op("gpsimd", lambda e, w3=w3, ee=ee, f=f: e.dma_start(out=wu_t[w3][:], in_=wu[ee, f]),
                       writes=[R_wu[w3]], dma=f"wu{w3}")
                    if ee > 0 and 4 <= f < 12:
                        acc_scatter(ee - 1, f - 4)
                    if f == 2:
                        load_wd(ee, 1)
                        if ee == 0:
                            load_wd(0, 0)
                    if ee // 4 + 1 < 4:
                        gn = ee // 4 + 1
                        dispatch_block((ee % 4) * 16 + f, range(4 * gn, 4 * gn + 4))
                        if ee % 4 == 3 and f == 15:
                            seal_group(gn)
                    for half in range(2):
                        hs = slice(half * 512, (half + 1) * 512)
                        pg = pbi % 8
                        pbi += 1
                        pu = pbi % 8
                        pbi += 1
                        for k in range(8):
                            op("tensor", lambda e, pg=pg, k=k, w3=w3, x2=x2, hs=hs: e.matmul(
                                pb[pg][:], lhsT=wg_t[w3][:, k, :], rhs=xgT[x2][:, k, hs], start=(k == 0), stop=(k == 7)),
                               reads=[R_wg[w3], R_xgT[x2]], writes=[R_pb[pg]])
                        for k in range(8):
                            op("tensor", lambda e, pu=pu, k=k, w3=w3, x2=x2, hs=hs: e.matmul(
                                pb[pu][:], lhsT=wu_t[w3][:, k, :], rhs=xgT[x2][:, k, hs], start=(k == 0), stop=(k == 7)),
                               reads=[R_wu[w3], R_xgT[x2]], writes=[R_pb[pu]])
                        s2 = sgi % 2
                        sgi += 1
                        op("scalar", lambda e, pg=pg, s2=s2: e.activation(out=sg_t[s2][:], in_=pb[pg][:], func=AF.Silu),
                           reads=[R_pb[pg]], writes=[R_sg[s2]])
                        op("vector", lambda e, pu=pu, s2=s2, f=f, hs=hs: e.tensor_tensor(
                            out=hidT[:, f, hs], in0=pb[pu][:], in1=sg_t[s2][:], op=ALU.mult),
                           reads=[R_pb[pu], R_sg[s2]], writes=[R_hid])
                gidx = 0
                for dh in range(2):
                    if dh == 1 and ee + 1 < 16:
                        load_wd(ee + 1, 0)
                    for cb in range(8):
                        if ee + 1 < 16 and gidx % 2 == 0:
                            gather_expert(ee + 1, [gidx // 2])
                        gidx += 1
                        pa = pbi % 8
                        pbi += 1
                        for f in range(16):
                            op("tensor", lambda e, pa=pa, f=f, cb=cb, dh=dh: e.matmul(
                                pb[pa][:], lhsT=hidT[:, f, cb * 128:(cb + 1) * 128], rhs=wd_t[dh][:, f, :],
                                start=(f == 0), stop=(f == 15)), reads=[R_hid, R_wd[dh]], writes=[R_pb[pa]])
                        if cb % 2 == 0:
                            op("vector", lambda e, pa=pa, cb=cb, dh=dh, x2=x2: e.tensor_scalar(
                                out=eo_t[:, cb, dh * 512:(dh + 1) * 512], in0=pb[pa][:], scalar1=gate_t[x2][:, cb:cb + 1],
                                scalar2=None, op0=ALU.mult), reads=[R_pb[pa], R_gate[x2]], writes=[R_eo])
                        else:
                            op("scalar", lambda e, pa=pa, cb=cb, dh=dh, x2=x2: e.activation(
                                out=eo_t[:, cb, dh * 512:(dh + 1) * 512], in_=pb[pa][:], func=AF.Copy,
                                scale=gate_t[x2][:, cb:cb + 1]), reads=[R_pb[pa], R_gate[x2]], writes=[R_eo])
                if ee == 15:
                    for cb in range(8):
                        acc_scatter(15, cb)
            P.barrier()
            P.emit(glob)
    _phase5()
    if phases <= 5:
        return finish(nc, P, glob, out)

    def _phase6():
        with ExitStack() as ph:
            gfin_t = sb(ph, "gfin_t", [128, D])
            xa = [sb(ph, f"xa{i}", [128, D]) for i in range(3)]
            ya = [sb(ph, f"ya{i}", [128, D]) for i in range(3)]
            junk = sb(ph, "junkF", [128, D])
            sm = sb(ph, "smF", [128, 4])
            R_xa = [Res(f"xa{i}") for i in range(3)]
            R_ya = [Res(f"ya{i}") for i in range(3)]
            R_junk, R_sm, R_g = Res("junkF"), Res("smF"), Res("gfin")
            R_out = Res("out", acc=True)
            op("sync", lambda e: e.dma_start(out=gfin_t[:], in_=gfin), writes=[R_g], dma="gfin")
            def loadF(b):
                op("sync", lambda e, b=b: e.dma_start(out=xa[b % 3][:], in_=X2[b * 128:(b + 1) * 128, :]),
                   writes=[R_xa[b % 3]], dma=f"xa{b % 3}")

            loadF(0)
            loadF(1)
            for b in range(64):
                b3 = b % 3
                if b + 2 < 64:
                    loadF(b + 2)
                op("scalar", lambda e, b3=b3: e.activation(out=junk[:], in_=xa[b3][:], func=AF.Square, accum_out=sm[:, 0:1]),
                   reads=[R_xa[b3]], writes=[R_junk, R_sm])
                op("scalar", lambda e: e.activation(out=sm[:, 1:2], in_=sm[:, 0:1], func=AF.Sqrt, bias=eps_c[:, 0:1], scale=1.0 / D),
                   reads=RC, writes=[R_sm])
                op("vector", lambda e: e.reciprocal(out=sm[:, 2:3], in_=sm[:, 1:2]), writes=[R_sm])
                op("vector", lambda e, b3=b3: e.scalar_tensor_tensor(
                    out=ya[b3][:], in0=xa[b3][:], scalar=sm[:, 2:3], in1=gfin_t[:], op0=ALU.mult, op1=ALU.mult),
                   reads=[R_xa[b3], R_sm, R_g], writes=[R_ya[b3]])
                op("sync", lambda e, b3=b3, b=b: e.dma_start(out=out[b * 128:(b + 1) * 128, :], in_=ya[b3][:]),
                   reads=[R_ya[b3]], writes=[R_out], dma=f"ya{b3}")
            P.barrier()
            P.emit(glob)
    _phase6()
    return finish(nc, P, glob, out)


def finish(nc, P, glob, out):
    P.barrier()
    P.emit(glob)
    glob.close()
    return nc


def _const_tables():
    ident = np.eye(128, dtype=np.float32)
    ut = np.triu(np.ones((128, 128), np.float32), 1)
    i = np.arange(128)[:, None]
    c = np.arange(256)[None, :]
    c = np.arange(384)[None, :]
    mask = ((c >= i + 64) & (c <= i + 192)).astype(np.float32)
    tokb = (np.arange(64)[None, :] * 128 + np.arange(128)[:, None]).astype(np.float32)
    cst = np.concatenate([ident, ut, mask, tokb], axis=1)
    pos = np.arange(S, dtype=np.float32)
    inv = (np.float32(500000.0) ** (-np.arange(0, 32, 2, dtype=np.float32) / np.float32(32))).astype(np.float32)
    ang = (pos[None, :] * inv[:, None]).astype(np.float32)
    cos, sin = np.cos(ang).astype(np.float32), np.sin(ang).astype(np.float32)
    one = np.ones((16, S), np.float32)
    zero = np.zeros((16, S), np.float32)
    cosT = np.concatenate([cos, one, cos, one], axis=0)
    sinT = np.concatenate([sin, zero, -sin, zero], axis=0)
    return cst, np.ascontiguousarray(cosT), np.ascontiguousarray(sinT)


HEAD_PERM = np.array(list(range(16)) + list(range(32, 48)) + list(range(16, 32)) + list(range(48, 128)))


def prep_shared(inp):
    f = lambda a: np.ascontiguousarray(np.asarray(a, dtype=np.float32))
    w_in = f(inp["w_in"])[0]
    b_in = f(inp["b_in"])[0]
    colperm = np.arange(8704)
    for c in range(16, 40):
        colperm[c * 128:(c + 1) * 128] = c * 128 + HEAD_PERM
    w_in = w_in[:, colperm]
    b_in = b_in[colperm]
    cst, cosT, sinT = _const_tables()
    sh = {
        "w_in": f(w_in.reshape(8, 128, NCH, 128).transpose(2, 1, 0, 3)),
        "b_in": f(b_in.reshape(NCH, 128).T),
        "gmix": f(inp["norm_mix"][0].reshape(8, 128).T),
        "convw": f(np.asarray(inp["conv_w"])[0].reshape(4, 8, 128).transpose(2, 0, 1)),
        "convb": f(np.asarray(inp["conv_b"])[0].reshape(8, 128).T),
        "rgw": f(np.asarray(inp["rg_w"])[0].reshape(4, 8, 128, 128).transpose(1, 2, 0, 3)),
        "rgb": f(np.asarray(inp["rg_b"])[0].reshape(4, 8, 128).transpose(2, 0, 1)),
        "lam": f(np.asarray(inp["rg_lambda"])[0].reshape(2, 8, 128).transpose(2, 0, 1)),
        "prnn": f(np.asarray(inp["p_rnn"])[0].reshape(8, 128, D).transpose(1, 0, 2)),
        "pattn": f(np.asarray(inp["p_attn"])[0].reshape(4, 128, D).transpose(1, 0, 2)),
        "wout": f(np.asarray(inp["w_out"])[0].reshape(8, 128, D).transpose(1, 0, 2)),
        "gffn": f(np.broadcast_to(np.asarray(inp["norm_ffn"])[0][None, :], (128, D))),
        "gfin": f(np.broadcast_to(np.asarray(inp["norm_final"])[None, :], (128, D))),
        "wr": f(np.asarray(inp["w_router"])[0].reshape(8, 128, 16).transpose(1, 0, 2)),
        "br": f(np.broadcast_to(np.asarray(inp["b_router"])[0][None, :], (128, 16))),
        "wg": f(np.asarray(inp["w_gate"])[0].reshape(16, 8, 128, 16, 128).transpose(0, 3, 2, 1, 4)),
        "wu": f(np.asarray(inp["w_up"])[0].reshape(16, 8, 128, 16, 128).transpose(0, 3, 2, 1, 4)),
        "wd": f(np.asarray(inp["w_down"])[0].reshape(16, 16, 128, 2, 512).transpose(0, 3, 2, 1, 4)),
        "cosT": cosT, "sinT": sinT, "cst": cst,
    }
    return sh


_NC_CACHE = {}


def kernel(**inputs):
    x = np.asarray(inputs["x"], dtype=np.float32)
    B = x.shape[0]
    sh = prep_shared(inputs)
    if "nc" not in _NC_CACHE:
        _NC_CACHE["nc"] = build()
    nc = _NC_CACHE["nc"]
    in_maps = []
    for b in range(B):
        m = dict(sh)
        m["xtok"] = np.ascontiguousarray(x[b])
        m["xT"] = np.ascontiguousarray(x[b].T)
        in_maps.append(m)
    res = run_bass_kernel_spmd(nc, in_maps, core_ids=list(range(B)))
    return np.stack([np.asarray(r["out"], dtype=np.float32) for r in res.results], axis=0)
```
